# Optimizing a Trainium2 kernel written in Bass

```python
import jax, jax.numpy as jnp
from jax import lax
import numpy as np

D_MODEL = 4096
BATCH = 2
SEQ = 8192
DEPTH = 1

MIX_WIDTH = D_MODEL
HEAD_DIM = 128
ATTN_WIDTH = MIX_WIDTH // 2
CONV_WIDTH = MIX_WIDTH - ATTN_WIDTH
N_Q_HEADS = ATTN_WIDTH // HEAD_DIM
N_KV_HEADS = 4
GQA_GROUP = N_Q_HEADS // N_KV_HEADS
KV_WIDTH = N_KV_HEADS * HEAD_DIM
CONV_K = 3
IN_COLS = ATTN_WIDTH + 2 * KV_WIDTH + 3 * CONV_WIDTH
SPLIT_POINTS = (ATTN_WIDTH,
                ATTN_WIDTH + KV_WIDTH,
                ATTN_WIDTH + 2 * KV_WIDTH,
                ATTN_WIDTH + 2 * KV_WIDTH + CONV_WIDTH,
                ATTN_WIDTH + 2 * KV_WIDTH + 2 * CONV_WIDTH)
GRID_W = 64
ROPE_THETA = 10000.0
Q_BLOCK = 128
N_KEYS = 128
N_EXPERTS = N_KEYS * N_KEYS
PEER_HEADS = 8
PEER_TOPK = 16
PEER_KEY_DIM = 128
PEER_QUERY_DIM = 2 * PEER_KEY_DIM
PEER_BLOCK = 128
EPS = 1e-6

kernel_name = "hymba_conv_axialgqa_peer_encoder_block"


def rms_norm(x, g):
    x32 = x.astype(jnp.float32)
    y = x32 * lax.rsqrt(jnp.mean(x32 * x32, axis=-1, keepdims=True) + EPS)
    return (y * g.astype(jnp.float32)).astype(x.dtype)


def axial_angles(seq_len):
    rows = seq_len // GRID_W
    row = jnp.repeat(jnp.arange(rows, dtype=jnp.float32), GRID_W)
    col = jnp.tile(jnp.arange(GRID_W, dtype=jnp.float32), rows)
    half = HEAD_DIM // 2
    inv = ROPE_THETA ** (-jnp.arange(0, half, 2, dtype=jnp.float32) / half)
    return row[:, None] * inv, col[:, None] * inv


def rotate(x, ang):
    cos = jnp.cos(ang)[:, None, :].astype(x.dtype)
    sin = jnp.sin(ang)[:, None, :].astype(x.dtype)
    x1, x2 = jnp.split(x, 2, axis=-1)
    return jnp.concatenate([x1 * cos - x2 * sin, x1 * sin + x2 * cos], axis=-1)


def apply_axial_rope(x, ang_row, ang_col):
    x_row, x_col = jnp.split(x, 2, axis=-1)
    return jnp.concatenate([rotate(x_row, ang_row), rotate(x_col, ang_col)], axis=-1)


def bidir_gqa(q, k, v):
    b, s, _, _ = q.shape
    nb = s // Q_BLOCK
    qb = q.reshape(b, nb, Q_BLOCK, N_KV_HEADS, GQA_GROUP, HEAD_DIM).transpose(1, 0, 2, 3, 4, 5)
    scale = HEAD_DIM ** -0.5

    def block(qi):
        sc = jnp.einsum('bqkgd,bskd->bkgqs', qi, k, preferred_element_type=jnp.float32) * scale
        p = jax.nn.softmax(sc, axis=-1).astype(v.dtype)
        return jnp.einsum('bkgqs,bskd->bqkgd', p, v)

    o = lax.map(block, qb)
    return o.transpose(1, 0, 2, 3, 4, 5).reshape(b, s, ATTN_WIDTH)


def short_conv(u, w):
    up = jnp.pad(u, ((0, 0), (1, 1), (0, 0)))
    return up[:, :-2] * w[0] + up[:, 1:-1] * w[1] + up[:, 2:] * w[2]


def hybrid_mixer(h, w_in, q_norm_g, k_norm_g, conv_w, attn_out_g, conv_out_g, w_out):
    b, s, _ = h.shape
    proj = h @ w_in
    q, k, v, cx, gb, gc = jnp.split(proj, SPLIT_POINTS, axis=-1)
    q = rms_norm(q.reshape(b, s, N_Q_HEADS, HEAD_DIM), q_norm_g)
    k = rms_norm(k.reshape(b, s, N_KV_HEADS, HEAD_DIM), k_norm_g)
    v = v.reshape(b, s, N_KV_HEADS, HEAD_DIM)
    ang_row, ang_col = axial_angles(s)
    q = apply_axial_rope(q, ang_row, ang_col)
    k = apply_axial_rope(k, ang_row, ang_col)
    attn = bidir_gqa(q, k, v)
    conv = gb * short_conv(gc * cx, conv_w)
    y = jnp.concatenate([rms_norm(attn, attn_out_g), rms_norm(conv, conv_out_g)], axis=-1)
    return y @ w_out


def peer_ffn(h, w_query, sub_keys, w_down, w_up):
    b, s, d = h.shape
    q = (h @ w_query).reshape(b, s, PEER_HEADS, 2, PEER_KEY_DIM)
    sc = jnp.einsum('bshpd,hpnd->bshpn', q, sub_keys)
    v_top, i_top = lax.top_k(sc, PEER_TOPK)
    cand_s = (v_top[..., 0, :, None] + v_top[..., 1, None, :]).reshape(
        b, s, PEER_HEADS, PEER_TOPK * PEER_TOPK)
    cand_i = (i_top[..., 0, :, None] * N_KEYS + i_top[..., 1, None, :]).reshape(
        b, s, PEER_HEADS, PEER_TOPK * PEER_TOPK)
    best_s, pos = lax.top_k(cand_s, PEER_TOPK)
    idx = jnp.take_along_axis(cand_i, pos, axis=-1)
    gate = jax.nn.softmax(best_s.astype(jnp.float32), axis=-1).astype(h.dtype)
    nblk = (b * s) // PEER_BLOCK
    xs = h.reshape(nblk, PEER_BLOCK, d)
    idx = idx.reshape(nblk, PEER_BLOCK, PEER_HEADS, PEER_TOPK)
    gate = gate.reshape(nblk, PEER_BLOCK, PEER_HEADS, PEER_TOPK)

    def block(args):
        xb, ib, gbk = args
        u = jnp.take(w_down, ib, axis=0)
        a = jax.nn.gelu(jnp.einsum('thkd,td->thk', u, xb), approximate=False) * gbk
        vv = jnp.take(w_up, ib, axis=0)
        return jnp.einsum('thk,thkd->td', a, vv)

    out = lax.map(block, (xs, idx, gate))
    return out.reshape(b, s, d)


def setup_inputs(seed: int = 0) -> dict:
    key = jax.random.key(seed)
    ks = jax.random.split(key, 16)
    f32 = jnp.float32

    def nrm(k, shape, scale):
        return jax.random.normal(k, shape, f32) * scale

    def gain(k, shape):
        return 1.0 + 0.02 * jax.random.normal(k, shape, f32)

    return {
        "x": nrm(ks[0], (BATCH, SEQ, D_MODEL), 1.0),
        "norm_mix_g": gain(ks[1], (DEPTH, D_MODEL)),
        "w_in": nrm(ks[2], (DEPTH, D_MODEL, IN_COLS), D_MODEL ** -0.5),
        "q_norm_g": gain(ks[3], (DEPTH, HEAD_DIM)),
        "k_norm_g": gain(ks[4], (DEPTH, HEAD_DIM)),
        "conv_w": nrm(ks[5], (DEPTH, CONV_K, CONV_WIDTH), CONV_K ** -0.5),
        "attn_out_g": gain(ks[6], (DEPTH, ATTN_WIDTH)),
        "conv_out_g": gain(ks[7], (DEPTH, CONV_WIDTH)),
        "w_out": nrm(ks[8], (DEPTH, MIX_WIDTH, D_MODEL), MIX_WIDTH ** -0.5),
        "norm_ffn_g": gain(ks[9], (DEPTH, D_MODEL)),
        "peer_w_query": nrm(ks[10], (DEPTH, D_MODEL, PEER_HEADS * PEER_QUERY_DIM), D_MODEL ** -0.5),
        "peer_sub_keys": nrm(ks[11], (DEPTH, PEER_HEADS, 2, N_KEYS, PEER_KEY_DIM), PEER_KEY_DIM ** -0.5),
        "peer_w_down": nrm(ks[12], (DEPTH, N_EXPERTS, D_MODEL), D_MODEL ** -0.5),
        "peer_w_up": nrm(ks[13], (DEPTH, N_EXPERTS, D_MODEL), 0.5),
        "norm_final_g": gain(ks[14], (D_MODEL,)),
    }


def reference(x, norm_mix_g, w_in, q_norm_g, k_norm_g, conv_w, attn_out_g, conv_out_g,
              w_out, norm_ffn_g, peer_w_query, peer_sub_keys, peer_w_down, peer_w_up,
              norm_final_g):
    for layer in range(DEPTH):
        h = rms_norm(x, norm_mix_g[layer])
        x = x + hybrid_mixer(h, w_in[layer], q_norm_g[layer], k_norm_g[layer], conv_w[layer],
                             attn_out_g[layer], conv_out_g[layer], w_out[layer])
        h = rms_norm(x, norm_ffn_g[layer])
        x = x + peer_ffn(h, peer_w_query[layer], peer_sub_keys[layer],
                         peer_w_down[layer], peer_w_up[layer])
    return rms_norm(x, norm_final_g)
```

```python
import numpy as np
from contextlib import ExitStack
import ml_dtypes
import concourse.bass as bass
import concourse.mybir as mybir
from concourse.bass_utils import run_bass_kernel_spmd

F32 = mybir.dt.float32
BF16 = mybir.dt.bfloat16
U32 = mybir.dt.uint32
AF = mybir.ActivationFunctionType
ALU = mybir.AluOpType
AX = mybir.AxisListType
ENGS = ["pe", "act", "dve", "pool", "sp"]
EPS = 1e-6
NEG = -1.0e30


class Prog:
    R = 8

    def __init__(self, nc):
        self.nc = nc
        self.pending = []
        self.gid = 0
        self.sig = {}
        self.eng_of = {}
        self.isdma = {}
        self.last_write = {}
        self.readers = {}
        self.cnt = {e: 0 for e in ENGS}
        self.known = {e: {} for e in ENGS}
        self.last_op = {e: None for e in ENGS}
        self.dma_n = {"sp": 0, "pool": 0, "act": 0}
        self.dma_hist = {"sp": [], "pool": [], "act": []}
        self.stack = ExitStack()
        self.sem = {e: self.stack.enter_context(nc.semaphore("s_" + e)) for e in ENGS}
        self.ring = {q: [self.stack.enter_context(nc.semaphore(f"r_{q}{i}")) for i in range(self.R)]
                     for q in ("sp", "pool", "act")}

    def add(self, eng, fn, r=(), w=(), dma=False):
        w = tuple(w) + tuple(k for k in r if k.startswith("ps") and k not in w)
        self.pending.append(dict(eng=eng, fn=fn, r=tuple(r), w=tuple(w), dma=dma, gid=self.gid, barrier=False))
        self.gid += 1

    def dma(self, q, out, in_, r=(), w=()):
        self.add(q, lambda e, o=out, i=in_: e.dma_start(out=o, in_=i), r, w, dma=True)

    def barrier(self):
        for e in ENGS:
            self.pending.append(dict(eng=e, fn=None, r=(), w=(), dma=False, gid=self.gid, barrier=True))
            self.gid += 1

    def flush(self):
        self.barrier()
        ops = self.pending
        self.pending = []
        needed = set()
        last_op = dict(self.last_op)
        dma_hist = {q: list(v) for q, v in self.dma_hist.items()}
        dma_n = dict(self.dma_n)
        for op in ops:
            g = op["gid"]
            self.eng_of[g] = op["eng"]
            self.isdma[g] = op["dma"]
            deps = set()
            if op["barrier"]:
                for e in ENGS:
                    if last_op[e] is not None:
                        deps.add(last_op[e])
                for q in dma_hist:
                    deps.update(dma_hist[q][-self.R:])
            else:
                for k in op["r"]:
                    if k in self.last_write:
                        deps.add(self.last_write[k])
                for k in op["w"]:
                    if k in self.last_write:
                        deps.add(self.last_write[k])
                    deps.update(self.readers.get(k, ()))
                for k in op["w"]:
                    self.last_write[k] = g
                    self.readers[k] = []
                for k in op["r"]:
                    if k not in op["w"]:
                        lst = self.readers.setdefault(k, [])
                        if not op["dma"]:
                            lst[:] = [x for x in lst if self.isdma[x] or self.eng_of[x] != op["eng"]]
                        lst.append(g)
                if op["dma"]:
                    q = op["eng"]
                    n = dma_n[q]
                    if n >= self.R:
                        deps.add(dma_hist[q][n - self.R])
                    dma_hist[q].append(g)
                    dma_n[q] = n + 1
                else:
                    last_op[op["eng"]] = g
            deps.discard(g)
            op["deps"] = deps
            for d in deps:
                if not self.isdma[d]:
                    needed.add(d)
        for op in ops:
            g = op["gid"]
            if op["barrier"]:
                continue
            e = op["eng"]
            if op["dma"]:
                n = self.dma_n[e]
                self.sig[g] = (self.ring[e][n % self.R], 16 * (n // self.R + 1))
                self.dma_n[e] = n + 1
                self.dma_hist[e].append(g)
                op["inc"] = True
            else:
                self.last_op[e] = g
                if g in needed:
                    self.cnt[e] += 1
                    self.sig[g] = (self.sem[e], self.cnt[e])
                    op["inc"] = True
                else:
                    op["inc"] = False
        for op in ops:
            e = op["eng"]
            waits = {}
            for d in sorted(op["deps"]):
                if (not self.isdma[d]) and self.eng_of[d] == "pe" and e == "pe":
                    continue
                s, v = self.sig[d]
                key = id(s)
                if self.known[e].get(key, 0) >= v:
                    continue
                if key not in waits or waits[key][1] < v:
                    waits[key] = (s, v)
            for key, (s, v) in waits.items():
                self.known[e][key] = v
            op["waits"] = list(waits.values())
        per = {e: [o for o in ops if o["eng"] == e] for e in ENGS}

        def run(eng_obj, lst):
            for op in lst:
                for (s, v) in op["waits"]:
                    eng_obj.wait_ge(s, v)
                if op["fn"] is None:
                    continue
                ins = op["fn"](eng_obj)
                if op["inc"]:
                    s, v = self.sig[op["gid"]]
                    ins.then_inc(s, 16 if op["dma"] else 1)

        with self.nc.Block() as block:
            @block.tensor
            def _(e):
                run(e, per["pe"])

            @block.scalar
            def _(e):
                run(e, per["act"])

            @block.vector
            def _(e):
                run(e, per["dve"])

            @block.gpsimd
            def _(e):
                run(e, per["pool"])

            @block.sync
            def _(e):
                run(e, per["sp"])
        self.last_write.clear()
        self.readers.clear()

    def close(self):
        self.stack.close()


class Ring:
    def __init__(self, st, nc, name, n, shape, dt):
        self.t = [st.enter_context(nc.sbuf_tensor(f"{name}{i}", shape, dt)) for i in range(n)]
        self.name = name
        self.i = 0

    def next(self):
        k = self.i % len(self.t)
        self.i += 1
        return self.t[k], f"{self.name}{k}"


class Cfg:
    def __init__(self, D=4096, SB=8192):
        self.D = D
        self.SB = SB
        self.OWN = SB // 4
        self.NCH = D // 128
        self.ATT = D // 2
        self.NQH = self.ATT // 128
        self.NKV = max(1, self.NQH // 4)
        self.GQ = self.NQH // self.NKV
        self.KVW = self.NKV * 128
        self.CW = D - self.ATT
        self.NCB = self.CW // 128
        self.INC = self.ATT + 2 * self.KVW + 3 * self.CW
        self.NT = SB // 128
        self.NTO = self.OWN // 128
        self.PQ = 2048
        self.NE = 16384


def mm_group(P, out_ap, pairs, r, w):
    def fn(e, pairs=pairs, out_ap=out_ap):
        n = len(pairs)
        ins = None
        for i, (l, rh) in enumerate(pairs):
            ins = e.matmul(out_ap, l, rh, start=(i == 0), stop=(i == n - 1))
        return ins
    P.add("pe", fn, r=r, w=w)


def build(c, debug=False):
    nc = bass.Bass("TRN2", target_bir_lowering=False)
    D, SB, OWN, NCH, ATT, NQH, NKV, GQ, KVW, CW, NCB, INC, NT, NTO, PQ, NE = (
        c.D, c.SB, c.OWN, c.NCH, c.ATT, c.NQH, c.NKV, c.GQ, c.KVW, c.CW, c.NCB, c.INC, c.NT, c.NTO, c.PQ, c.NE)
    NYC = NQH + NCB

    def din(name, shape, dt=F32):
        return nc.dram_tensor(name, shape, dt, kind="ExternalInput").ap()

    def dscr(name, shape, dt):
        return nc.dram_tensor(name, shape, dt, kind=("ExternalOutput" if debug else "Internal")).ap()

    x = din("x", [SB, D])
    w_in = din("w_in", [D, INC])
    w_out = din("w_out", [D, D])
    w_query = din("w_query", [D, PQ])
    sub_keys = din("sub_keys", [16, 128, 128])
    w_down = din("w_down", [NE, D])
    w_up = din("w_up", [NE, D])
    g_mix = din("g_mix", [1, D])
    g_ffn = din("g_ffn", [1, D])
    g_fin = din("g_fin", [1, D])
    qk_g = din("qk_g", [128, 2])
    conv_w = din("conv_w", [128, NCB, 3])
    ao_g = din("ao_g", [128, NQH])
    co_g = din("co_g", [128, NCB])
    cosT = din("cosT", [128, SB])
    sinT = din("sinT", [128, SB])
    ident_d = din("ident", [128, 128], BF16)
    identf_d = din("identf", [128, 128])
    perm_d = din("perm", [128, 128], BF16)
    iota_d = din("iota", [128, 128])
    edge_d = din("edge", [128, 2])
    out = nc.dram_tensor("out", [OWN, D], F32, kind="ExternalOutput").ap()

    hT = dscr("hT", [NT, 128, NCH, 128], BF16)
    KT = dscr("KT", [NKV, 128, SB], BF16)
    Vs = dscr("Vs", [NKV, 128, NT, 128], BF16)
    QT = dscr("QT", [NQH, 128, OWN], BF16)
    YT = dscr("YT", [NYC, 128, OWN], BF16)
    X1 = dscr("X1", [OWN, D], F32)
    H2T = dscr("H2T", [NTO, 128, NCH, 128], BF16)
    QPT = dscr("QPT", [16, 128, OWN], BF16)
    WDT = dscr("WDT", [128, 128, NCH, 128], BF16)
    GS = dscr("GS", [NTO, 128, 128, 128], BF16)
    STA = dscr("STA", [128, 2 * NTO], F32)
    NWT = (ATT + 3 * CW) // 256
    WB = dscr("WB", [NWT, 128, NCH, 256], BF16)

    def wb_col0(i):
        return i * 256 if i < ATT // 256 else (ATT + 2 * KVW) + (i - ATT // 256) * 256

    P = Prog(nc)
    gst = ExitStack()

    def gsb(name, shape, dt):
        return gst.enter_context(nc.sbuf_tensor(name, shape, dt))

    psall = gst.enter_context(nc.psum_tensor("psall", [128, 4096], F32))
    ps = [psall[:, i * 512:(i + 1) * 512] for i in range(8)]
    ident = gsb("identS", [128, 128], BF16)
    identf = gsb("identfS", [128, 128], F32)
    ones = gsb("onesS", [128, 128], BF16)
    onesf = gsb("onesfS", [128, 128], F32)
    epsT = gsb("epsT", [128, 1], F32)
    consts = ["ident", "identf", "ones", "onesf", "eps"]

    def load_consts():
        P.dma("sp", ident[:], ident_d, w=["ident"])
        P.dma("sp", identf[:], identf_d, w=["identf"])
        P.add("dve", lambda e: e.memset(ones[:], 1.0), w=["ones"])
        P.add("dve", lambda e: e.memset(onesf[:], 1.0), w=["onesf"])
        P.add("dve", lambda e: e.memset(epsT[:], EPS), w=["eps"])

    def phase_norm_T(src, gain_d, dst, ntiles, tag, extra=None):
        with ExitStack() as st:
            extra_fn = extra(st) if extra is not None else None
            def sb(name, shape, dt):
                return st.enter_context(nc.sbuf_tensor(tag + name, shape, dt))
            gB = sb("gB", [128, D], F32)
            P.dma("sp", gB[:], gain_d.partition_broadcast(128), w=["gB"])
            xt = Ring(st, nc, tag + "xt", 2, [128, D], F32)
            junk = Ring(st, nc, tag + "junk", 2, [128, D], BF16)
            ss = Ring(st, nc, tag + "ss", 2, [128, 1], F32)
            hb = Ring(st, nc, tag + "hb", 2, [128, D], BF16)
            hTt = Ring(st, nc, tag + "hTt", 2, [128, NCH, 128], BF16)
            bi = 0
            for t in range(ntiles):
                xtt, xk = xt.next()
                jt, jk = junk.next()
                sst, sk = ss.next()
                hbt, hk = hb.next()
                htt, htk = hTt.next()
                P.dma("sp", xtt[:], src[t * 128:(t + 1) * 128, :], w=[xk])
                P.add("dve", lambda e, a=jt, b=xtt, s=sst: e.scalar_tensor_tensor(
                    out=a[:], in0=b[:], scalar=1.0, in1=b[:], op0=ALU.mult, op1=ALU.mult, accum_out=s[:]),
                    r=[xk], w=[jk, sk])
                P.add("act", lambda e, s=sst: e.activation(out=s[:], in_=s[:], func=AF.Sqrt, scale=1.0 / D,
                                                           bias=epsT[:, 0:1]), r=[sk, "eps"], w=[sk])
                P.add("dve", lambda e, s=sst: e.reciprocal(out=s[:], in_=s[:]), r=[sk], w=[sk])
                P.add("dve", lambda e, a=hbt, b=xtt, s=sst: e.scalar_tensor_tensor(
                    out=a[:], in0=b[:], scalar=s[:, 0:1], in1=gB[:], op0=ALU.mult, op1=ALU.mult),
                    r=[xk, sk, "gB"], w=[hk])
                for c0 in range(0, NCH, 8):
                    bk = bi % 4
                    bi += 1
                    pb = ps[bk][:].bitcast(BF16)

                    def tr(e, hbt=hbt, c0=c0, pb=pb):
                        ins = None
                        for cc in range(c0, c0 + 8):
                            ins = e.transpose(out=pb[:, (cc - c0) * 128:(cc - c0 + 1) * 128],
                                              in_=hbt[:, cc * 128:(cc + 1) * 128], identity=ident[:])
                        return ins
                    P.add("pe", tr, r=[hk, "ident"], w=[f"ps{bk}"])
                    P.add("act", lambda e, htt=htt, c0=c0, pb=pb: e.copy(
                        out=htt[:, c0:c0 + 8, :], in_=pb.rearrange("p (c t) -> p c t", c=8)),
                        r=[f"ps{bk}"], w=[htk])
                P.dma("act", dst[t], htt[:], r=[htk], w=[tag + "dst"])
                if extra_fn is not None:
                    extra_fn(t, ntiles)
            P.flush()

    def make_rope(st, tag):
        tmp = dict(
            qg=Ring(st, nc, tag + "qg", 2, [128, 512], BF16),
            sq=Ring(st, nc, tag + "sq", 2, [128, 512], BF16),
            rst=Ring(st, nc, tag + "rst", 2, [128, 512], F32),
            t1=Ring(st, nc, tag + "t1", 2, [128, 512], F32),
            t2=Ring(st, nc, tag + "t2", 2, [128, 512], F32),
        )
        return tmp

    def rope_epilogue(tmp, pq, pqk, gcol, Ct, Ck, St, Sk, perm, outt, outk, b1, b2):
        qg, qgk = tmp["qg"].next()
        sq, sqk = tmp["sq"].next()
        rst, rsk = tmp["rst"].next()
        t1, t1k = tmp["t1"].next()
        t2, t2k = tmp["t2"].next()
        P.add("dve", lambda e: e.tensor_scalar(out=qg[:], in0=pq, scalar1=gcol, scalar2=None, op0=ALU.mult),
              r=[pqk, "qkg"], w=[qgk])
        P.add("act", lambda e: e.activation(out=sq[:], in_=pq, func=AF.Square), r=[pqk], w=[sqk])
        P.add("pe", lambda e: e.matmul(ps[b1][:], ones[:], sq[:], start=True, stop=True), r=["ones", sqk], w=[f"ps{b1}"])
        P.add("pe", lambda e: e.matmul(ps[b2][:], perm[:], qg[:], start=True, stop=True), r=["perm", qgk], w=[f"ps{b2}"])
        P.add("act", lambda e: e.activation(out=rst[:], in_=ps[b1][:], func=AF.Sqrt, scale=1.0 / 128, bias=epsT[:, 0:1]),
              r=[f"ps{b1}", "eps"], w=[rsk])
        P.add("dve", lambda e: e.reciprocal(out=rst[:], in_=rst[:]), r=[rsk], w=[rsk])
        P.add("dve", lambda e: e.tensor_tensor(out=t1[:], in0=qg[:], in1=Ct, op=ALU.mult), r=[qgk, Ck], w=[t1k])
        P.add("dve", lambda e: e.tensor_tensor(out=t2[:], in0=ps[b2][:], in1=St, op=ALU.mult), r=[f"ps{b2}", Sk], w=[t2k])
        P.add("dve", lambda e: e.tensor_tensor(out=t1[:], in0=t1[:], in1=t2[:], op=ALU.add), r=[t1k, t2k], w=[t1k])
        P.add("dve", lambda e: e.tensor_tensor(out=outt, in0=t1[:], in1=rst[:], op=ALU.mult), r=[t1k, rsk], w=[outk])

    def phase_kv():
        with ExitStack() as st:
            def sb(name, shape, dt):
                return st.enter_context(nc.sbuf_tensor("kv" + name, shape, dt))
            Wk = sb("Wk", [128, NCH, KVW], BF16)
            Wv = sb("Wv", [128, NCH, KVW], BF16)
            perm = sb("perm", [128, 128], BF16)
            qkg = sb("qkg", [128, 2], F32)
            P.dma("sp", perm[:], perm_d, w=["perm"])
            P.dma("sp", qkg[:], qk_g, w=["qkg"])
            P.dma("pool", Wk[:], w_in[:, ATT:ATT + KVW].rearrange("(c p) n -> p c n", p=128), w=["Wk"])
            P.dma("pool", Wv[:], w_in[:, ATT + KVW:ATT + 2 * KVW].rearrange("(c p) n -> p c n", p=128), w=["Wv"])
            hg = Ring(st, nc, "kvhg", 2, [128, 4, NCH, 128], BF16)
            Cr = Ring(st, nc, "kvC", 2, [128, 512], F32)
            Sr = Ring(st, nc, "kvS", 2, [128, 512], F32)
            ktr = Ring(st, nc, "kvkt", 2, [128, 512], BF16)
            vtr = Ring(st, nc, "kvvt", 2, [128, 4, KVW], BF16)
            tmp = make_rope(st, "kv")
            cvr = Ring(st, nc, "kvcv", 2, [128, NCH, 256], BF16)
            ngr = SB // 512
            cv_i = 0
            cv_prev = None
            bi = 0
            for tg in range(SB // 512):
                while cv_i < NWT and cv_i * ngr < (tg + 1) * NWT:
                    cvt, cvk = cvr.next()
                    c0_ = wb_col0(cv_i)
                    P.dma("pool", cvt[:], w_in[:, c0_:c0_ + 256].rearrange("(c p) n -> p c n", p=128), w=[cvk])
                    if cv_prev is not None:
                        P.dma("pool", WB[cv_prev[0]], cv_prev[1][:], r=[cv_prev[2]], w=["WBd"])
                    cv_prev = (cv_i, cvt, cvk)
                    cv_i += 1
                hgt, hk = hg.next()
                Ct, Ck = Cr.next()
                St, Sk = Sr.next()
                P.dma("sp", hgt[:], hT[tg * 4:(tg + 1) * 4].rearrange("t p c k -> p t c k"), w=[hk])
                P.dma("sp", Ct[:], cosT[:, tg * 512:(tg + 1) * 512], w=[Ck])
                P.dma("sp", St[:], sinT[:, tg * 512:(tg + 1) * 512], w=[Sk])
                for j in range(NKV):
                    b = bi % 2
                    bi += 1
                    mm_group(P, ps[b][:].rearrange("p (t k) -> p t k", t=4),
                             [(Wk[:, cc, j * 128:(j + 1) * 128], hgt[:, :, cc, :]) for cc in range(NCH)],
                             r=["Wk", hk], w=[f"ps{b}"])
                    kt, kk = ktr.next()
                    rope_epilogue(tmp, ps[b][:], f"ps{b}", qkg[:, 1:2], Ct[:], Ck, St[:], Sk, perm, kt[:], kk, 2 + b, 4 + b)
                    P.dma("act", KT[j, :, tg * 512:(tg + 1) * 512], kt[:], r=[kk], w=["KTd"])
                vt, vk = vtr.next()
                for s in range(4):
                    b = 6 + (s % 2)
                    mm_group(P, ps[b][:, 0:KVW], [(hgt[:, s, cc, :], Wv[:, cc, :]) for cc in range(NCH)],
                             r=["Wv", hk], w=[f"ps{b}"])
                    P.add("act", lambda e, vt=vt, s=s, b=b: e.copy(out=vt[:, s, :], in_=ps[b][:, 0:KVW]),
                          r=[f"ps{b}"], w=[vk])
                for j in range(NKV):
                    P.dma("act", Vs[j, :, tg * 4:(tg + 1) * 4, :], vt[:, :, j * 128:(j + 1) * 128], r=[vk], w=["Vd"])
            if cv_prev is not None:
                P.dma("pool", WB[cv_prev[0]], cv_prev[1][:], r=[cv_prev[2]], w=["WBd"])
            P.flush()

    def phase_qc():
        with ExitStack() as st:
            def sb(name, shape, dt):
                return st.enter_context(nc.sbuf_tensor("qc" + name, shape, dt))
            perm = sb("perm", [128, 128], BF16)
            qkg = sb("qkg", [128, 2], F32)
            cw = sb("cw", [128, NCB, 3], F32)
            cog = sb("cog", [128, NCB], F32)
            edge = sb("edge", [128, 2], F32)
            P.dma("sp", perm[:], perm_d, w=["perm"])
            P.dma("sp", qkg[:], qk_g, w=["qkg"])
            P.dma("sp", cw[:], conv_w, w=["cw"])
            P.dma("sp", cog[:], co_g, w=["cog"])
            P.dma("sp", edge[:], edge_d, w=["edge"])
            hgt = sb("hg", [128, 4, NCH, 128], BF16)
            hL = sb("hL", [128, NCH, 128], BF16)
            hR = sb("hR", [128, NCH, 128], BF16)
            hH = sb("hH", [128, NCH, 2], BF16)
            Ct = sb("C", [128, 512], F32)
            St = sb("S", [128, 512], F32)
            Wr = Ring(st, nc, "qcW", 6, [128, NCH, 256], BF16)
            qtr = Ring(st, nc, "qcqt", 2, [128, 512], BF16)
            tmp = make_rope(st, "qc")
            gcS = Ring(st, nc, "qcgc", 2, [128, 512], F32)
            hS = Ring(st, nc, "qchS", 2, [128, 4], F32)
            ur = Ring(st, nc, "qcu", 2, [128, 514], F32)
            vr = Ring(st, nc, "qcv", 2, [128, 512], F32)
            ycr = Ring(st, nc, "qcyc", 2, [128, 512], F32)
            sqr = Ring(st, nc, "qcsqc", 2, [128, 512], F32)
            ybr = Ring(st, nc, "qcyb", 2, [128, 512], BF16)
            ssc = sb("ssc", [128, 512], F32)
            stc = sb("stc", [128, NTO], F32)
            bi = 0

            def wload(col0):
                Wt, Wk_ = Wr.next()
                wi = col0 // 256 if col0 < ATT else ATT // 256 + (col0 - (ATT + 2 * KVW)) // 256
                P.dma("pool", Wt[:], WB[wi], w=[Wk_])
                return Wt, Wk_

            for tg in range(OWN // 512):
                P.dma("sp", hgt[:], hT[tg * 4:(tg + 1) * 4].rearrange("t p c k -> p t c k"), w=["hg"])
                P.dma("sp", hL[:], hT[(tg * 4 - 1) % NT], w=["hL"])
                P.dma("sp", hR[:], hT[(tg * 4 + 4) % NT], w=["hR"])
                P.dma("sp", Ct[:], cosT[:, tg * 512:(tg + 1) * 512], w=["C"])
                P.dma("sp", St[:], sinT[:, tg * 512:(tg + 1) * 512], w=["S"])
                P.add("act", lambda e: e.copy(out=hH[:, :, 0:1], in_=hL[:, :, 127:128]), r=["hL"], w=["hH"])
                P.add("act", lambda e: e.copy(out=hH[:, :, 1:2], in_=hR[:, :, 0:1]), r=["hR", "hH"], w=["hH"])
                for q4 in range(ATT // 256):
                    Wt, Wk_ = wload(q4 * 256)
                    for hh in range(2):
                        h = q4 * 2 + hh
                        b = bi % 2
                        bi += 1
                        mm_group(P, ps[b][:].rearrange("p (t k) -> p t k", t=4),
                                 [(Wt[:, cc, hh * 128:(hh + 1) * 128], hgt[:, :, cc, :]) for cc in range(NCH)],
                                 r=[Wk_, "hg"], w=[f"ps{b}"])
                        qt, qk_ = qtr.next()
                        rope_epilogue(tmp, ps[b][:], f"ps{b}", qkg[:, 0:1], Ct[:], "C", St[:], "S", perm, qt[:], qk_, 2 + b, 4 + b)
                        P.dma("act", QT[h, :, tg * 512:(tg + 1) * 512], qt[:], r=[qk_], w=["QTd"])
                c0 = ATT + 2 * KVW
                for c4 in range(CW // 256):
                    Wx, Wxk = wload(c0 + c4 * 256)
                    Wb, Wbk = wload(c0 + CW + c4 * 256)
                    Wc, Wck = wload(c0 + 2 * CW + c4 * 256)
                    for hh in range(2):
                        cb = c4 * 2 + hh
                        cols = slice(hh * 128, (hh + 1) * 128)
                        for (bk, Wt, Wk_) in ((0, Wx, Wxk), (1, Wc, Wck), (2, Wb, Wbk)):
                            mm_group(P, ps[bk][:].rearrange("p (t k) -> p t k", t=4),
                                     [(Wt[:, cc, cols], hgt[:, :, cc, :]) for cc in range(NCH)],
                                     r=[Wk_, "hg"], w=[f"ps{bk}"])
                        mm_group(P, ps[3][:, 0:2], [(Wx[:, cc, cols], hH[:, cc, :]) for cc in range(NCH)],
                                 r=[Wxk, "hH"], w=["ps3"])
                        mm_group(P, ps[3][:, 2:4], [(Wc[:, cc, cols], hH[:, cc, :]) for cc in range(NCH)],
                                 r=[Wck, "hH", "ps3"], w=["ps3"])
                        gc, gck = gcS.next()
                        hs, hsk = hS.next()
                        u, uk = ur.next()
                        v, vk = vr.next()
                        yc, yck = ycr.next()
                        sq, sqk = sqr.next()
                        yb, ybk = ybr.next()
                        P.add("act", lambda e, gc=gc: e.copy(out=gc[:], in_=ps[1][:]), r=["ps1"], w=[gck])
                        P.add("act", lambda e, hs=hs: e.copy(out=hs[:], in_=ps[3][:, 0:4]), r=["ps3"], w=[hsk])
                        P.add("dve", lambda e, u=u, gc=gc: e.tensor_tensor(out=u[:, 1:513], in0=ps[0][:], in1=gc[:], op=ALU.mult),
                              r=["ps0", gck], w=[uk])
                        if tg == 0:
                            P.add("dve", lambda e, u=u, hs=hs: e.scalar_tensor_tensor(
                                out=u[:, 0:1], in0=hs[:, 0:1], scalar=edge[:, 0:1], in1=hs[:, 2:3], op0=ALU.mult, op1=ALU.mult),
                                r=[hsk, "edge", uk], w=[uk])
                        else:
                            P.add("dve", lambda e, u=u, hs=hs: e.tensor_tensor(
                                out=u[:, 0:1], in0=hs[:, 0:1], in1=hs[:, 2:3], op=ALU.mult), r=[hsk, uk], w=[uk])
                        if tg == OWN // 512 - 1:
                            P.add("dve", lambda e, u=u, hs=hs: e.scalar_tensor_tensor(
                                out=u[:, 513:514], in0=hs[:, 1:2], scalar=edge[:, 1:2], in1=hs[:, 3:4], op0=ALU.mult, op1=ALU.mult),
                                r=[hsk, "edge", uk], w=[uk])
                        else:
                            P.add("dve", lambda e, u=u, hs=hs: e.tensor_tensor(
                                out=u[:, 513:514], in0=hs[:, 1:2], in1=hs[:, 3:4], op=ALU.mult), r=[hsk, uk], w=[uk])
                        P.add("dve", lambda e, u=u, v=v, cb=cb: e.tensor_scalar(
                            out=v[:], in0=u[:, 0:512], scalar1=cw[:, cb, 0:1], scalar2=None, op0=ALU.mult),
                            r=[uk, "cw"], w=[vk])
                        P.add("dve", lambda e, u=u, v=v, cb=cb: e.scalar_tensor_tensor(
                            out=v[:], in0=u[:, 1:513], scalar=cw[:, cb, 1:2], in1=v[:], op0=ALU.mult, op1=ALU.add),
                            r=[uk, "cw", vk], w=[vk])
                        P.add("dve", lambda e, u=u, v=v, cb=cb: e.scalar_tensor_tensor(
                            out=v[:], in0=u[:, 2:514], scalar=cw[:, cb, 2:3], in1=v[:], op0=ALU.mult, op1=ALU.add),
                            r=[uk, "cw", vk], w=[vk])
                        P.add("dve", lambda e, v=v, yc=yc: e.tensor_tensor(out=yc[:], in0=ps[2][:], in1=v[:], op=ALU.mult),
                              r=["ps2", vk], w=[yck])
                        P.add("act", lambda e, sq=sq, yc=yc: e.activation(out=sq[:], in_=yc[:], func=AF.Square), r=[yck], w=[sqk])
                        if cb == 0:
                            P.add("pool", lambda e, sq=sq: e.tensor_copy(out=ssc[:], in_=sq[:]), r=[sqk], w=["ssc"])
                        else:
                            P.add("pool", lambda e, sq=sq: e.tensor_tensor(out=ssc[:], in0=ssc[:], in1=sq[:], op=ALU.add),
                                  r=[sqk, "ssc"], w=["ssc"])
                        P.add("dve", lambda e, yb=yb, yc=yc, cb=cb: e.tensor_scalar(
                            out=yb[:], in0=yc[:], scalar1=cog[:, cb:cb + 1], scalar2=None, op0=ALU.mult),
                            r=[yck, "cog"], w=[ybk])
                        P.dma("act", YT[NQH + cb, :, tg * 512:(tg + 1) * 512], yb[:], r=[ybk], w=["YTd"])
                for s in range(4):
                    P.add("pe", lambda e, s=s: e.matmul(ps[4][:, s:s + 1], ssc[:, s * 128:(s + 1) * 128], onesf[:, 0:1],
                                                        start=True, stop=True), r=["ssc", "onesf"], w=["ps4"])
                P.add("act", lambda e, tg=tg: e.copy(out=stc[:, tg * 4:(tg + 1) * 4], in_=ps[4][:, 0:4]), r=["ps4"], w=["stc"])
            P.dma("sp", STA[:, NTO:2 * NTO], stc[:], r=["stc"], w=["STAd"])
            P.flush()

    def phase_attn():
        with ExitStack() as st:
            def sb(name, shape, dt):
                return st.enter_context(nc.sbuf_tensor("at" + name, shape, dt))
            NKB = SB // 128
            aog = sb("aog", [128, NQH], F32)
            P.dma("sp", aog[:], ao_g, w=["aog"])
            Kr = Ring(st, nc, "atK", 2, [128, SB], BF16)
            Vr = Ring(st, nc, "atV", 2, [128, NKB, 128], BF16)
            Qr = Ring(st, nc, "atQ", 2, [128, 512], BF16)
            Pr = Ring(st, nc, "atP", 4, [128, 1024], BF16)
            owr = Ring(st, nc, "atow", 2, [128, 512], F32)
            d0r = Ring(st, nc, "atd0", 2, [128, 1024], F32)
            d1r = Ring(st, nc, "atd1", 2, [128, 1024], F32)
            rdr = Ring(st, nc, "atrd", 2, [128, 512], F32)
            orr = Ring(st, nc, "ato", 2, [128, 512], F32)
            sqr = Ring(st, nc, "atsq", 2, [128, 512], F32)
            ybr = Ring(st, nc, "atyb", 2, [128, 512], BF16)
            ssa = sb("ssa", [128, OWN], F32)
            sta = sb("sta", [128, NTO], F32)
            scale = 128.0 ** -0.5
            NKP = NKB // 2
            items = [(j, tg, g) for j in range(NKV) for tg in range(OWN // 512) for g in range(GQ)]
            kv = {}

            def load_kv(j):
                Kt, Kk = Kr.next()
                Vt, Vk = Vr.next()
                P.dma("sp", Kt[:], KT[j], w=[Kk])
                P.dma("sp", Vt[:], Vs[j], w=[Vk])
                kv[j] = (Kt, Kk, Vt, Vk)

            qs = {}

            def load_q(i):
                j, tg, g = items[i]
                Qt, Qk = Qr.next()
                P.dma("sp", Qt[:], QT[j * GQ + g, :, tg * 512:(tg + 1) * 512], w=[Qk])
                qs[i] = (Qt, Qk)

            def s_mm(i, kp):
                j = items[i][0]
                Kt, Kk = kv[j][0], kv[j][1]
                Qt, Qk = qs[i]
                b0 = 2 * ((i * NKP + kp) % 3)

                def fn(e, kp=kp, b0=b0, Kt=Kt, Qt=Qt):
                    ins = None
                    for u in range(2):
                        kb = kp * 2 + u
                        ins = e.matmul(ps[b0 + u][:], Kt[:, kb * 128:(kb + 1) * 128], Qt[:], start=True, stop=True)
                    return ins
                P.add("pe", fn, r=[Kk, Qk], w=[f"ps{b0}", f"ps{b0 + 1}"])

            load_kv(0)
            load_q(0)
            s_mm(0, 0)
            s_mm(0, 1)
            for i, (j, tg, g) in enumerate(items):
                h = j * GQ + g
                Kt, Kk, Vt, Vk = kv[j]
                if i + 1 < len(items):
                    if items[i + 1][0] != j:
                        load_kv(items[i + 1][0])
                    load_q(i + 1)
                bo = 6
                bd = 7
                d0, d0k = d0r.next()
                d1, d1k = d1r.next()
                pe_den_started = False
                inited = set()
                for kp in range(NKP):
                    if kp + 2 < NKP:
                        s_mm(i, kp + 2)
                    elif i + 1 < len(items):
                        s_mm(i + 1, kp + 2 - NKP)
                    b0 = 2 * ((i * NKP + kp) % 3)
                    pt, pk = Pr.next()
                    P.add("act", lambda e, pt=pt, b0=b0: e.activation(
                        out=pt[:], in_=psall[:, b0 * 512:(b0 + 2) * 512], func=AF.Exp, scale=scale),
                        r=[f"ps{b0}", f"ps{b0 + 1}"], w=[pk])

                    def pv(e, pt=pt, kp=kp, bo=bo, Vt=Vt):
                        ins = None
                        for u in range(2):
                            kb = kp * 2 + u
                            ins = e.matmul(ps[bo][:], Vt[:, kb, :], pt[:, u * 512:(u + 1) * 512],
                                           start=(kb == 0), stop=(kb == NKB - 1))
                        return ins
                    P.add("pe", pv, r=[Vk, pk], w=[f"ps{bo}"])
                    who = ("dve", "pool", "pe", "dve", "pool", "dve", "pool", "pe")[kp % 8]
                    if who == "pe":
                        def dn(e, pt=pt, bd=bd, first=(not pe_den_started)):
                            ins = None
                            for u in range(2):
                                ins = e.matmul(ps[bd][:], ones[:], pt[:, u * 512:(u + 1) * 512],
                                               start=(first and u == 0), stop=False)
                            return ins
                        P.add("pe", dn, r=["ones", pk], w=[f"ps{bd}"])
                        pe_den_started = True
                    else:
                        eng, dd, ddk = ("dve", d0, d0k) if who == "dve" else ("pool", d1, d1k)
                        if ddk not in inited:
                            inited.add(ddk)
                            P.add(eng, lambda e, dd=dd, pt=pt: e.tensor_copy(out=dd[:], in_=pt[:]), r=[pk], w=[ddk])
                        else:
                            P.add(eng, lambda e, dd=dd, pt=pt: e.tensor_tensor(out=dd[:], in0=dd[:], in1=pt[:], op=ALU.add),
                                  r=[pk, ddk], w=[ddk])

                def dfin(e, d0=d0, d1=d1, bd=bd, st0=(not pe_den_started)):
                    ins = None
                    srcs = [d0[:, 0:512], d0[:, 512:1024], d1[:, 0:512], d1[:, 512:1024]]
                    for i_, sr in enumerate(srcs):
                        ins = e.matmul(ps[bd][:], onesf[:], sr, start=(st0 and i_ == 0), stop=(i_ == 3))
                    return ins
                ow, owk = owr.next()
                P.add("act", lambda e, ow=ow, bo=bo: e.copy(out=ow[:], in_=ps[bo][:]), r=[f"ps{bo}"], w=[owk])
                P.add("pe", dfin, r=["onesf", d0k, d1k], w=[f"ps{bd}"])
                rd, rdk = rdr.next()
                o, ok_ = orr.next()
                sq, sqk = sqr.next()
                yb, ybk = ybr.next()
                P.add("dve", lambda e, rd=rd, bd=bd: e.reciprocal(out=rd[:], in_=ps[bd][:]), r=[f"ps{bd}"], w=[rdk])
                P.add("dve", lambda e, o=o, rd=rd, ow=ow: e.tensor_tensor(out=o[:], in0=ow[:], in1=rd[:], op=ALU.mult),
                      r=[owk, rdk], w=[ok_])
                P.add("act", lambda e, sq=sq, o=o: e.activation(out=sq[:], in_=o[:], func=AF.Square), r=[ok_], w=[sqk])
                sl = slice(tg * 512, (tg + 1) * 512)
                if h == 0:
                    P.add("pool", lambda e, sq=sq, sl=sl: e.tensor_copy(out=ssa[:, sl], in_=sq[:]), r=[sqk], w=[f"ssa{tg}"])
                else:
                    P.add("pool", lambda e, sq=sq, sl=sl: e.tensor_tensor(out=ssa[:, sl], in0=ssa[:, sl], in1=sq[:], op=ALU.add),
                          r=[sqk, f"ssa{tg}"], w=[f"ssa{tg}"])
                P.add("dve", lambda e, yb=yb, o=o, h=h: e.tensor_scalar(
                    out=yb[:], in0=o[:], scalar1=aog[:, h:h + 1], scalar2=None, op0=ALU.mult), r=[ok_, "aog"], w=[ybk])
                P.dma("act", YT[h, :, sl], yb[:], r=[ybk], w=["YTd"])
            for t in range(NTO):
                P.add("pe", lambda e, t=t: e.matmul(ps[7][:, t:t + 1], ssa[:, t * 128:(t + 1) * 128], onesf[:, 0:1],
                                                    start=True, stop=True), r=[f"ssa{t // 4}", "onesf"], w=["ps7"])
            P.add("act", lambda e: e.copy(out=sta[:], in_=ps[7][:, 0:NTO]), r=["ps7"], w=["sta"])
            P.dma("sp", STA[:, 0:NTO], sta[:], r=["sta"], w=["STAd"])
            P.flush()

    def phase_out():
        with ExitStack() as st:
            def sb(name, shape, dt):
                return st.enter_context(nc.sbuf_tensor("op" + name, shape, dt))
            rs = sb("rs", [128, 2 * NTO], F32)
            P.dma("sp", rs[:], STA, w=["rs"])
            P.add("act", lambda e: e.activation(out=rs[:], in_=rs[:], func=AF.Sqrt, scale=1.0 / ATT, bias=epsT[:, 0:1]),
                  r=["rs", "eps"], w=["rs"])
            P.add("dve", lambda e: e.reciprocal(out=rs[:], in_=rs[:]), r=["rs"], w=["rs"])
            ygr = Ring(st, nc, "opyg", 2, [128, NYC, 512], BF16)
            Wr = Ring(st, nc, "opW", 2, [128, NYC, 512], BF16)
            xr = Ring(st, nc, "opx", 4, [128, 512], F32)
            tr_ = Ring(st, nc, "opt", 2, [128, 512], F32)
            orr = Ring(st, nc, "opo", 3, [128, 512], F32)
            bi = 0
            for dc in range(D // 512):
                Wt, Wk_ = Wr.next()
                P.dma("pool", Wt[:], w_out[:, dc * 512:(dc + 1) * 512].rearrange("(c p) n -> p c n", p=128), w=[Wk_])
                for tg in range(OWN // 512):
                    yg, ygk = ygr.next()
                    P.dma("sp", yg[:], YT[:, :, tg * 512:(tg + 1) * 512].rearrange("c p t -> p c t"), w=[ygk])
                    for s in range(4):
                        ba = (bi % 4) * 2
                        bi += 1
                        t_ = tg * 4 + s
                        xp, xk = xr.next()
                        P.dma("sp", xp[:], x[t_ * 128:(t_ + 1) * 128, dc * 512:(dc + 1) * 512], w=[xk])
                        mm_group(P, ps[ba][:], [(yg[:, cc, s * 128:(s + 1) * 128], Wt[:, cc, :]) for cc in range(NQH)],
                                 r=[ygk, Wk_], w=[f"ps{ba}"])
                        mm_group(P, ps[ba + 1][:], [(yg[:, cc, s * 128:(s + 1) * 128], Wt[:, cc, :]) for cc in range(NQH, NYC)],
                                 r=[ygk, Wk_], w=[f"ps{ba + 1}"])
                        tt, tk = tr_.next()
                        ot, ok_ = orr.next()
                        P.add("dve", lambda e, tt=tt, ba=ba, t_=t_, xp=xp: e.scalar_tensor_tensor(
                            out=tt[:], in0=ps[ba][:], scalar=rs[:, t_:t_ + 1], in1=xp[:], op0=ALU.mult, op1=ALU.add),
                            r=[f"ps{ba}", "rs", xk], w=[tk])
                        P.add("dve", lambda e, tt=tt, ot=ot, ba=ba, t_=t_: e.scalar_tensor_tensor(
                            out=ot[:], in0=ps[ba + 1][:], scalar=rs[:, NTO + t_:NTO + t_ + 1], in1=tt[:], op0=ALU.mult, op1=ALU.add),
                            r=[f"ps{ba + 1}", "rs", tk], w=[ok_])
                        P.dma("act", X1[t_ * 128:(t_ + 1) * 128, dc * 512:(dc + 1) * 512], ot[:], r=[ok_], w=["X1d"])
            P.flush()

    def wdt_prep(st):
        HC = max(NCH // 2, 1)
        HD = HC * 128
        NPJ = NCH // HC
        Wr = Ring(st, nc, "ppW", 2, [128, HD], BF16)
        Wfr = Ring(st, nc, "ppWf", 2, [128, HD], F32)
        Tr = Ring(st, nc, "ppT", 2, [128, HC, 128], BF16)
        wdv = w_down.rearrange("(i j) d -> j i d", j=128)
        state = {"bi": 0, "n": 0}

        def one(n):
            j, hp = n // NPJ, n % NPJ
            Wt, Wk_ = Wr.next()
            Tt, Tk = Tr.next()
            Wf, Wfk = Wfr.next()
            P.dma("sp", Wf[:], wdv[j][:, hp * HD:(hp + 1) * HD], w=[Wfk])
            per_tile = max(total // NTO, 1)
            eng = "act" if (n % per_tile) < (3 * per_tile) // 8 else "pool"
            if eng == "pool":
                P.add("pool", lambda e, Wt=Wt, Wf=Wf: e.tensor_copy(out=Wt[:], in_=Wf[:]), r=[Wfk], w=[Wk_])
            else:
                P.add("act", lambda e, Wt=Wt, Wf=Wf: e.copy(out=Wt[:], in_=Wf[:]), r=[Wfk], w=[Wk_])
            for c0 in range(0, HC, 8):
                nb = min(8, HC - c0)
                bk = (4, 7)[state["bi"] % 2]
                state["bi"] += 1
                pb = ps[bk][:].bitcast(BF16)

                def tr(e, Wt=Wt, c0=c0, pb=pb, nb=nb):
                    ins = None
                    for cc in range(c0, c0 + nb):
                        ins = e.transpose(out=pb[:, (cc - c0) * 128:(cc - c0 + 1) * 128],
                                          in_=Wt[:, cc * 128:(cc + 1) * 128], identity=ident[:])
                    return ins
                P.add("pe", tr, r=[Wk_, "ident"], w=[f"ps{bk}"])
                P.add("act", lambda e, Tt=Tt, c0=c0, pb=pb, nb=nb: e.copy(
                    out=Tt[:, c0:c0 + nb, :], in_=pb[:, 0:nb * 128].rearrange("p (c t) -> p c t", c=nb)), r=[f"ps{bk}"], w=[Tk])
            P.dma("act", WDT[j][:, hp * HC:(hp + 1) * HC, :], Tt[:], r=[Tk], w=["WDTd"])

        total = 128 * NPJ

        def step(k, nsteps):
            hi_ = (total * (k + 1)) // nsteps
            while state["n"] < hi_:
                one(state["n"])
                state["n"] += 1
        return step

    def phase_peer_prep():
        with ExitStack() as st:
            def sb(name, shape, dt):
                return st.enter_context(nc.sbuf_tensor("pq" + name, shape, dt))
            hgr = Ring(st, nc, "pqhg", 2, [128, 4, NCH, 128], BF16)
            Wr = Ring(st, nc, "pqW", 2, [128, NCH, 512], BF16)
            qr = Ring(st, nc, "pqq", 3, [128, 512], BF16)
            bi = 0
            for q4 in range(PQ // 512):
                Wt, Wk_ = Wr.next()
                P.dma("pool", Wt[:], w_query[:, q4 * 512:(q4 + 1) * 512].rearrange("(c p) n -> p c n", p=128), w=[Wk_])
                for tg in range(OWN // 512):
                    hgt, hgk = hgr.next()
                    P.dma("sp", hgt[:], H2T[tg * 4:(tg + 1) * 4].rearrange("t p c k -> p t c k"), w=[hgk])
                    for hh in range(4):
                        b = bi % 4
                        bi += 1
                        mm_group(P, ps[b][:].rearrange("p (t k) -> p t k", t=4),
                                 [(Wt[:, cc, hh * 128:(hh + 1) * 128], hgt[:, :, cc, :]) for cc in range(NCH)],
                                 r=[Wk_, hgk], w=[f"ps{b}"])
                        qt, qk_ = qr.next()
                        P.add("act", lambda e, qt=qt, b=b: e.copy(out=qt[:], in_=ps[b][:]), r=[f"ps{b}"], w=[qk_])
                        P.dma("act", QPT[q4 * 4 + hh, :, tg * 512:(tg + 1) * 512], qt[:], r=[qk_], w=["QPTd"])
            P.flush()

    def phase_peer_pick():
        with ExitStack() as st:
            def sb(name, shape, dt):
                return st.enter_context(nc.sbuf_tensor("pk" + name, shape, dt))
            kn = sb("kn", [128, 16, 128], BF16)
            kT = sb("kT", [128, 16, 128], BF16)
            iot = sb("iot", [128, 128], F32)
            P.dma("pool", kn[:], sub_keys.rearrange("g n k -> n g k"), w=["kn"])
            P.dma("sp", iot[:], iota_d, w=["iot"])
            for g0 in range(0, 16, 8):
                pb = ps[0][:].bitcast(BF16)

                def tr(e, g0=g0, pb=pb):
                    ins = None
                    for g in range(g0, g0 + 8):
                        ins = e.transpose(out=pb[:, (g - g0) * 128:(g - g0 + 1) * 128], in_=kn[:, g, :], identity=ident[:])
                    return ins
                P.add("pe", tr, r=["kn", "ident"], w=["ps0"])
                P.add("act", lambda e, g0=g0, pb=pb: e.copy(out=kT[:, g0:g0 + 8, :], in_=pb.rearrange("p (c t) -> p c t", c=8)),
                      r=["ps0"], w=["kT"])
            qpr = Ring(st, nc, "pkqp", 2, [128, 16, 128], BF16)
            S = sb("S", [128, 16, 128], F32)
            S2 = sb("S2", [128, 16, 128], F32)
            V = sb("V", [128, 16, 16], F32)
            IX = sb("IX", [128, 16, 16], U32)
            IXf = sb("IXf", [128, 16, 16], F32)
            cand = sb("cand", [128, 8, 256], F32)
            cand2 = sb("cand2", [128, 8, 256], F32)
            Bv = sb("Bv", [128, 8, 16], F32)
            PX = sb("PX", [128, 8, 16], U32)
            R1u = sb("R1u", [128, 8, 16], U32)
            R2u = sb("R2u", [128, 8, 16], U32)
            R1 = sb("R1", [128, 8, 16], F32)
            R2 = sb("R2", [128, 8, 16], F32)
            E = sb("E", [128, 8, 16], F32)
            Z = sb("Z", [128, 8], F32)
            eq = sb("eq", [128, 8, 16, 16], F32)
            PI = sb("PI", [128, 128], F32)
            PJ = sb("PJ", [128, 128], F32)
            PG = sb("PG", [128, 128], F32)
            PT = sb("PT", [128, 3, 128], F32)
            OI = sb("OI", [128, 128, 128], BF16)
            OJ = sb("OJ", [128, 128, 128], BF16)
            Gr = Ring(st, nc, "pkG", 1, [128, 128, 128], BF16)
            prep_step = wdt_prep(st)
            prep_k = 0

            def dv(fn, r, w):
                P.add("dve", fn, r=r, w=w)

            def scores(tt):
                qp, qpk = qpr.next()
                P.dma("sp", qp[:], QPT[:, :, tt * 128:(tt + 1) * 128].rearrange("g p t -> p g t"), w=[qpk])
                for b in range(4):
                    for g4 in range(4):
                        g = b * 4 + g4
                        P.add("pe", lambda e, g=g, g4=g4, b=b, qp=qp: e.matmul(
                            ps[b][:, g4 * 128:(g4 + 1) * 128], qp[:, g, :], kT[:, g, :], start=True, stop=True),
                            r=[qpk, "kT"] + ([f"ps{b}"] if g4 else []), w=[f"ps{b}"])
                    P.add("act", lambda e, b=b: e.copy(out=S[:, b * 4:(b + 1) * 4, :],
                                                       in_=ps[b][:].rearrange("p (g n) -> p g n", g=4)),
                          r=[f"ps{b}"], w=[f"S{g_}" for g_ in range(b * 4, b * 4 + 4)])

            def topk():
                G16 = range(16)
                for g in G16:
                    dv(lambda e, g=g: e.max(out=V[:, g, 0:8], in_=S[:, g, :]), [f"S{g}"], [f"Va{g}"])
                for g in G16:
                    dv(lambda e, g=g: e.max_index(out=IX[:, g, 0:8], in_max=V[:, g, 0:8], in_values=S[:, g, :]),
                       [f"S{g}", f"Va{g}"], [f"IXa{g}"])
                for g in G16:
                    dv(lambda e, g=g: e.match_replace(out=S2[:, g, :], in_to_replace=V[:, g, 0:8], in_values=S[:, g, :],
                                                      imm_value=NEG), [f"S{g}", f"Va{g}"], [f"S2{g}"])
                for g in G16:
                    dv(lambda e, g=g: e.max(out=V[:, g, 8:16], in_=S2[:, g, :]), [f"S2{g}"], [f"Vb{g}"])
                for g in G16:
                    dv(lambda e, g=g: e.max_index(out=IX[:, g, 8:16], in_max=V[:, g, 8:16], in_values=S2[:, g, :]),
                       [f"S2{g}", f"Vb{g}"], [f"IXb{g}"])
                allV = [f"Va{g}" for g in G16] + [f"Vb{g}" for g in G16]
                allIX = [f"IXa{g}" for g in G16] + [f"IXb{g}" for g in G16]
                dv(lambda e: e.tensor_copy(out=IXf[:], in_=IX[:]), allIX, ["IXf"])
                Vv = V[:].rearrange("p (h two) k -> p h two k", two=2)
                H8 = range(8)
                dv(lambda e, Vv=Vv: e.tensor_tensor(
                    out=cand[:].rearrange("p h (a b) -> p h a b", a=16),
                    in0=Vv[:, :, 0, :].unsqueeze(3).to_broadcast([128, 8, 16, 16]),
                    in1=Vv[:, :, 1, :].unsqueeze(2).to_broadcast([128, 8, 16, 16]), op=ALU.add), allV, [f"cand{h}" for h in H8])
                for h in H8:
                    dv(lambda e, h=h: e.max(out=Bv[:, h, 0:8], in_=cand[:, h, :]), [f"cand{h}"], [f"Ba{h}"])
                for h in H8:
                    dv(lambda e, h=h: e.max_index(out=PX[:, h, 0:8], in_max=Bv[:, h, 0:8], in_values=cand[:, h, :]),
                       [f"cand{h}", f"Ba{h}"], [f"PXa{h}"])
                for h in H8:
                    dv(lambda e, h=h: e.match_replace(out=cand2[:, h, :], in_to_replace=Bv[:, h, 0:8], in_values=cand[:, h, :],
                                                      imm_value=NEG), [f"cand{h}", f"Ba{h}"], [f"cand2{h}"])
                for h in H8:
                    dv(lambda e, h=h: e.max(out=Bv[:, h, 8:16], in_=cand2[:, h, :]), [f"cand2{h}"], [f"Bb{h}"])
                for h in H8:
                    dv(lambda e, h=h: e.max_index(out=PX[:, h, 8:16], in_max=Bv[:, h, 8:16], in_values=cand2[:, h, :]),
                       [f"cand2{h}", f"Bb{h}"], [f"PXb{h}"])
                allB = [f"Ba{h}" for h in H8] + [f"Bb{h}" for h in H8]
                allPX = [f"PXa{h}" for h in H8] + [f"PXb{h}" for h in H8]
                dv(lambda e: e.tensor_tensor(out=E[:], in0=Bv[:], in1=Bv[:, :, 0:1].to_broadcast([128, 8, 16]), op=ALU.subtract),
                   allB, ["E"])
                P.add("act", lambda e: e.activation(out=E[:], in_=E[:], func=AF.Exp), r=["E"], w=["E"])
                dv(lambda e: e.tensor_reduce(out=Z[:], in_=E[:], axis=AX.X, op=ALU.add), ["E"], ["Z"])
                dv(lambda e: e.reciprocal(out=Z[:], in_=Z[:]), ["Z"], ["Z"])
                dv(lambda e: e.tensor_tensor(out=PG[:].rearrange("p (h k) -> p h k", h=8), in0=E[:],
                                             in1=Z[:].unsqueeze(2).to_broadcast([128, 8, 16]), op=ALU.mult), ["E", "Z"], ["PG"])
                dv(lambda e: e.tensor_single_scalar(out=R1u[:], in_=PX[:], scalar=4, op=ALU.logical_shift_right), allPX, ["R1u"])
                dv(lambda e: e.tensor_single_scalar(out=R2u[:], in_=PX[:], scalar=15, op=ALU.bitwise_and), allPX, ["R2u"])
                dv(lambda e: e.tensor_copy(out=R1[:], in_=R1u[:]), ["R1u"], ["R1"])
                dv(lambda e: e.tensor_copy(out=R2[:], in_=R2u[:]), ["R2u"], ["R2"])
                IXv = IXf[:].rearrange("p (h two) k -> p h two k", two=2)
                for (Rr, Rk, two, Pout, Pk) in ((R1, "R1", 0, PI, "PI"), (R2, "R2", 1, PJ, "PJ")):
                    dv(lambda e, Rr=Rr: e.tensor_tensor(
                        out=eq[:], in0=Rr[:].unsqueeze(3).to_broadcast([128, 8, 16, 16]),
                        in1=iot[:, 0:16].unsqueeze(1).unsqueeze(1).to_broadcast([128, 8, 16, 16]), op=ALU.is_equal),
                        [Rk, "iot"], ["eq"])
                    dv(lambda e, two=two, IXv=IXv: e.tensor_tensor(
                        out=eq[:], in0=eq[:], in1=IXv[:, :, two, :].unsqueeze(2).to_broadcast([128, 8, 16, 16]), op=ALU.mult),
                        ["eq", "IXf"], ["eq"])
                    dv(lambda e, Pout=Pout: e.tensor_reduce(out=Pout[:].rearrange("p (h k) -> p h k", h=8), in_=eq[:],
                                                            axis=AX.X, op=ALU.add), ["eq"], [Pk])

            scores(0)
            topk()
            for tt in range(NTO):
                for n_, (Pin, Pk) in enumerate(((PI, "PI"), (PJ, "PJ"), (PG, "PG"))):
                    P.add("pe", lambda e, n_=n_, Pin=Pin: e.transpose(out=ps[4][:, n_ * 128:(n_ + 1) * 128], in_=Pin[:], identity=identf[:]),
                          r=[Pk, "identf"] + (["ps4"] if n_ else []), w=["ps4"])
                P.add("act", lambda e: e.copy(out=PT[:], in_=ps[4][:, 0:384].rearrange("p (a t) -> p a t", a=3)), r=["ps4"], w=["PT"])
                dv(lambda e: e.tensor_tensor(
                    out=OJ[:], in0=PT[:, 1, :].unsqueeze(2).to_broadcast([128, 128, 128]),
                    in1=iot[:].unsqueeze(1).to_broadcast([128, 128, 128]), op=ALU.is_equal), ["PT", "iot"], ["OJ"])
                P.add("pool", lambda e: e.tensor_tensor(
                    out=OJ[:], in0=OJ[:], in1=PT[:, 2, :].unsqueeze(2).to_broadcast([128, 128, 128]), op=ALU.mult),
                    r=["PT", "OJ"], w=["OJ"])
                dv(lambda e: e.tensor_tensor(
                    out=OI[:], in0=PT[:, 0, :].unsqueeze(2).to_broadcast([128, 128, 128]),
                    in1=iot[:].unsqueeze(1).to_broadcast([128, 128, 128]), op=ALU.is_equal), ["PT", "iot"], ["OI"])
                if tt + 1 < NTO:
                    scores(tt + 1)
                    topk()
                Gt, Gk = Gr.next()
                prep_step(prep_k, NTO)
                prep_k += 1
                for t4 in range(32):
                    b = 5 + (t4 % 2)

                    def gm(e, t4=t4, b=b):
                        ins = None
                        for u in range(4):
                            t_ = t4 * 4 + u
                            ins = e.matmul(ps[b][:, u * 128:(u + 1) * 128], OI[:, t_, :], OJ[:, t_, :], start=True, stop=True)
                        return ins
                    P.add("pe", gm, r=["OI", "OJ"], w=[f"ps{b}"])
                    outv = Gt[:, :, t4 * 4:(t4 + 1) * 4].rearrange("p j t -> p t j")
                    inv = ps[b][:].rearrange("p (t j) -> p t j", t=4)
                    if t4 < 20:
                        P.add("act", lambda e, outv=outv, inv=inv: e.copy(out=outv, in_=inv), r=[f"ps{b}"], w=[Gk])
                    else:
                        P.add("dve", lambda e, outv=outv, inv=inv: e.tensor_copy(out=outv, in_=inv), r=[f"ps{b}"], w=[Gk])
                P.dma("act", GS[tt], Gt[:], r=[Gk], w=["GSd"])
            P.flush()

    def phase_peer_main():
        with ExitStack() as st:
            def sb(name, shape, dt):
                return st.enter_context(nc.sbuf_tensor("pm" + name, shape, dt))
            gB = sb("gB", [128, D], F32)
            P.dma("sp", gB[:], g_fin.partition_broadcast(128), w=["gB"])
            hgt = sb("hg", [128, 4, NCH, 128], BF16)
            acc = sb("acc", [128, 4, D], F32)
            WDr = Ring(st, nc, "pmWD", 4, [128, NCH, 128], BF16)
            WUr = Ring(st, nc, "pmWU", 2, [128, 2, D], BF16)
            Gr = Ring(st, nc, "pmG", 4, [128, 4, 2, 128], BF16)
            Dgr = Ring(st, nc, "pmDg", 2, [128, 2, 512], BF16)
            Ar = Ring(st, nc, "pmA", 3, [128, 2, 512], BF16)
            ss = sb("ss", [128, 4], F32)
            wuv = w_up.rearrange("(i j) d -> i j d", j=128)
            NJG = 64
            NUP = 4 * (D // 512)
            ui = 0
            for tg in range(OWN // 512):
                P.dma("sp", hgt[:], H2T[tg * 4:(tg + 1) * 4].rearrange("t p c k -> p t c k"), w=["hg"])
                P.dma("sp", acc[:], X1[tg * 512:(tg + 1) * 512, :].rearrange("(s p) d -> p s d", p=128), w=["acc"])
                stt = {}

                def load_down(jg, tg=tg, stt=stt):
                    d = stt.setdefault(jg, {})
                    d["WD"] = []
                    for jj in range(2):
                        WD, WDk = WDr.next()
                        P.dma("sp", WD[:], WDT[jg * 2 + jj], w=[WDk])
                        d["WD"].append((WD, WDk))
                    Gt, Gk = Gr.next()
                    P.dma("sp", Gt[:], GS[tg * 4:(tg + 1) * 4, :, jg * 2:(jg + 1) * 2, :].rearrange("s i j t -> i s j t"), w=[Gk])
                    d["G"] = (Gt, Gk)

                def load_up(jg, stt=stt):
                    WU, WUk = WUr.next()
                    P.dma("pool", WU[:], wuv[:, jg * 2:(jg + 1) * 2, :], w=[WUk])
                    stt.setdefault(jg, {})["WU"] = (WU, WUk)

                def down_pieces(jg, stt=stt):
                    d = stt[jg]
                    Dg, Dgk = Dgr.next()
                    A, Ak = Ar.next()
                    d["A"] = (A, Ak)
                    Gt, Gk = d["G"]
                    pieces = []
                    for k in range(NUP):
                        def piece(k=k, d=d, Dg=Dg, Dgk=Dgk, A=A, Ak=Ak, Gt=Gt, Gk=Gk):
                            ms = []
                            for m in (2 * k, 2 * k + 1):
                                jj, cc = m // NCH, m % NCH
                                ms.append((jj, cc))
                            jj = ms[0][0]
                            WD, WDk = d["WD"][jj]

                            def fn(e, ms=ms, WD=WD, jj=jj):
                                ins = None
                                for (_, cc) in ms:
                                    ins = e.matmul(ps[jj][:].rearrange("p (t k) -> p t k", t=4), WD[:, cc, :], hgt[:, :, cc, :],
                                                   start=(cc == 0), stop=(cc == NCH - 1))
                                return ins
                            P.add("pe", fn, r=[WDk, "hg"], w=[f"ps{jj}"])
                            if ms[-1][1] == NCH - 1:
                                P.add("act", lambda e, Dg=Dg, jj=jj: e.activation(out=Dg[:, jj, :], in_=ps[jj][:], func=AF.Gelu),
                                      r=[f"ps{jj}"], w=[Dgk])
                                if jj == 1:
                                    P.add("pool", lambda e, A=A, Dg=Dg, Gt=Gt: e.tensor_tensor(
                                        out=A[:].rearrange("p j (s t) -> p j s t", s=4),
                                        in0=Dg[:].rearrange("p j (s t) -> p j s t", s=4),
                                        in1=Gt[:].rearrange("p s j t -> p j s t"), op=ALU.mult), r=[Dgk, Gk], w=[Ak])
                        pieces.append(piece)
                    return pieces

                load_down(0)
                load_down(1)
                load_up(0)
                for pc in down_pieces(0):
                    pc()
                load_down(2)
                for pc in down_pieces(1):
                    pc()
                for jg in range(NJG):
                    if jg + 3 < NJG:
                        load_down(jg + 3)
                    if jg + 1 < NJG:
                        load_up(jg + 1)
                    pieces = down_pieces(jg + 2) if jg + 2 < NJG else []
                    A, Ak = stt[jg]["A"]
                    WU, WUk = stt[jg]["WU"]
                    k = 0
                    for s_ in range(4):
                        for dc in range(D // 512):
                            b = 2 + (ui % 6)
                            ui += 1
                            mm_group(P, ps[b][:], [(A[:, jj, s_ * 128:(s_ + 1) * 128], WU[:, jj, dc * 512:(dc + 1) * 512]) for jj in range(2)],
                                     r=[Ak, WUk], w=[f"ps{b}"])
                            P.add("dve", lambda e, s_=s_, dc=dc, b=b: e.tensor_tensor(
                                out=acc[:, s_, dc * 512:(dc + 1) * 512], in0=acc[:, s_, dc * 512:(dc + 1) * 512], in1=ps[b][:], op=ALU.add),
                                r=[f"ps{b}", "acc"], w=["acc"])
                            if pieces:
                                pieces[k]()
                            k += 1
                    del stt[jg]
                for s in range(4):
                    P.add("dve", lambda e, s=s: e.scalar_tensor_tensor(
                        out=hgt[:].rearrange("p a c k -> p (a c k)")[:, 0:D], in0=acc[:, s, :], scalar=1.0, in1=acc[:, s, :],
                        op0=ALU.mult, op1=ALU.mult, accum_out=ss[:, s:s + 1]), r=["acc", "hg"], w=["hg", "ss"])
                P.add("act", lambda e: e.activation(out=ss[:], in_=ss[:], func=AF.Sqrt, scale=1.0 / D, bias=epsT[:, 0:1]),
                      r=["ss", "eps"], w=["ss"])
                P.add("dve", lambda e: e.reciprocal(out=ss[:], in_=ss[:]), r=["ss"], w=["ss"])
                for s in range(4):
                    P.add("dve", lambda e, s=s: e.scalar_tensor_tensor(
                        out=acc[:, s, :], in0=acc[:, s, :], scalar=ss[:, s:s + 1], in1=gB[:], op0=ALU.mult, op1=ALU.mult),
                        r=["acc", "ss", "gB"], w=["acc"])
                P.dma("sp", out[tg * 512:(tg + 1) * 512, :].rearrange("(s p) d -> p s d", p=128), acc[:], r=["acc"], w=["outd"])
            P.flush()

    load_consts()
    stages = getattr(c, "stages", 99)
    phase_norm_T(x, g_mix, hT, NT, "na")
    if stages >= 2:
        phase_kv()
    if stages >= 3:
        phase_qc()
    if stages >= 4:
        phase_attn()
    if stages >= 5:
        phase_out()
    if stages >= 6:
        phase_norm_T(X1, g_ffn, H2T, NTO, "nf")
    if stages >= 7:
        phase_peer_prep()
    if stages >= 8:
        phase_peer_pick()
    if stages >= 9:
        phase_peer_main()
    gst.close()
    P.close()
    return nc


def rope_tables(SB):
    half = 64
    inv = (10000.0 ** (-np.arange(0, half, 2, dtype=np.float32) / half)).astype(np.float32)
    t = np.arange(SB)
    row = (t // 64).astype(np.float32)
    col = (t % 64).astype(np.float32)
    ang_r = row[:, None] * inv
    ang_c = col[:, None] * inv
    cosT = np.empty((128, SB), np.float32)
    sinT = np.empty((128, SB), np.float32)
    for blk, ang in ((0, ang_r), (1, ang_c)):
        c = np.cos(ang).T.astype(np.float32)
        s = np.sin(ang).T.astype(np.float32)
        o = blk * 64
        cosT[o:o + 32] = c
        cosT[o + 32:o + 64] = c
        sinT[o:o + 32] = -s
        sinT[o + 32:o + 64] = s
    return cosT, sinT


def make_in_maps(c, inputs, n_cores=8):
    D, SB, OWN = c.D, c.SB, c.OWN
    bf = ml_dtypes.bfloat16
    cosT, sinT = rope_tables(SB)
    perm = np.zeros((128, 128), np.float32)
    for d in range(128):
        perm[(d // 64) * 64 + ((d % 64) + 32) % 64, d] = 1.0
    iota = np.tile(np.arange(128, dtype=np.float32)[None, :], (128, 1))
    f = lambda a: np.ascontiguousarray(a, dtype=np.float32)
    shared = {
        "w_in": f(inputs["w_in"][0]), "w_out": f(inputs["w_out"][0]), "w_query": f(inputs["peer_w_query"][0]),
        "sub_keys": f(inputs["peer_sub_keys"][0].reshape(16, 128, 128)),
        "w_down": f(inputs["peer_w_down"][0]), "w_up": f(inputs["peer_w_up"][0]),
        "g_mix": f(inputs["norm_mix_g"][0][None, :]), "g_ffn": f(inputs["norm_ffn_g"][0][None, :]),
        "g_fin": f(inputs["norm_final_g"][None, :]),
        "qk_g": f(np.stack([inputs["q_norm_g"][0], inputs["k_norm_g"][0]], axis=1)),
        "conv_w": f(inputs["conv_w"][0].T.reshape(c.NCB, 128, 3).transpose(1, 0, 2)),
        "ao_g": f(inputs["attn_out_g"][0].reshape(c.NQH, 128).T),
        "co_g": f(inputs["conv_out_g"][0].reshape(c.NCB, 128).T),
        "ident": np.eye(128, dtype=np.float32).astype(bf), "identf": np.eye(128, dtype=np.float32),
        "perm": perm.astype(bf), "iota": iota,
    }
    maps = []
    for core in range(n_cores):
        b, qd = core // 4, core % 4
        sh = qd * OWN
        m = dict(shared)
        m["x"] = f(np.roll(inputs["x"][b], -sh, axis=0))
        m["cosT"] = f(np.roll(cosT, -sh, axis=1))
        m["sinT"] = f(np.roll(sinT, -sh, axis=1))
        edge = np.ones((128, 2), np.float32)
        if qd == 0:
            edge[:, 0] = 0.0
        if qd == 3:
            edge[:, 1] = 0.0
        m["edge"] = edge
        maps.append(m)
    return maps


def kernel(**inputs):
    inputs = {k: np.asarray(v) for k, v in inputs.items()}
    c = Cfg()
    nc = build(c)
    maps = make_in_maps(c, inputs)
    res = run_bass_kernel_spmd(nc, maps, core_ids=list(range(8)))
    outs = [np.asarray(r["out"], dtype=np.float32) for r in res.results]
    B = inputs["x"].shape[0]
    full = np.stack([np.concatenate(outs[b * 4:(b + 1) * 4], axis=0) for b in range(B)], axis=0)
    return full.astype(np.float32)
```

```python
import numpy as np
from contextlib import ExitStack
import ml_dtypes
import concourse.bass as bass
import concourse.mybir as mybir
from concourse.bass_utils import run_bass_kernel_spmd

F32 = mybir.dt.float32
BF16 = mybir.dt.bfloat16
U32 = mybir.dt.uint32
AF = mybir.ActivationFunctionType
ALU = mybir.AluOpType
AX = mybir.AxisListType
ENGS = ["pe", "act", "dve", "pool", "sp"]
EPS = 1e-6
NEG = -1.0e30


class Prog:
    R = 8

    def __init__(self, nc):
        self.nc = nc
        self.pending = []
        self.gid = 0
        self.sig = {}
        self.eng_of = {}
        self.isdma = {}
        self.last_write = {}
        self.readers = {}
        self.cnt = {e: 0 for e in ENGS}
        self.known = {e: {} for e in ENGS}
        self.last_op = {e: None for e in ENGS}
        self.dma_n = {"sp": 0, "pool": 0, "act": 0}
        self.dma_hist = {"sp": [], "pool": [], "act": []}
        self.stack = ExitStack()
        self.sem = {e: self.stack.enter_context(nc.semaphore("s_" + e)) for e in ENGS}
        self.ring = {q: [self.stack.enter_context(nc.semaphore(f"r_{q}{i}")) for i in range(self.R)]
                     for q in ("sp", "pool", "act")}

    def add(self, eng, fn, r=(), w=(), dma=False):
        w = tuple(w) + tuple(k for k in r if k.startswith("ps") and k not in w)
        self.pending.append(dict(eng=eng, fn=fn, r=tuple(r), w=tuple(w), dma=dma, gid=self.gid, barrier=False))
        self.gid += 1

    def dma(self, q, out, in_, r=(), w=()):
        self.add(q, lambda e, o=out, i=in_: e.dma_start(out=o, in_=i), r, w, dma=True)

    def barrier(self):
        for e in ENGS:
            self.pending.append(dict(eng=e, fn=None, r=(), w=(), dma=False, gid=self.gid, barrier=True))
            self.gid += 1

    def flush(self):
        self.barrier()
        ops = self.pending
        self.pending = []
        needed = set()
        last_op = dict(self.last_op)
        dma_hist = {q: list(v) for q, v in self.dma_hist.items()}
        dma_n = dict(self.dma_n)
        for op in ops:
            g = op["gid"]
            self.eng_of[g] = op["eng"]
            self.isdma[g] = op["dma"]
            deps = set()
            if op["barrier"]:
                for e in ENGS:
                    if last_op[e] is not None:
                        deps.add(last_op[e])
                for q in dma_hist:
                    deps.update(dma_hist[q][-self.R:])
            else:
                for k in op["r"]:
                    if k in self.last_write:
                        deps.add(self.last_write[k])
                for k in op["w"]:
                    if k in self.last_write:
                        deps.add(self.last_write[k])
                    deps.update(self.readers.get(k, ()))
                for k in op["w"]:
                    self.last_write[k] = g
                    self.readers[k] = []
                for k in op["r"]:
                    if k not in op["w"]:
                        lst = self.readers.setdefault(k, [])
                        if not op["dma"]:
                            lst[:] = [x for x in lst if self.isdma[x] or self.eng_of[x] != op["eng"]]
                        lst.append(g)
                if op["dma"]:
                    q = op["eng"]
                    n = dma_n[q]
                    if n >= self.R:
                        deps.add(dma_hist[q][n - self.R])
                    dma_hist[q].append(g)
                    dma_n[q] = n + 1
                else:
                    last_op[op["eng"]] = g
            deps.discard(g)
            op["deps"] = deps
            for d in deps:
                if not self.isdma[d]:
                    needed.add(d)
        for op in ops:
            g = op["gid"]
            if op["barrier"]:
                continue
            e = op["eng"]
            if op["dma"]:
                n = self.dma_n[e]
                self.sig[g] = (self.ring[e][n % self.R], 16 * (n // self.R + 1))
                self.dma_n[e] = n + 1
                self.dma_hist[e].append(g)
                op["inc"] = True
            else:
                self.last_op[e] = g
                if g in needed:
                    self.cnt[e] += 1
                    self.sig[g] = (self.sem[e], self.cnt[e])
                    op["inc"] = True
                else:
                    op["inc"] = False
        for op in ops:
            e = op["eng"]
            waits = {}
            for d in sorted(op["deps"]):
                if (not self.isdma[d]) and self.eng_of[d] == "pe" and e == "pe":
                    continue
                s, v = self.sig[d]
                key = id(s)
                if self.known[e].get(key, 0) >= v:
                    continue
                if key not in waits or waits[key][1] < v:
                    waits[key] = (s, v)
            for key, (s, v) in waits.items():
                self.known[e][key] = v
            op["waits"] = list(waits.values())
        per = {e: [o for o in ops if o["eng"] == e] for e in ENGS}

        def run(eng_obj, lst):
            for op in lst:
                for (s, v) in op["waits"]:
                    eng_obj.wait_ge(s, v)
                if op["fn"] is None:
                    continue
                ins = op["fn"](eng_obj)
                if op["inc"]:
                    s, v = self.sig[op["gid"]]
                    ins.then_inc(s, 16 if op["dma"] else 1)

        with self.nc.Block() as block:
            @block.tensor
            def _(e):
                run(e, per["pe"])

            @block.scalar
            def _(e):
                run(e, per["act"])

            @block.vector
            def _(e):
                run(e, per["dve"])

            @block.gpsimd
            def _(e):
                run(e, per["pool"])

            @block.sync
            def _(e):
                run(e, per["sp"])
        self.last_write.clear()
        self.readers.clear()

    def close(self):
        self.stack.close()


class Ring:
    def __init__(self, st, nc, name, n, shape, dt):
        self.t = [st.enter_context(nc.sbuf_tensor(f"{name}{i}", shape, dt)) for i in range(n)]
        self.name = name
        self.i = 0

    def next(self):
        k = self.i % len(self.t)
        self.i += 1
        return self.t[k], f"{self.name}{k}"


class Cfg:
    def __init__(self, D=4096, SB=8192):
        self.D = D
        self.SB = SB
        self.OWN = SB // 4
        self.NCH = D // 128
        self.ATT = D // 2
        self.NQH = self.ATT // 128
        self.NKV = max(1, self.NQH // 4)
        self.GQ = self.NQH // self.NKV
        self.KVW = self.NKV * 128
        self.CW = D - self.ATT
        self.NCB = self.CW // 128
        self.INC = self.ATT + 2 * self.KVW + 3 * self.CW
        self.NT = SB // 128
        self.NTO = self.OWN // 128
        self.PQ = 2048
        self.NE = 16384


def mm_group(P, out_ap, pairs, r, w):
    def fn(e, pairs=pairs, out_ap=out_ap):
        n = len(pairs)
        ins = None
        for i, (l, rh) in enumerate(pairs):
            ins = e.matmul(out_ap, l, rh, start=(i == 0), stop=(i == n - 1))
        return ins
    P.add("pe", fn, r=r, w=w)


def build(c, debug=False):
    nc = bass.Bass("TRN2", target_bir_lowering=False)
    D, SB, OWN, NCH, ATT, NQH, NKV, GQ, KVW, CW, NCB, INC, NT, NTO, PQ, NE = (
        c.D, c.SB, c.OWN, c.NCH, c.ATT, c.NQH, c.NKV, c.GQ, c.KVW, c.CW, c.NCB, c.INC, c.NT, c.NTO, c.PQ, c.NE)
    NYC = NQH + NCB

    def din(name, shape, dt=F32):
        return nc.dram_tensor(name, shape, dt, kind="ExternalInput").ap()

    def dscr(name, shape, dt):
        return nc.dram_tensor(name, shape, dt, kind=("ExternalOutput" if debug else "Internal")).ap()

    x = din("x", [SB, D])
    w_in = din("w_in", [D, INC])
    w_out = din("w_out", [D, D])
    w_query = din("w_query", [D, PQ])
    sub_keys = din("sub_keys", [16, 128, 128])
    w_down = din("w_down", [NE, D])
    w_up = din("w_up", [NE, D])
    g_mix = din("g_mix", [1, D])
    g_ffn = din("g_ffn", [1, D])
    g_fin = din("g_fin", [1, D])
    qk_g = din("qk_g", [128, 2])
    conv_w = din("conv_w", [128, NCB, 3])
    ao_g = din("ao_g", [128, NQH])
    co_g = din("co_g", [128, NCB])
    cosT = din("cosT", [128, SB])
    sinT = din("sinT", [128, SB])
    ident_d = din("ident", [128, 128], BF16)
    identf_d = din("identf", [128, 128])
    perm_d = din("perm", [128, 128], BF16)
    iota_d = din("iota", [128, 128])
    edge_d = din("edge", [128, 2])
    out = nc.dram_tensor("out", [OWN, D], F32, kind="ExternalOutput").ap()

    hT = dscr("hT", [NT, 128, NCH, 128], BF16)
    KT = dscr("KT", [NKV, 128, SB], BF16)
    Vs = dscr("Vs", [NKV, 128, NT, 128], BF16)
    QT = dscr("QT", [NQH, 128, OWN], BF16)
    YT = dscr("YT", [NYC, 128, OWN], BF16)
    X1 = dscr("X1", [OWN, D], F32)
    H2T = dscr("H2T", [NTO, 128, NCH, 128], BF16)
    QPT = dscr("QPT", [16, 128, OWN], BF16)
    WDT = dscr("WDT", [128, 128, NCH, 128], BF16)
    GS = dscr("GS", [NTO, 128, 128, 128], BF16)
    STA = dscr("STA", [128, 2 * NTO], F32)
    NWT = (ATT + 3 * CW) // 256
    WB = dscr("WB", [NWT, 128, NCH, 256], BF16)

    def wb_col0(i):
        return i * 256 if i < ATT // 256 else (ATT + 2 * KVW) + (i - ATT // 256) * 256

    P = Prog(nc)
    gst = ExitStack()

    def gsb(name, shape, dt):
        return gst.enter_context(nc.sbuf_tensor(name, shape, dt))

    psall = gst.enter_context(nc.psum_tensor("psall", [128, 4096], F32))
    ps = [psall[:, i * 512:(i + 1) * 512] for i in range(8)]
    ident = gsb("identS", [128, 128], BF16)
    identf = gsb("identfS", [128, 128], F32)
    ones = gsb("onesS", [128, 128], BF16)
    onesf = gsb("onesfS", [128, 128], F32)
    epsT = gsb("epsT", [128, 1], F32)
    consts = ["ident", "identf", "ones", "onesf", "eps"]

    def load_consts():
        P.dma("sp", ident[:], ident_d, w=["ident"])
        P.dma("sp", identf[:], identf_d, w=["identf"])
        P.add("dve", lambda e: e.memset(ones[:], 1.0), w=["ones"])
        P.add("dve", lambda e: e.memset(onesf[:], 1.0), w=["onesf"])
        P.add("dve", lambda e: e.memset(epsT[:], EPS), w=["eps"])

    def phase_norm_T(src, gain_d, dst, ntiles, tag, extra=None):
        with ExitStack() as st:
            extra_fn = extra(st) if extra is not None else None
            def sb(name, shape, dt):
                return st.enter_context(nc.sbuf_tensor(tag + name, shape, dt))
            gB = sb("gB", [128, D], F32)
            P.dma("sp", gB[:], gain_d.partition_broadcast(128), w=["gB"])
            xt = Ring(st, nc, tag + "xt", 2, [128, D], F32)
            junk = Ring(st, nc, tag + "junk", 2, [128, D], BF16)
            ss = Ring(st, nc, tag + "ss", 2, [128, 1], F32)
            hb = Ring(st, nc, tag + "hb", 2, [128, D], BF16)
            hTt = Ring(st, nc, tag + "hTt", 2, [128, NCH, 128], BF16)
            bi = 0
            for t in range(ntiles):
                xtt, xk = xt.next()
                jt, jk = junk.next()
                sst, sk = ss.next()
                hbt, hk = hb.next()
                htt, htk = hTt.next()
                P.dma("sp", xtt[:], src[t * 128:(t + 1) * 128, :], w=[xk])
                P.add("dve", lambda e, a=jt, b=xtt, s=sst: e.scalar_tensor_tensor(
                    out=a[:], in0=b[:], scalar=1.0, in1=b[:], op0=ALU.mult, op1=ALU.mult, accum_out=s[:]),
                    r=[xk], w=[jk, sk])
                P.add("act", lambda e, s=sst: e.activation(out=s[:], in_=s[:], func=AF.Sqrt, scale=1.0 / D,
                                                           bias=epsT[:, 0:1]), r=[sk, "eps"], w=[sk])
                P.add("dve", lambda e, s=sst: e.reciprocal(out=s[:], in_=s[:]), r=[sk], w=[sk])
                P.add("dve", lambda e, a=hbt, b=xtt, s=sst: e.scalar_tensor_tensor(
                    out=a[:], in0=b[:], scalar=s[:, 0:1], in1=gB[:], op0=ALU.mult, op1=ALU.mult),
                    r=[xk, sk, "gB"], w=[hk])
                for c0 in range(0, NCH, 8):
                    bk = bi % 4
                    bi += 1
                    pb = ps[bk][:].bitcast(BF16)

                    def tr(e, hbt=hbt, c0=c0, pb=pb):
                        ins = None
                        for cc in range(c0, c0 + 8):
                            ins = e.transpose(out=pb[:, (cc - c0) * 128:(cc - c0 + 1) * 128],
                                              in_=hbt[:, cc * 128:(cc + 1) * 128], identity=ident[:])
                        return ins
                    P.add("pe", tr, r=[hk, "ident"], w=[f"ps{bk}"])
                    P.add("act", lambda e, htt=htt, c0=c0, pb=pb: e.copy(
                        out=htt[:, c0:c0 + 8, :], in_=pb.rearrange("p (c t) -> p c t", c=8)),
                        r=[f"ps{bk}"], w=[htk])
                P.dma("act", dst[t], htt[:], r=[htk], w=[tag + "dst"])
                if extra_fn is not None:
                    extra_fn(t, ntiles)
            P.flush()

    def make_rope(st, tag):
        tmp = dict(
            qg=Ring(st, nc, tag + "qg", 2, [128, 512], BF16),
            sq=Ring(st, nc, tag + "sq", 2, [128, 512], BF16),
            rst=Ring(st, nc, tag + "rst", 2, [128, 512], F32),
            t1=Ring(st, nc, tag + "t1", 2, [128, 512], F32),
            t2=Ring(st, nc, tag + "t2", 2, [128, 512], F32),
        )
        return tmp

    def rope_epilogue(tmp, pq, pqk, gcol, Ct, Ck, St, Sk, perm, outt, outk, b1, b2):
        qg, qgk = tmp["qg"].next()
        sq, sqk = tmp["sq"].next()
        rst, rsk = tmp["rst"].next()
        t1, t1k = tmp["t1"].next()
        t2, t2k = tmp["t2"].next()
        P.add("dve", lambda e: e.tensor_scalar(out=qg[:], in0=pq, scalar1=gcol, scalar2=None, op0=ALU.mult),
              r=[pqk, "qkg"], w=[qgk])
        P.add("act", lambda e: e.activation(out=sq[:], in_=pq, func=AF.Square), r=[pqk], w=[sqk])
        P.add("pe", lambda e: e.matmul(ps[b1][:], ones[:], sq[:], start=True, stop=True), r=["ones", sqk], w=[f"ps{b1}"])
        P.add("pe", lambda e: e.matmul(ps[b2][:], perm[:], qg[:], start=True, stop=True), r=["perm", qgk], w=[f"ps{b2}"])
        P.add("act", lambda e: e.activation(out=rst[:], in_=ps[b1][:], func=AF.Sqrt, scale=1.0 / 128, bias=epsT[:, 0:1]),
              r=[f"ps{b1}", "eps"], w=[rsk])
        P.add("dve", lambda e: e.reciprocal(out=rst[:], in_=rst[:]), r=[rsk], w=[rsk])
        P.add("dve", lambda e: e.tensor_tensor(out=t1[:], in0=qg[:], in1=Ct, op=ALU.mult), r=[qgk, Ck], w=[t1k])
        P.add("dve", lambda e: e.tensor_tensor(out=t2[:], in0=ps[b2][:], in1=St, op=ALU.mult), r=[f"ps{b2}", Sk], w=[t2k])
        P.add("dve", lambda e: e.tensor_tensor(out=t1[:], in0=t1[:], in1=t2[:], op=ALU.add), r=[t1k, t2k], w=[t1k])
        P.add("dve", lambda e: e.tensor_tensor(out=outt, in0=t1[:], in1=rst[:], op=ALU.mult), r=[t1k, rsk], w=[outk])

    def phase_kv():
        with ExitStack() as st:
            def sb(name, shape, dt):
                return st.enter_context(nc.sbuf_tensor("kv" + name, shape, dt))
            Wk = sb("Wk", [128, NCH, KVW], BF16)
            Wv = sb("Wv", [128, NCH, KVW], BF16)
            perm = sb("perm", [128, 128], BF16)
            qkg = sb("qkg", [128, 2], F32)
            P.dma("sp", perm[:], perm_d, w=["perm"])
            P.dma("sp", qkg[:], qk_g, w=["qkg"])
            P.dma("pool", Wk[:], w_in[:, ATT:ATT + KVW].rearrange("(c p) n -> p c n", p=128), w=["Wk"])
            P.dma("pool", Wv[:], w_in[:, ATT + KVW:ATT + 2 * KVW].rearrange("(c p) n -> p c n", p=128), w=["Wv"])
            hg = Ring(st, nc, "kvhg", 2, [128, 4, NCH, 128], BF16)
            Cr = Ring(st, nc, "kvC", 2, [128, 512], F32)
            Sr = Ring(st, nc, "kvS", 2, [128, 512], F32)
            ktr = Ring(st, nc, "kvkt", 2, [128, 512], BF16)
            vtr = Ring(st, nc, "kvvt", 2, [128, 4, KVW], BF16)
            tmp = make_rope(st, "kv")
            cvr = Ring(st, nc, "kvcv", 2, [128, NCH, 256], BF16)
            ngr = SB // 512
            cv_i = 0
            cv_prev = None
            bi = 0
            for tg in range(SB // 512):
                while cv_i < NWT and cv_i * ngr < (tg + 1) * NWT:
                    cvt, cvk = cvr.next()
                    c0_ = wb_col0(cv_i)
                    P.dma("pool", cvt[:], w_in[:, c0_:c0_ + 256].rearrange("(c p) n -> p c n", p=128), w=[cvk])
                    if cv_prev is not None:
                        P.dma("pool", WB[cv_prev[0]], cv_prev[1][:], r=[cv_prev[2]], w=["WBd"])
                    cv_prev = (cv_i, cvt, cvk)
                    cv_i += 1
                hgt, hk = hg.next()
                Ct, Ck = Cr.next()
                St, Sk = Sr.next()
                P.dma("sp", hgt[:], hT[tg * 4:(tg + 1) * 4].rearrange("t p c k -> p t c k"), w=[hk])
                P.dma("sp", Ct[:], cosT[:, tg * 512:(tg + 1) * 512], w=[Ck])
                P.dma("sp", St[:], sinT[:, tg * 512:(tg + 1) * 512], w=[Sk])
                for j in range(NKV):
                    b = bi % 2
                    bi += 1
                    mm_group(P, ps[b][:].rearrange("p (t k) -> p t k", t=4),
                             [(Wk[:, cc, j * 128:(j + 1) * 128], hgt[:, :, cc, :]) for cc in range(NCH)],
                             r=["Wk", hk], w=[f"ps{b}"])
                    kt, kk = ktr.next()
                    rope_epilogue(tmp, ps[b][:], f"ps{b}", qkg[:, 1:2], Ct[:], Ck, St[:], Sk, perm, kt[:], kk, 2 + b, 4 + b)
                    P.dma("act", KT[j, :, tg * 512:(tg + 1) * 512], kt[:], r=[kk], w=["KTd"])
                vt, vk = vtr.next()
                for s in range(4):
                    b = 6 + (s % 2)
                    mm_group(P, ps[b][:, 0:KVW], [(hgt[:, s, cc, :], Wv[:, cc, :]) for cc in range(NCH)],
                             r=["Wv", hk], w=[f"ps{b}"])
                    P.add("act", lambda e, vt=vt, s=s, b=b: e.copy(out=vt[:, s, :], in_=ps[b][:, 0:KVW]),
                          r=[f"ps{b}"], w=[vk])
                for j in range(NKV):
                    P.dma("act", Vs[j, :, tg * 4:(tg + 1) * 4, :], vt[:, :, j * 128:(j + 1) * 128], r=[vk], w=["Vd"])
            if cv_prev is not None:
                P.dma("pool", WB[cv_prev[0]], cv_prev[1][:], r=[cv_prev[2]], w=["WBd"])
            P.flush()

    def phase_qc():
        with ExitStack() as st:
            def sb(name, shape, dt):
                return st.enter_context(nc.sbuf_tensor("qc" + name, shape, dt))
            perm = sb("perm", [128, 128], BF16)
            qkg = sb("qkg", [128, 2], F32)
            cw = sb("cw", [128, NCB, 3], F32)
            cog = sb("cog", [128, NCB], F32)
            edge = sb("edge", [128, 2], F32)
            P.dma("sp", perm[:], perm_d, w=["perm"])
            P.dma("sp", qkg[:], qk_g, w=["qkg"])
            P.dma("sp", cw[:], conv_w, w=["cw"])
            P.dma("sp", cog[:], co_g, w=["cog"])
            P.dma("sp", edge[:], edge_d, w=["edge"])
            hgt = sb("hg", [128, 4, NCH, 128], BF16)
            hL = sb("hL", [128, NCH, 128], BF16)
            hR = sb("hR", [128, NCH, 128], BF16)
            hH = sb("hH", [128, NCH, 2], BF16)
            Ct = sb("C", [128, 512], F32)
            St = sb("S", [128, 512], F32)
            Wr = Ring(st, nc, "qcW", 6, [128, NCH, 256], BF16)
            qtr = Ring(st, nc, "qcqt", 2, [128, 512], BF16)
            tmp = make_rope(st, "qc")
            gcS = Ring(st, nc, "qcgc", 2, [128, 512], F32)
            hS = Ring(st, nc, "qchS", 2, [128, 4], F32)
            ur = Ring(st, nc, "qcu", 2, [128, 514], F32)
            vr = Ring(st, nc, "qcv", 2, [128, 512], F32)
            ycr = Ring(st, nc, "qcyc", 2, [128, 512], F32)
            sqr = Ring(st, nc, "qcsqc", 2, [128, 512], F32)
            ybr = Ring(st, nc, "qcyb", 2, [128, 512], BF16)
            ssc = sb("ssc", [128, 512], F32)
            stc = sb("stc", [128, NTO], F32)
            bi = 0

            def wload(col0):
                Wt, Wk_ = Wr.next()
                wi = col0 // 256 if col0 < ATT else ATT // 256 + (col0 - (ATT + 2 * KVW)) // 256
                P.dma("pool", Wt[:], WB[wi], w=[Wk_])
                return Wt, Wk_

            for tg in range(OWN // 512):
                P.dma("sp", hgt[:], hT[tg * 4:(tg + 1) * 4].rearrange("t p c k -> p t c k"), w=["hg"])
                P.dma("sp", hL[:], hT[(tg * 4 - 1) % NT], w=["hL"])
                P.dma("sp", hR[:], hT[(tg * 4 + 4) % NT], w=["hR"])
                P.dma("sp", Ct[:], cosT[:, tg * 512:(tg + 1) * 512], w=["C"])
                P.dma("sp", St[:], sinT[:, tg * 512:(tg + 1) * 512], w=["S"])
                P.add("act", lambda e: e.copy(out=hH[:, :, 0:1], in_=hL[:, :, 127:128]), r=["hL"], w=["hH"])
                P.add("act", lambda e: e.copy(out=hH[:, :, 1:2], in_=hR[:, :, 0:1]), r=["hR", "hH"], w=["hH"])
                for q4 in range(ATT // 256):
                    Wt, Wk_ = wload(q4 * 256)
                    for hh in range(2):
                        h = q4 * 2 + hh
                        b = bi % 2
                        bi += 1
                        mm_group(P, ps[b][:].rearrange("p (t k) -> p t k", t=4),
                                 [(Wt[:, cc, hh * 128:(hh + 1) * 128], hgt[:, :, cc, :]) for cc in range(NCH)],
                                 r=[Wk_, "hg"], w=[f"ps{b}"])
                        qt, qk_ = qtr.next()
                        rope_epilogue(tmp, ps[b][:], f"ps{b}", qkg[:, 0:1], Ct[:], "C", St[:], "S", perm, qt[:], qk_, 2 + b, 4 + b)
                        P.dma("act", QT[h, :, tg * 512:(tg + 1) * 512], qt[:], r=[qk_], w=["QTd"])
                c0 = ATT + 2 * KVW
                for c4 in range(CW // 256):
                    Wx, Wxk = wload(c0 + c4 * 256)
                    Wb, Wbk = wload(c0 + CW + c4 * 256)
                    Wc, Wck = wload(c0 + 2 * CW + c4 * 256)
                    for hh in range(2):
                        cb = c4 * 2 + hh
                        cols = slice(hh * 128, (hh + 1) * 128)
                        for (bk, Wt, Wk_) in ((0, Wx, Wxk), (1, Wc, Wck), (2, Wb, Wbk)):
                            mm_group(P, ps[bk][:].rearrange("p (t k) -> p t k", t=4),
                                     [(Wt[:, cc, cols], hgt[:, :, cc, :]) for cc in range(NCH)],
                                     r=[Wk_, "hg"], w=[f"ps{bk}"])
                        mm_group(P, ps[3][:, 0:2], [(Wx[:, cc, cols], hH[:, cc, :]) for cc in range(NCH)],
                                 r=[Wxk, "hH"], w=["ps3"])
                        mm_group(P, ps[3][:, 2:4], [(Wc[:, cc, cols], hH[:, cc, :]) for cc in range(NCH)],
                                 r=[Wck, "hH", "ps3"], w=["ps3"])
                        gc, gck = gcS.next()
                        hs, hsk = hS.next()
                        u, uk = ur.next()
                        v, vk = vr.next()
                        yc, yck = ycr.next()
                        sq, sqk = sqr.next()
                        yb, ybk = ybr.next()
                        P.add("act", lambda e, gc=gc: e.copy(out=gc[:], in_=ps[1][:]), r=["ps1"], w=[gck])
                        P.add("act", lambda e, hs=hs: e.copy(out=hs[:], in_=ps[3][:, 0:4]), r=["ps3"], w=[hsk])
                        P.add("dve", lambda e, u=u, gc=gc: e.tensor_tensor(out=u[:, 1:513], in0=ps[0][:], in1=gc[:], op=ALU.mult),
                              r=["ps0", gck], w=[uk])
                        if tg == 0:
                            P.add("dve", lambda e, u=u, hs=hs: e.scalar_tensor_tensor(
                                out=u[:, 0:1], in0=hs[:, 0:1], scalar=edge[:, 0:1], in1=hs[:, 2:3], op0=ALU.mult, op1=ALU.mult),
                                r=[hsk, "edge", uk], w=[uk])
                        else:
                            P.add("dve", lambda e, u=u, hs=hs: e.tensor_tensor(
                                out=u[:, 0:1], in0=hs[:, 0:1], in1=hs[:, 2:3], op=ALU.mult), r=[hsk, uk], w=[uk])
                        if tg == OWN // 512 - 1:
                            P.add("dve", lambda e, u=u, hs=hs: e.scalar_tensor_tensor(
                                out=u[:, 513:514], in0=hs[:, 1:2], scalar=edge[:, 1:2], in1=hs[:, 3:4], op0=ALU.mult, op1=ALU.mult),
                                r=[hsk, "edge", uk], w=[uk])
                        else:
                            P.add("dve", lambda e, u=u, hs=hs: e.tensor_tensor(
                                out=u[:, 513:514], in0=hs[:, 1:2], in1=hs[:, 3:4], op=ALU.mult), r=[hsk, uk], w=[uk])
                        P.add("dve", lambda e, u=u, v=v, cb=cb: e.tensor_scalar(
                            out=v[:], in0=u[:, 0:512], scalar1=cw[:, cb, 0:1], scalar2=None, op0=ALU.mult),
                            r=[uk, "cw"], w=[vk])
                        P.add("dve", lambda e, u=u, v=v, cb=cb: e.scalar_tensor_tensor(
                            out=v[:], in0=u[:, 1:513], scalar=cw[:, cb, 1:2], in1=v[:], op0=ALU.mult, op1=ALU.add),
                            r=[uk, "cw", vk], w=[vk])
                        P.add("dve", lambda e, u=u, v=v, cb=cb: e.scalar_tensor_tensor(
                            out=v[:], in0=u[:, 2:514], scalar=cw[:, cb, 2:3], in1=v[:], op0=ALU.mult, op1=ALU.add),
                            r=[uk, "cw", vk], w=[vk])
                        P.add("dve", lambda e, v=v, yc=yc: e.tensor_tensor(out=yc[:], in0=ps[2][:], in1=v[:], op=ALU.mult),
                              r=["ps2", vk], w=[yck])
                        P.add("act", lambda e, sq=sq, yc=yc: e.activation(out=sq[:], in_=yc[:], func=AF.Square), r=[yck], w=[sqk])
                        if cb == 0:
                            P.add("pool", lambda e, sq=sq: e.tensor_copy(out=ssc[:], in_=sq[:]), r=[sqk], w=["ssc"])
                        else:
                            P.add("pool", lambda e, sq=sq: e.tensor_tensor(out=ssc[:], in0=ssc[:], in1=sq[:], op=ALU.add),
                                  r=[sqk, "ssc"], w=["ssc"])
                        P.add("dve", lambda e, yb=yb, yc=yc, cb=cb: e.tensor_scalar(
                            out=yb[:], in0=yc[:], scalar1=cog[:, cb:cb + 1], scalar2=None, op0=ALU.mult),
                            r=[yck, "cog"], w=[ybk])
                        P.dma("act", YT[NQH + cb, :, tg * 512:(tg + 1) * 512], yb[:], r=[ybk], w=["YTd"])
                for s in range(4):
                    P.add("pe", lambda e, s=s: e.matmul(ps[4][:, s:s + 1], ssc[:, s * 128:(s + 1) * 128], onesf[:, 0:1],
                                                        start=True, stop=True), r=["ssc", "onesf"], w=["ps4"])
                P.add("act", lambda e, tg=tg: e.copy(out=stc[:, tg * 4:(tg + 1) * 4], in_=ps[4][:, 0:4]), r=["ps4"], w=["stc"])
            P.dma("sp", STA[:, NTO:2 * NTO], stc[:], r=["stc"], w=["STAd"])
            P.flush()

    def phase_attn():
        with ExitStack() as st:
            def sb(name, shape, dt):
                return st.enter_context(nc.sbuf_tensor("at" + name, shape, dt))
            NKB = SB // 128
            aog = sb("aog", [128, NQH], F32)
            P.dma("sp", aog[:], ao_g, w=["aog"])
            Kr = Ring(st, nc, "atK", 2, [128, SB], BF16)
            Vr = Ring(st, nc, "atV", 2, [128, NKB, 128], BF16)
            Qr = Ring(st, nc, "atQ", 2, [128, 512], BF16)
            Pr = Ring(st, nc, "atP", 4, [128, 1024], BF16)
            owr = Ring(st, nc, "atow", 2, [128, 512], F32)
            d0r = Ring(st, nc, "atd0", 2, [128, 1024], F32)
            d1r = Ring(st, nc, "atd1", 2, [128, 1024], F32)
            rdr = Ring(st, nc, "atrd", 2, [128, 512], F32)
            orr = Ring(st, nc, "ato", 2, [128, 512], F32)
            sqr = Ring(st, nc, "atsq", 2, [128, 512], F32)
            ybr = Ring(st, nc, "atyb", 2, [128, 512], BF16)
            ssa = sb("ssa", [128, OWN], F32)
            sta = sb("sta", [128, NTO], F32)
            scale = 128.0 ** -0.5
            NKP = NKB // 2
            items = [(j, tg, g) for j in range(NKV) for tg in range(OWN // 512) for g in range(GQ)]
            kv = {}

            def load_kv(j):
                Kt, Kk = Kr.next()
                Vt, Vk = Vr.next()
                P.dma("sp", Kt[:], KT[j], w=[Kk])
                P.dma("sp", Vt[:], Vs[j], w=[Vk])
                kv[j] = (Kt, Kk, Vt, Vk)

            qs = {}

            def load_q(i):
                j, tg, g = items[i]
                Qt, Qk = Qr.next()
                P.dma("sp", Qt[:], QT[j * GQ + g, :, tg * 512:(tg + 1) * 512], w=[Qk])
                qs[i] = (Qt, Qk)

            def s_mm(i, kp):
                j = items[i][0]
                Kt, Kk = kv[j][0], kv[j][1]
                Qt, Qk = qs[i]
                b0 = 2 * ((i * NKP + kp) % 3)

                def fn(e, kp=kp, b0=b0, Kt=Kt, Qt=Qt):
                    ins = None
                    for u in range(2):
                        kb = kp * 2 + u
                        ins = e.matmul(ps[b0 + u][:], Kt[:, kb * 128:(kb + 1) * 128], Qt[:], start=True, stop=True)
                    return ins
                P.add("pe", fn, r=[Kk, Qk], w=[f"ps{b0}", f"ps{b0 + 1}"])

            load_kv(0)
            load_q(0)
            s_mm(0, 0)
            s_mm(0, 1)
            for i, (j, tg, g) in enumerate(items):
                h = j * GQ + g
                Kt, Kk, Vt, Vk = kv[j]
                if i + 1 < len(items):
                    if items[i + 1][0] != j:
                        load_kv(items[i + 1][0])
                    load_q(i + 1)
                bo = 6
                bd = 7
                d0, d0k = d0r.next()
                d1, d1k = d1r.next()
                pe_den_started = False
                inited = set()
                for kp in range(NKP):
                    if kp + 2 < NKP:
                        s_mm(i, kp + 2)
                    elif i + 1 < len(items):
                        s_mm(i + 1, kp + 2 - NKP)
                    b0 = 2 * ((i * NKP + kp) % 3)
                    pt, pk = Pr.next()
                    P.add("act", lambda e, pt=pt, b0=b0: e.activation(
                        out=pt[:], in_=psall[:, b0 * 512:(b0 + 2) * 512], func=AF.Exp, scale=scale),
                        r=[f"ps{b0}", f"ps{b0 + 1}"], w=[pk])

                    def pv(e, pt=pt, kp=kp, bo=bo, Vt=Vt):
                        ins = None
                        for u in range(2):
                            kb = kp * 2 + u
                            ins = e.matmul(ps[bo][:], Vt[:, kb, :], pt[:, u * 512:(u + 1) * 512],
                                           start=(kb == 0), stop=(kb == NKB - 1))
                        return ins
                    P.add("pe", pv, r=[Vk, pk], w=[f"ps{bo}"])
                    who = ("dve", "pool", "pe", "dve", "pool", "dve", "pool", "pe")[kp % 8]
                    if who == "pe":
                        def dn(e, pt=pt, bd=bd, first=(not pe_den_started)):
                            ins = None
                            for u in range(2):
                                ins = e.matmul(ps[bd][:], ones[:], pt[:, u * 512:(u + 1) * 512],
                                               start=(first and u == 0), stop=False)
                            return ins
                        P.add("pe", dn, r=["ones", pk], w=[f"ps{bd}"])
                        pe_den_started = True
                    else:
                        eng, dd, ddk = ("dve", d0, d0k) if who == "dve" else ("pool", d1, d1k)
                        if ddk not in inited:
                            inited.add(ddk)
                            P.add(eng, lambda e, dd=dd, pt=pt: e.tensor_copy(out=dd[:], in_=pt[:]), r=[pk], w=[ddk])
                        else:
                            P.add(eng, lambda e, dd=dd, pt=pt: e.tensor_tensor(out=dd[:], in0=dd[:], in1=pt[:], op=ALU.add),
                                  r=[pk, ddk], w=[ddk])

                def dfin(e, d0=d0, d1=d1, bd=bd, st0=(not pe_den_started)):
                    ins = None
                    srcs = [d0[:, 0:512], d0[:, 512:1024], d1[:, 0:512], d1[:, 512:1024]]
                    for i_, sr in enumerate(srcs):
                        ins = e.matmul(ps[bd][:], onesf[:], sr, start=(st0 and i_ == 0), stop=(i_ == 3))
                    return ins
                ow, owk = owr.next()
                P.add("act", lambda e, ow=ow, bo=bo: e.copy(out=ow[:], in_=ps[bo][:]), r=[f"ps{bo}"], w=[owk])
                P.add("pe", dfin, r=["onesf", d0k, d1k], w=[f"ps{bd}"])
                rd, rdk = rdr.next()
                o, ok_ = orr.next()
                sq, sqk = sqr.next()
                yb, ybk = ybr.next()
                P.add("dve", lambda e, rd=rd, bd=bd: e.reciprocal(out=rd[:], in_=ps[bd][:]), r=[f"ps{bd}"], w=[rdk])
                P.add("dve", lambda e, o=o, rd=rd, ow=ow: e.tensor_tensor(out=o[:], in0=ow[:], in1=rd[:], op=ALU.mult),
                      r=[owk, rdk], w=[ok_])
                P.add("act", lambda e, sq=sq, o=o: e.activation(out=sq[:], in_=o[:], func=AF.Square), r=[ok_], w=[sqk])
                sl = slice(tg * 512, (tg + 1) * 512)
                if h == 0:
                    P.add("pool", lambda e, sq=sq, sl=sl: e.tensor_copy(out=ssa[:, sl], in_=sq[:]), r=[sqk], w=[f"ssa{tg}"])
                else:
                    P.add("pool", lambda e, sq=sq, sl=sl: e.tensor_tensor(out=ssa[:, sl], in0=ssa[:, sl], in1=sq[:], op=ALU.add),
                          r=[sqk, f"ssa{tg}"], w=[f"ssa{tg}"])
                P.add("dve", lambda e, yb=yb, o=o, h=h: e.tensor_scalar(
                    out=yb[:], in0=o[:], scalar1=aog[:, h:h + 1], scalar2=None, op0=ALU.mult), r=[ok_, "aog"], w=[ybk])
                P.dma("act", YT[h, :, sl], yb[:], r=[ybk], w=["YTd"])
            for t in range(NTO):
                P.add("pe", lambda e, t=t: e.matmul(ps[7][:, t:t + 1], ssa[:, t * 128:(t + 1) * 128], onesf[:, 0:1],
                                                    start=True, stop=True), r=[f"ssa{t // 4}", "onesf"], w=["ps7"])
            P.add("act", lambda e: e.copy(out=sta[:], in_=ps[7][:, 0:NTO]), r=["ps7"], w=["sta"])
            P.dma("sp", STA[:, 0:NTO], sta[:], r=["sta"], w=["STAd"])
            P.flush()

    def phase_out():
        with ExitStack() as st:
            def sb(name, shape, dt):
                return st.enter_context(nc.sbuf_tensor("op" + name, shape, dt))
            rs = sb("rs", [128, 2 * NTO], F32)
            P.dma("sp", rs[:], STA, w=["rs"])
            P.add("act", lambda e: e.activation(out=rs[:], in_=rs[:], func=AF.Sqrt, scale=1.0 / ATT, bias=epsT[:, 0:1]),
                  r=["rs", "eps"], w=["rs"])
            P.add("dve", lambda e: e.reciprocal(out=rs[:], in_=rs[:]), r=["rs"], w=["rs"])
            ygr = Ring(st, nc, "opyg", 2, [128, NYC, 512], BF16)
            Wr = Ring(st, nc, "opW", 2, [128, NYC, 512], BF16)
            xr = Ring(st, nc, "opx", 4, [128, 512], F32)
            tr_ = Ring(st, nc, "opt", 2, [128, 512], F32)
            orr = Ring(st, nc, "opo", 3, [128, 512], F32)
            bi = 0
            for dc in range(D // 512):
                Wt, Wk_ = Wr.next()
                P.dma("pool", Wt[:], w_out[:, dc * 512:(dc + 1) * 512].rearrange("(c p) n -> p c n", p=128), w=[Wk_])
                for tg in range(OWN // 512):
                    yg, ygk = ygr.next()
                    P.dma("sp", yg[:], YT[:, :, tg * 512:(tg + 1) * 512].rearrange("c p t -> p c t"), w=[ygk])
                    for s in range(4):
                        ba = (bi % 4) * 2
                        bi += 1
                        t_ = tg * 4 + s
                        xp, xk = xr.next()
                        P.dma("sp", xp[:], x[t_ * 128:(t_ + 1) * 128, dc * 512:(dc + 1) * 512], w=[xk])
                        mm_group(P, ps[ba][:], [(yg[:, cc, s * 128:(s + 1) * 128], Wt[:, cc, :]) for cc in range(NQH)],
                                 r=[ygk, Wk_], w=[f"ps{ba}"])
                        mm_group(P, ps[ba + 1][:], [(yg[:, cc, s * 128:(s + 1) * 128], Wt[:, cc, :]) for cc in range(NQH, NYC)],
                                 r=[ygk, Wk_], w=[f"ps{ba + 1}"])
                        tt, tk = tr_.next()
                        ot, ok_ = orr.next()
                        P.add("dve", lambda e, tt=tt, ba=ba, t_=t_, xp=xp: e.scalar_tensor_tensor(
                            out=tt[:], in0=ps[ba][:], scalar=rs[:, t_:t_ + 1], in1=xp[:], op0=ALU.mult, op1=ALU.add),
                            r=[f"ps{ba}", "rs", xk], w=[tk])
                        P.add("dve", lambda e, tt=tt, ot=ot, ba=ba, t_=t_: e.scalar_tensor_tensor(
                            out=ot[:], in0=ps[ba + 1][:], scalar=rs[:, NTO + t_:NTO + t_ + 1], in1=tt[:], op0=ALU.mult, op1=ALU.add),
                            r=[f"ps{ba + 1}", "rs", tk], w=[ok_])
                        P.dma("act", X1[t_ * 128:(t_ + 1) * 128, dc * 512:(dc + 1) * 512], ot[:], r=[ok_], w=["X1d"])
            P.flush()

    def wdt_prep(st):
        HC = max(NCH // 2, 1)
        HD = HC * 128
        NPJ = NCH // HC
        Wr = Ring(st, nc, "ppW", 2, [128, HD], BF16)
        Wfr = Ring(st, nc, "ppWf", 2, [128, HD], F32)
        Tr = Ring(st, nc, "ppT", 2, [128, HC, 128], BF16)
        wdv = w_down.rearrange("(i j) d -> j i d", j=128)
        state = {"bi": 0, "n": 0}

        def one(n):
            j, hp = n // NPJ, n % NPJ
            Wt, Wk_ = Wr.next()
            Tt, Tk = Tr.next()
            Wf, Wfk = Wfr.next()
            P.dma("sp", Wf[:], wdv[j][:, hp * HD:(hp + 1) * HD], w=[Wfk])
            per_tile = max(total // NTO, 1)
            eng = "act" if (n % per_tile) < (3 * per_tile) // 8 else "pool"
            if eng == "pool":
                P.add("pool", lambda e, Wt=Wt, Wf=Wf: e.tensor_copy(out=Wt[:], in_=Wf[:]), r=[Wfk], w=[Wk_])
            else:
                P.add("act", lambda e, Wt=Wt, Wf=Wf: e.copy(out=Wt[:], in_=Wf[:]), r=[Wfk], w=[Wk_])
            for c0 in range(0, HC, 8):
                nb = min(8, HC - c0)
                bk = (4, 7)[state["bi"] % 2]
                state["bi"] += 1
                pb = ps[bk][:].bitcast(BF16)

                def tr(e, Wt=Wt, c0=c0, pb=pb, nb=nb):
                    ins = None
                    for cc in range(c0, c0 + nb):
                        ins = e.transpose(out=pb[:, (cc - c0) * 128:(cc - c0 + 1) * 128],
                                          in_=Wt[:, cc * 128:(cc + 1) * 128], identity=ident[:])
                    return ins
                P.add("pe", tr, r=[Wk_, "ident"], w=[f"ps{bk}"])
                P.add("act", lambda e, Tt=Tt, c0=c0, pb=pb, nb=nb: e.copy(
                    out=Tt[:, c0:c0 + nb, :], in_=pb[:, 0:nb * 128].rearrange("p (c t) -> p c t", c=nb)), r=[f"ps{bk}"], w=[Tk])
            P.dma("act", WDT[j][:, hp * HC:(hp + 1) * HC, :], Tt[:], r=[Tk], w=["WDTd"])

        total = 128 * NPJ

        def step(k, nsteps):
            hi_ = (total * (k + 1)) // nsteps
            while state["n"] < hi_:
                one(state["n"])
                state["n"] += 1
        return step

    def phase_peer_prep():
        with ExitStack() as st:
            def sb(name, shape, dt):
                return st.enter_context(nc.sbuf_tensor("pq" + name, shape, dt))
            hgr = Ring(st, nc, "pqhg", 2, [128, 4, NCH, 128], BF16)
            Wr = Ring(st, nc, "pqW", 2, [128, NCH, 512], BF16)
            qr = Ring(st, nc, "pqq", 3, [128, 512], BF16)
            bi = 0
            for q4 in range(PQ // 512):
                Wt, Wk_ = Wr.next()
                P.dma("pool", Wt[:], w_query[:, q4 * 512:(q4 + 1) * 512].rearrange("(c p) n -> p c n", p=128), w=[Wk_])
                for tg in range(OWN // 512):
                    hgt, hgk = hgr.next()
                    P.dma("sp", hgt[:], H2T[tg * 4:(tg + 1) * 4].rearrange("t p c k -> p t c k"), w=[hgk])
                    for hh in range(4):
                        b = bi % 4
                        bi += 1
                        mm_group(P, ps[b][:].rearrange("p (t k) -> p t k", t=4),
                                 [(Wt[:, cc, hh * 128:(hh + 1) * 128], hgt[:, :, cc, :]) for cc in range(NCH)],
                                 r=[Wk_, hgk], w=[f"ps{b}"])
                        qt, qk_ = qr.next()
                        P.add("act", lambda e, qt=qt, b=b: e.copy(out=qt[:], in_=ps[b][:]), r=[f"ps{b}"], w=[qk_])
                        P.dma("act", QPT[q4 * 4 + hh, :, tg * 512:(tg + 1) * 512], qt[:], r=[qk_], w=["QPTd"])
            P.flush()

    def phase_peer_pick():
        with ExitStack() as st:
            def sb(name, shape, dt):
                return st.enter_context(nc.sbuf_tensor("pk" + name, shape, dt))
            kn = sb("kn", [128, 16, 128], BF16)
            kT = sb("kT", [128, 16, 128], BF16)
            iot = sb("iot", [128, 128], F32)
            P.dma("pool", kn[:], sub_keys.rearrange("g n k -> n g k"), w=["kn"])
            P.dma("sp", iot[:], iota_d, w=["iot"])
            for g0 in range(0, 16, 8):
                pb = ps[0][:].bitcast(BF16)

                def tr(e, g0=g0, pb=pb):
                    ins = None
                    for g in range(g0, g0 + 8):
                        ins = e.transpose(out=pb[:, (g - g0) * 128:(g - g0 + 1) * 128], in_=kn[:, g, :], identity=ident[:])
                    return ins
                P.add("pe", tr, r=["kn", "ident"], w=["ps0"])
                P.add("act", lambda e, g0=g0, pb=pb: e.copy(out=kT[:, g0:g0 + 8, :], in_=pb.rearrange("p (c t) -> p c t", c=8)),
                      r=["ps0"], w=["kT"])
            qpr = Ring(st, nc, "pkqp", 2, [128, 16, 128], BF16)
            S = sb("S", [128, 16, 128], F32)
            S2 = sb("S2", [128, 16, 128], F32)
            V = sb("V", [128, 16, 16], F32)
            IX = sb("IX", [128, 16, 16], U32)
            IXf = sb("IXf", [128, 16, 16], F32)
            cand = sb("cand", [128, 8, 256], F32)
            cand2 = sb("cand2", [128, 8, 256], F32)
            Bv = sb("Bv", [128, 8, 16], F32)
            PX = sb("PX", [128, 8, 16], U32)
            R1u = sb("R1u", [128, 8, 16], U32)
            R2u = sb("R2u", [128, 8, 16], U32)
            R1 = sb("R1", [128, 8, 16], F32)
            R2 = sb("R2", [128, 8, 16], F32)
            E = sb("E", [128, 8, 16], F32)
            Z = sb("Z", [128, 8], F32)
            eq = sb("eq", [128, 8, 16, 16], F32)
            PI = sb("PI", [128, 128], F32)
            PJ = sb("PJ", [128, 128], F32)
            PG = sb("PG", [128, 128], F32)
            PT = sb("PT", [128, 3, 128], F32)
            OI = sb("OI", [128, 128, 128], BF16)
            OJ = sb("OJ", [128, 128, 128], BF16)
            Gr = Ring(st, nc, "pkG", 1, [128, 128, 128], BF16)
            prep_step = wdt_prep(st)
            prep_k = 0

            def dv(fn, r, w):
                P.add("dve", fn, r=r, w=w)

            def scores(tt):
                qp, qpk = qpr.next()
                P.dma("sp", qp[:], QPT[:, :, tt * 128:(tt + 1) * 128].rearrange("g p t -> p g t"), w=[qpk])
                for b in range(4):
                    for g4 in range(4):
                        g = b * 4 + g4
                        P.add("pe", lambda e, g=g, g4=g4, b=b, qp=qp: e.matmul(
                            ps[b][:, g4 * 128:(g4 + 1) * 128], qp[:, g, :], kT[:, g, :], start=True, stop=True),
                            r=[qpk, "kT"] + ([f"ps{b}"] if g4 else []), w=[f"ps{b}"])
                    P.add("act", lambda e, b=b: e.copy(out=S[:, b * 4:(b + 1) * 4, :],
                                                       in_=ps[b][:].rearrange("p (g n) -> p g n", g=4)),
                          r=[f"ps{b}"], w=[f"S{g_}" for g_ in range(b * 4, b * 4 + 4)])

            def topk():
                G16 = range(16)
                for g in G16:
                    dv(lambda e, g=g: e.max(out=V[:, g, 0:8], in_=S[:, g, :]), [f"S{g}"], [f"Va{g}"])
                for g in G16:
                    dv(lambda e, g=g: e.max_index(out=IX[:, g, 0:8], in_max=V[:, g, 0:8], in_values=S[:, g, :]),
                       [f"S{g}", f"Va{g}"], [f"IXa{g}"])
                for g in G16:
                    dv(lambda e, g=g: e.match_replace(out=S2[:, g, :], in_to_replace=V[:, g, 0:8], in_values=S[:, g, :],
                                                      imm_value=NEG), [f"S{g}", f"Va{g}"], [f"S2{g}"])
                for g in G16:
                    dv(lambda e, g=g: e.max(out=V[:, g, 8:16], in_=S2[:, g, :]), [f"S2{g}"], [f"Vb{g}"])
                for g in G16:
                    dv(lambda e, g=g: e.max_index(out=IX[:, g, 8:16], in_max=V[:, g, 8:16], in_values=S2[:, g, :]),
                       [f"S2{g}", f"Vb{g}"], [f"IXb{g}"])
                allV = [f"Va{g}" for g in G16] + [f"Vb{g}" for g in G16]
                allIX = [f"IXa{g}" for g in G16] + [f"IXb{g}" for g in G16]
                dv(lambda e: e.tensor_copy(out=IXf[:], in_=IX[:]), allIX, ["IXf"])
                Vv = V[:].rearrange("p (h two) k -> p h two k", two=2)
                H8 = range(8)
                dv(lambda e, Vv=Vv: e.tensor_tensor(
                    out=cand[:].rearrange("p h (a b) -> p h a b", a=16),
                    in0=Vv[:, :, 0, :].unsqueeze(3).to_broadcast([128, 8, 16, 16]),
                    in1=Vv[:, :, 1, :].unsqueeze(2).to_broadcast([128, 8, 16, 16]), op=ALU.add), allV, [f"cand{h}" for h in H8])
                for h in H8:
                    dv(lambda e, h=h: e.max(out=Bv[:, h, 0:8], in_=cand[:, h, :]), [f"cand{h}"], [f"Ba{h}"])
                for h in H8:
                    dv(lambda e, h=h: e.max_index(out=PX[:, h, 0:8], in_max=Bv[:, h, 0:8], in_values=cand[:, h, :]),
                       [f"cand{h}", f"Ba{h}"], [f"PXa{h}"])
                for h in H8:
                    dv(lambda e, h=h: e.match_replace(out=cand2[:, h, :], in_to_replace=Bv[:, h, 0:8], in_values=cand[:, h, :],
                                                      imm_value=NEG), [f"cand{h}", f"Ba{h}"], [f"cand2{h}"])
                for h in H8:
                    dv(lambda e, h=h: e.max(out=Bv[:, h, 8:16], in_=cand2[:, h, :]), [f"cand2{h}"], [f"Bb{h}"])
                for h in H8:
                    dv(lambda e, h=h: e.max_index(out=PX[:, h, 8:16], in_max=Bv[:, h, 8:16], in_values=cand2[:, h, :]),
                       [f"cand2{h}", f"Bb{h}"], [f"PXb{h}"])
                allB = [f"Ba{h}" for h in H8] + [f"Bb{h}" for h in H8]
                allPX = [f"PXa{h}" for h in H8] + [f"PXb{h}" for h in H8]

            def topk_b():
                H8 = range(8)
                allB = [f"Ba{h}" for h in H8] + [f"Bb{h}" for h in H8]
                allPX = [f"PXa{h}" for h in H8] + [f"PXb{h}" for h in H8]
                dv(lambda e: e.tensor_tensor(out=E[:], in0=Bv[:], in1=Bv[:, :, 0:1].to_broadcast([128, 8, 16]), op=ALU.subtract),
                   allB, ["E"])
                P.add("act", lambda e: e.activation(out=E[:], in_=E[:], func=AF.Exp), r=["E"], w=["E"])
                dv(lambda e: e.tensor_reduce(out=Z[:], in_=E[:], axis=AX.X, op=ALU.add), ["E"], ["Z"])
                dv(lambda e: e.reciprocal(out=Z[:], in_=Z[:]), ["Z"], ["Z"])
                dv(lambda e: e.tensor_tensor(out=PG[:].rearrange("p (h k) -> p h k", h=8), in0=E[:],
                                             in1=Z[:].unsqueeze(2).to_broadcast([128, 8, 16]), op=ALU.mult), ["E", "Z"], ["PG"])
                dv(lambda e: e.tensor_single_scalar(out=R1u[:], in_=PX[:], scalar=4, op=ALU.logical_shift_right), allPX, ["R1u"])
                dv(lambda e: e.tensor_single_scalar(out=R2u[:], in_=PX[:], scalar=15, op=ALU.bitwise_and), allPX, ["R2u"])
                dv(lambda e: e.tensor_copy(out=R1[:], in_=R1u[:]), ["R1u"], ["R1"])
                dv(lambda e: e.tensor_copy(out=R2[:], in_=R2u[:]), ["R2u"], ["R2"])
                IXv = IXf[:].rearrange("p (h two) k -> p h two k", two=2)
                for (Rr, Rk, two, Pout, Pk) in ((R1, "R1", 0, PI, "PI"), (R2, "R2", 1, PJ, "PJ")):
                    dv(lambda e, Rr=Rr: e.tensor_tensor(
                        out=eq[:], in0=Rr[:].unsqueeze(3).to_broadcast([128, 8, 16, 16]),
                        in1=iot[:, 0:16].unsqueeze(1).unsqueeze(1).to_broadcast([128, 8, 16, 16]), op=ALU.is_equal),
                        [Rk, "iot"], ["eq"])
                    dv(lambda e, two=two, IXv=IXv: e.tensor_tensor(
                        out=eq[:], in0=eq[:], in1=IXv[:, :, two, :].unsqueeze(2).to_broadcast([128, 8, 16, 16]), op=ALU.mult),
                        ["eq", "IXf"], ["eq"])
                    dv(lambda e, Pout=Pout: e.tensor_reduce(out=Pout[:].rearrange("p (h k) -> p h k", h=8), in_=eq[:],
                                                            axis=AX.X, op=ALU.add), ["eq"], [Pk])

            scores(0)
            topk()
            topk_b()
            for tt in range(NTO):
                for n_, (Pin, Pk) in enumerate(((PI, "PI"), (PJ, "PJ"), (PG, "PG"))):
                    P.add("pe", lambda e, n_=n_, Pin=Pin: e.transpose(out=ps[4][:, n_ * 128:(n_ + 1) * 128], in_=Pin[:], identity=identf[:]),
                          r=[Pk, "identf"] + (["ps4"] if n_ else []), w=["ps4"])
                P.add("act", lambda e: e.copy(out=PT[:], in_=ps[4][:, 0:384].rearrange("p (a t) -> p a t", a=3)), r=["ps4"], w=["PT"])
                dv(lambda e: e.tensor_tensor(
                    out=OJ[:], in0=PT[:, 1, :].unsqueeze(2).to_broadcast([128, 128, 128]),
                    in1=iot[:].unsqueeze(1).to_broadcast([128, 128, 128]), op=ALU.is_equal), ["PT", "iot"], ["OJ"])
                P.add("pool", lambda e: e.tensor_tensor(
                    out=OJ[:], in0=OJ[:], in1=PT[:, 2, :].unsqueeze(2).to_broadcast([128, 128, 128]), op=ALU.mult),
                    r=["PT", "OJ"], w=["OJ"])
                dv(lambda e: e.tensor_tensor(
                    out=OI[:], in0=PT[:, 0, :].unsqueeze(2).to_broadcast([128, 128, 128]),
                    in1=iot[:].unsqueeze(1).to_broadcast([128, 128, 128]), op=ALU.is_equal), ["PT", "iot"], ["OI"])
                if tt + 1 < NTO:
                    scores(tt + 1)
                    topk()
                Gt, Gk = Gr.next()
                prep_step(prep_k, NTO)
                prep_k += 1
                for t4 in range(32):
                    b = 5 + (t4 % 2)

                    def gm(e, t4=t4, b=b):
                        ins = None
                        for u in range(4):
                            t_ = t4 * 4 + u
                            ins = e.matmul(ps[b][:, u * 128:(u + 1) * 128], OI[:, t_, :], OJ[:, t_, :], start=True, stop=True)
                        return ins
                    P.add("pe", gm, r=["OI", "OJ"], w=[f"ps{b}"])
                    outv = Gt[:, :, t4 * 4:(t4 + 1) * 4].rearrange("p j t -> p t j")
                    inv = ps[b][:].rearrange("p (t j) -> p t j", t=4)
                    if t4 < 20:
                        P.add("act", lambda e, outv=outv, inv=inv: e.copy(out=outv, in_=inv), r=[f"ps{b}"], w=[Gk])
                    else:
                        P.add("dve", lambda e, outv=outv, inv=inv: e.tensor_copy(out=outv, in_=inv), r=[f"ps{b}"], w=[Gk])
                P.dma("act", GS[tt], Gt[:], r=[Gk], w=["GSd"])
                if tt + 1 < NTO:
                    topk_b()
            P.flush()

    def phase_peer_main():
        with ExitStack() as st:
            def sb(name, shape, dt):
                return st.enter_context(nc.sbuf_tensor("pm" + name, shape, dt))
            gB = sb("gB", [128, D], F32)
            P.dma("sp", gB[:], g_fin.partition_broadcast(128), w=["gB"])
            hgt = sb("hg", [128, 4, NCH, 128], BF16)
            acc = sb("acc", [128, 4, D], F32)
            WDr = Ring(st, nc, "pmWD", 4, [128, NCH, 128], BF16)
            WUr = Ring(st, nc, "pmWU", 2, [128, 2, D], BF16)
            Gr = Ring(st, nc, "pmG", 4, [128, 4, 2, 128], BF16)
            Dgr = Ring(st, nc, "pmDg", 2, [128, 2, 512], BF16)
            Ar = Ring(st, nc, "pmA", 3, [128, 2, 512], BF16)
            ss = sb("ss", [128, 4], F32)
            wuv = w_up.rearrange("(i j) d -> i j d", j=128)
            NJG = 64
            NUP = 4 * (D // 512)
            ui = 0
            for tg in range(OWN // 512):
                P.dma("sp", hgt[:], H2T[tg * 4:(tg + 1) * 4].rearrange("t p c k -> p t c k"), w=["hg"])
                P.dma("sp", acc[:], X1[tg * 512:(tg + 1) * 512, :].rearrange("(s p) d -> p s d", p=128), w=["acc"])
                stt = {}

                def load_down(jg, tg=tg, stt=stt):
                    d = stt.setdefault(jg, {})
                    d["WD"] = []
                    for jj in range(2):
                        WD, WDk = WDr.next()
                        P.dma("sp", WD[:], WDT[jg * 2 + jj], w=[WDk])
                        d["WD"].append((WD, WDk))
                    Gt, Gk = Gr.next()
                    P.dma("sp", Gt[:], GS[tg * 4:(tg + 1) * 4, :, jg * 2:(jg + 1) * 2, :].rearrange("s i j t -> i s j t"), w=[Gk])
                    d["G"] = (Gt, Gk)

                def load_up(jg, stt=stt):
                    WU, WUk = WUr.next()
                    P.dma("pool", WU[:], wuv[:, jg * 2:(jg + 1) * 2, :], w=[WUk])
                    stt.setdefault(jg, {})["WU"] = (WU, WUk)

                def down_pieces(jg, stt=stt):
                    d = stt[jg]
                    Dg, Dgk = Dgr.next()
                    A, Ak = Ar.next()
                    d["A"] = (A, Ak)
                    Gt, Gk = d["G"]
                    pieces = []
                    for k in range(NUP):
                        def piece(k=k, d=d, Dg=Dg, Dgk=Dgk, A=A, Ak=Ak, Gt=Gt, Gk=Gk):
                            ms = []
                            for m in (2 * k, 2 * k + 1):
                                jj, cc = m // NCH, m % NCH
                                ms.append((jj, cc))
                            jj = ms[0][0]
                            WD, WDk = d["WD"][jj]

                            def fn(e, ms=ms, WD=WD, jj=jj):
                                ins = None
                                for (_, cc) in ms:
                                    ins = e.matmul(ps[jj][:].rearrange("p (t k) -> p t k", t=4), WD[:, cc, :], hgt[:, :, cc, :],
                                                   start=(cc == 0), stop=(cc == NCH - 1))
                                return ins
                            P.add("pe", fn, r=[WDk, "hg"], w=[f"ps{jj}"])
                            if ms[-1][1] == NCH - 1:
                                P.add("act", lambda e, Dg=Dg, jj=jj: e.activation(out=Dg[:, jj, :], in_=ps[jj][:], func=AF.Gelu),
                                      r=[f"ps{jj}"], w=[Dgk])
                                if jj == 1:
                                    P.add("pool", lambda e, A=A, Dg=Dg, Gt=Gt: e.tensor_tensor(
                                        out=A[:].rearrange("p j (s t) -> p j s t", s=4),
                                        in0=Dg[:].rearrange("p j (s t) -> p j s t", s=4),
                                        in1=Gt[:].rearrange("p s j t -> p j s t"), op=ALU.mult), r=[Dgk, Gk], w=[Ak])
                        pieces.append(piece)
                    return pieces

                load_down(0)
                load_down(1)
                load_up(0)
                for pc in down_pieces(0):
                    pc()
                load_down(2)
                for pc in down_pieces(1):
                    pc()
                for jg in range(NJG):
                    if jg + 3 < NJG:
                        load_down(jg + 3)
                    if jg + 1 < NJG:
                        load_up(jg + 1)
                    pieces = down_pieces(jg + 2) if jg + 2 < NJG else []
                    A, Ak = stt[jg]["A"]
                    WU, WUk = stt[jg]["WU"]
                    k = 0
                    for s_ in range(4):
                        for dc in range(D // 512):
                            b = 2 + (ui % 6)
                            ui += 1
                            mm_group(P, ps[b][:], [(A[:, jj, s_ * 128:(s_ + 1) * 128], WU[:, jj, dc * 512:(dc + 1) * 512]) for jj in range(2)],
                                     r=[Ak, WUk], w=[f"ps{b}"])
                            P.add("dve", lambda e, s_=s_, dc=dc, b=b: e.tensor_tensor(
                                out=acc[:, s_, dc * 512:(dc + 1) * 512], in0=acc[:, s_, dc * 512:(dc + 1) * 512], in1=ps[b][:], op=ALU.add),
                                r=[f"ps{b}", "acc"], w=["acc"])
                            if pieces:
                                pieces[k]()
                            k += 1
                    del stt[jg]
                for s in range(4):
                    P.add("dve", lambda e, s=s: e.scalar_tensor_tensor(
                        out=hgt[:].rearrange("p a c k -> p (a c k)")[:, 0:D], in0=acc[:, s, :], scalar=1.0, in1=acc[:, s, :],
                        op0=ALU.mult, op1=ALU.mult, accum_out=ss[:, s:s + 1]), r=["acc", "hg"], w=["hg", "ss"])
                P.add("act", lambda e: e.activation(out=ss[:], in_=ss[:], func=AF.Sqrt, scale=1.0 / D, bias=epsT[:, 0:1]),
                      r=["ss", "eps"], w=["ss"])
                P.add("dve", lambda e: e.reciprocal(out=ss[:], in_=ss[:]), r=["ss"], w=["ss"])
                for s in range(4):
                    P.add("dve", lambda e, s=s: e.scalar_tensor_tensor(
                        out=acc[:, s, :], in0=acc[:, s, :], scalar=ss[:, s:s + 1], in1=gB[:], op0=ALU.mult, op1=ALU.mult),
                        r=["acc", "ss", "gB"], w=["acc"])
                P.dma("sp", out[tg * 512:(tg + 1) * 512, :].rearrange("(s p) d -> p s d", p=128), acc[:], r=["acc"], w=["outd"])
            P.flush()

    load_consts()
    stages = getattr(c, "stages", 99)
    phase_norm_T(x, g_mix, hT, NT, "na")
    if stages >= 2:
        phase_kv()
    if stages >= 3:
        phase_qc()
    if stages >= 4:
        phase_attn()
    if stages >= 5:
        phase_out()
    if stages >= 6:
        phase_norm_T(X1, g_ffn, H2T, NTO, "nf")
    if stages >= 7:
        phase_peer_prep()
    if stages >= 8:
        phase_peer_pick()
    if stages >= 9:
        phase_peer_main()
    gst.close()
    P.close()
    return nc


def rope_tables(SB):
    half = 64
    inv = (10000.0 ** (-np.arange(0, half, 2, dtype=np.float32) / half)).astype(np.float32)
    t = np.arange(SB)
    row = (t // 64).astype(np.float32)
    col = (t % 64).astype(np.float32)
    ang_r = row[:, None] * inv
    ang_c = col[:, None] * inv
    cosT = np.empty((128, SB), np.float32)
    sinT = np.empty((128, SB), np.float32)
    for blk, ang in ((0, ang_r), (1, ang_c)):
        c = np.cos(ang).T.astype(np.float32)
        s = np.sin(ang).T.astype(np.float32)
        o = blk * 64
        cosT[o:o + 32] = c
        cosT[o + 32:o + 64] = c
        sinT[o:o + 32] = -s
        sinT[o + 32:o + 64] = s
    return cosT, sinT


def make_in_maps(c, inputs, n_cores=8):
    D, SB, OWN = c.D, c.SB, c.OWN
    bf = ml_dtypes.bfloat16
    cosT, sinT = rope_tables(SB)
    perm = np.zeros((128, 128), np.float32)
    for d in range(128):
        perm[(d // 64) * 64 + ((d % 64) + 32) % 64, d] = 1.0
    iota = np.tile(np.arange(128, dtype=np.float32)[None, :], (128, 1))
    f = lambda a: np.ascontiguousarray(a, dtype=np.float32)
    shared = {
        "w_in": f(inputs["w_in"][0]), "w_out": f(inputs["w_out"][0]), "w_query": f(inputs["peer_w_query"][0]),
        "sub_keys": f(inputs["peer_sub_keys"][0].reshape(16, 128, 128)),
        "w_down": f(inputs["peer_w_down"][0]), "w_up": f(inputs["peer_w_up"][0]),
        "g_mix": f(inputs["norm_mix_g"][0][None, :]), "g_ffn": f(inputs["norm_ffn_g"][0][None, :]),
        "g_fin": f(inputs["norm_final_g"][None, :]),
        "qk_g": f(np.stack([inputs["q_norm_g"][0], inputs["k_norm_g"][0]], axis=1)),
        "conv_w": f(inputs["conv_w"][0].T.reshape(c.NCB, 128, 3).transpose(1, 0, 2)),
        "ao_g": f(inputs["attn_out_g"][0].reshape(c.NQH, 128).T),
        "co_g": f(inputs["conv_out_g"][0].reshape(c.NCB, 128).T),
        "ident": np.eye(128, dtype=np.float32).astype(bf), "identf": np.eye(128, dtype=np.float32),
        "perm": perm.astype(bf), "iota": iota,
    }
    maps = []
    for core in range(n_cores):
        b, qd = core // 4, core % 4
        sh = qd * OWN
        m = dict(shared)
        m["x"] = f(np.roll(inputs["x"][b], -sh, axis=0))
        m["cosT"] = f(np.roll(cosT, -sh, axis=1))
        m["sinT"] = f(np.roll(sinT, -sh, axis=1))
        edge = np.ones((128, 2), np.float32)
        if qd == 0:
            edge[:, 0] = 0.0
        if qd == 3:
            edge[:, 1] = 0.0
        m["edge"] = edge
        maps.append(m)
    return maps


def kernel(**inputs):
    inputs = {k: np.asarray(v) for k, v in inputs.items()}
    c = Cfg()
    nc = build(c)
    maps = make_in_maps(c, inputs)
    res = run_bass_kernel_spmd(nc, maps, core_ids=list(range(8)))
    outs = [np.asarray(r["out"], dtype=np.float32) for r in res.results]
    B = inputs["x"].shape[0]
    full = np.stack([np.concatenate(outs[b * 4:(b + 1) * 4], axis=0) for b in range(B)], axis=0)
    return full.astype(np.float32)
```

```python
import numpy as np
from contextlib import ExitStack
import ml_dtypes
import concourse.bass as bass
import concourse.mybir as mybir
from concourse.bass_utils import run_bass_kernel_spmd

F32 = mybir.dt.float32
BF16 = mybir.dt.bfloat16
U32 = mybir.dt.uint32
AF = mybir.ActivationFunctionType
ALU = mybir.AluOpType
AX = mybir.AxisListType
ENGS = ["pe", "act", "dve", "pool", "sp"]
EPS = 1e-6
NEG = -1.0e30


class Prog:
    R = 8

    def __init__(self, nc):
        self.nc = nc
        self.pending = []
        self.gid = 0
        self.sig = {}
        self.eng_of = {}
        self.isdma = {}
        self.last_write = {}
        self.readers = {}
        self.cnt = {e: 0 for e in ENGS}
        self.known = {e: {} for e in ENGS}
        self.last_op = {e: None for e in ENGS}
        self.dma_n = {"sp": 0, "pool": 0, "act": 0}
        self.dma_hist = {"sp": [], "pool": [], "act": []}
        self.stack = ExitStack()
        self.sem = {e: self.stack.enter_context(nc.semaphore("s_" + e)) for e in ENGS}
        self.ring = {q: [self.stack.enter_context(nc.semaphore(f"r_{q}{i}")) for i in range(self.R)]
                     for q in ("sp", "pool", "act")}

    def add(self, eng, fn, r=(), w=(), dma=False):
        w = tuple(w) + tuple(k for k in r if k.startswith("ps") and k not in w)
        self.pending.append(dict(eng=eng, fn=fn, r=tuple(r), w=tuple(w), dma=dma, gid=self.gid, barrier=False))
        self.gid += 1

    def dma(self, q, out, in_, r=(), w=()):
        self.add(q, lambda e, o=out, i=in_: e.dma_start(out=o, in_=i), r, w, dma=True)

    def barrier(self):
        for e in ENGS:
            self.pending.append(dict(eng=e, fn=None, r=(), w=(), dma=False, gid=self.gid, barrier=True))
            self.gid += 1

    def flush(self):
        self.barrier()
        ops = self.pending
        self.pending = []
        needed = set()
        last_op = dict(self.last_op)
        dma_hist = {q: list(v) for q, v in self.dma_hist.items()}
        dma_n = dict(self.dma_n)
        for op in ops:
            g = op["gid"]
            self.eng_of[g] = op["eng"]
            self.isdma[g] = op["dma"]
            deps = set()
            if op["barrier"]:
                for e in ENGS:
                    if last_op[e] is not None:
                        deps.add(last_op[e])
                for q in dma_hist:
                    deps.update(dma_hist[q][-self.R:])
            else:
                for k in op["r"]:
                    if k in self.last_write:
                        deps.add(self.last_write[k])
                for k in op["w"]:
                    if k in self.last_write:
                        deps.add(self.last_write[k])
                    deps.update(self.readers.get(k, ()))
                for k in op["w"]:
                    self.last_write[k] = g
                    self.readers[k] = []
                for k in op["r"]:
                    if k not in op["w"]:
                        lst = self.readers.setdefault(k, [])
                        if not op["dma"]:
                            lst[:] = [x for x in lst if self.isdma[x] or self.eng_of[x] != op["eng"]]
                        lst.append(g)
                if op["dma"]:
                    q = op["eng"]
                    n = dma_n[q]
                    if n >= self.R:
                        deps.add(dma_hist[q][n - self.R])
                    dma_hist[q].append(g)
                    dma_n[q] = n + 1
                else:
                    last_op[op["eng"]] = g
            deps.discard(g)
            op["deps"] = deps
            for d in deps:
                if not self.isdma[d]:
                    needed.add(d)
        for op in ops:
            g = op["gid"]
            if op["barrier"]:
                continue
            e = op["eng"]
            if op["dma"]:
                n = self.dma_n[e]
                self.sig[g] = (self.ring[e][n % self.R], 16 * (n // self.R + 1))
                self.dma_n[e] = n + 1
                self.dma_hist[e].append(g)
                op["inc"] = True
            else:
                self.last_op[e] = g
                if g in needed:
                    self.cnt[e] += 1
                    self.sig[g] = (self.sem[e], self.cnt[e])
                    op["inc"] = True
                else:
                    op["inc"] = False
        for op in ops:
            e = op["eng"]
            waits = {}
            for d in sorted(op["deps"]):
                if (not self.isdma[d]) and self.eng_of[d] == "pe" and e == "pe":
                    continue
                s, v = self.sig[d]
                key = id(s)
                if self.known[e].get(key, 0) >= v:
                    continue
                if key not in waits or waits[key][1] < v:
                    waits[key] = (s, v)
            for key, (s, v) in waits.items():
                self.known[e][key] = v
            op["waits"] = list(waits.values())
        per = {e: [o for o in ops if o["eng"] == e] for e in ENGS}

        def run(eng_obj, lst):
            for op in lst:
                for (s, v) in op["waits"]:
                    eng_obj.wait_ge(s, v)
                if op["fn"] is None:
                    continue
                ins = op["fn"](eng_obj)
                if op["inc"]:
                    s, v = self.sig[op["gid"]]
                    ins.then_inc(s, 16 if op["dma"] else 1)

        with self.nc.Block() as block:
            @block.tensor
            def _(e):
                run(e, per["pe"])

            @block.scalar
            def _(e):
                run(e, per["act"])

            @block.vector
            def _(e):
                run(e, per["dve"])

            @block.gpsimd
            def _(e):
                run(e, per["pool"])

            @block.sync
            def _(e):
                run(e, per["sp"])
        self.last_write.clear()
        self.readers.clear()

    def close(self):
        self.stack.close()


class Ring:
    def __init__(self, st, nc, name, n, shape, dt):
        self.t = [st.enter_context(nc.sbuf_tensor(f"{name}{i}", shape, dt)) for i in range(n)]
        self.name = name
        self.i = 0

    def next(self):
        k = self.i % len(self.t)
        self.i += 1
        return self.t[k], f"{self.name}{k}"


class Cfg:
    def __init__(self, D=4096, SB=8192):
        self.D = D
        self.SB = SB
        self.OWN = SB // 4
        self.NCH = D // 128
        self.ATT = D // 2
        self.NQH = self.ATT // 128
        self.NKV = max(1, self.NQH // 4)
        self.GQ = self.NQH // self.NKV
        self.KVW = self.NKV * 128
        self.CW = D - self.ATT
        self.NCB = self.CW // 128
        self.INC = self.ATT + 2 * self.KVW + 3 * self.CW
        self.NT = SB // 128
        self.NTO = self.OWN // 128
        self.PQ = 2048
        self.NE = 16384


def mm_group(P, out_ap, pairs, r, w):
    def fn(e, pairs=pairs, out_ap=out_ap):
        n = len(pairs)
        ins = None
        for i, (l, rh) in enumerate(pairs):
            ins = e.matmul(out_ap, l, rh, start=(i == 0), stop=(i == n - 1))
        return ins
    P.add("pe", fn, r=r, w=w)


def build(c, debug=False):
    nc = bass.Bass("TRN2", target_bir_lowering=False)
    D, SB, OWN, NCH, ATT, NQH, NKV, GQ, KVW, CW, NCB, INC, NT, NTO, PQ, NE = (
        c.D, c.SB, c.OWN, c.NCH, c.ATT, c.NQH, c.NKV, c.GQ, c.KVW, c.CW, c.NCB, c.INC, c.NT, c.NTO, c.PQ, c.NE)
    NYC = NQH + NCB

    def din(name, shape, dt=F32):
        return nc.dram_tensor(name, shape, dt, kind="ExternalInput").ap()

    def dscr(name, shape, dt):
        return nc.dram_tensor(name, shape, dt, kind=("ExternalOutput" if debug else "Internal")).ap()

    x = din("x", [SB, D])
    w_in = din("w_in", [D, INC])
    w_out = din("w_out", [D, D])
    w_query = din("w_query", [D, PQ])
    sub_keys = din("sub_keys", [16, 128, 128])
    w_down = din("w_down", [NE, D])
    w_up = din("w_up", [NE, D])
    g_mix = din("g_mix", [1, D])
    g_ffn = din("g_ffn", [1, D])
    g_fin = din("g_fin", [1, D])
    qk_g = din("qk_g", [128, 2])
    conv_w = din("conv_w", [128, NCB, 3])
    ao_g = din("ao_g", [128, NQH])
    co_g = din("co_g", [128, NCB])
    cosT = din("cosT", [128, SB])
    sinT = din("sinT", [128, SB])
    ident_d = din("ident", [128, 128], BF16)
    identf_d = din("identf", [128, 128])
    perm_d = din("perm", [128, 128], BF16)
    iota_d = din("iota", [128, 128])
    edge_d = din("edge", [128, 2])
    out = nc.dram_tensor("out", [OWN, D], F32, kind="ExternalOutput").ap()

    hT = dscr("hT", [NT, 128, NCH, 128], BF16)
    KT = dscr("KT", [NKV, 128, SB], BF16)
    Vs = dscr("Vs", [NKV, 128, NT, 128], BF16)
    QT = dscr("QT", [NQH, 128, OWN], BF16)
    YT = dscr("YT", [NYC, 128, OWN], BF16)
    X1 = dscr("X1", [OWN, D], F32)
    H2T = dscr("H2T", [NTO, 128, NCH, 128], BF16)
    QPT = dscr("QPT", [16, 128, OWN], BF16)
    WDT = dscr("WDT", [128, 128, NCH, 128], BF16)
    GS = dscr("GS", [NTO, 128, 128, 128], BF16)
    STA = dscr("STA", [128, 2 * NTO], F32)
    NWT = (ATT + 3 * CW) // 256
    WB = dscr("WB", [NWT, 128, NCH, 256], BF16)

    def wb_col0(i):
        return i * 256 if i < ATT // 256 else (ATT + 2 * KVW) + (i - ATT // 256) * 256

    P = Prog(nc)
    gst = ExitStack()

    def gsb(name, shape, dt):
        return gst.enter_context(nc.sbuf_tensor(name, shape, dt))

    psall = gst.enter_context(nc.psum_tensor("psall", [128, 4096], F32))
    ps = [psall[:, i * 512:(i + 1) * 512] for i in range(8)]
    ident = gsb("identS", [128, 128], BF16)
    identf = gsb("identfS", [128, 128], F32)
    ones = gsb("onesS", [128, 128], BF16)
    onesf = gsb("onesfS", [128, 128], F32)
    epsT = gsb("epsT", [128, 1], F32)
    consts = ["ident", "identf", "ones", "onesf", "eps"]

    def load_consts():
        P.dma("sp", ident[:], ident_d, w=["ident"])
        P.dma("sp", identf[:], identf_d, w=["identf"])
        P.add("dve", lambda e: e.memset(ones[:], 1.0), w=["ones"])
        P.add("dve", lambda e: e.memset(onesf[:], 1.0), w=["onesf"])
        P.add("dve", lambda e: e.memset(epsT[:], EPS), w=["eps"])

    def phase_norm_T(src, gain_d, dst, ntiles, tag, extra=None):
        with ExitStack() as st:
            extra_fn = extra(st) if extra is not None else None
            def sb(name, shape, dt):
                return st.enter_context(nc.sbuf_tensor(tag + name, shape, dt))
            gB = sb("gB", [128, D], F32)
            P.dma("sp", gB[:], gain_d.partition_broadcast(128), w=["gB"])
            xt = Ring(st, nc, tag + "xt", 2, [128, D], F32)
            junk = Ring(st, nc, tag + "junk", 2, [128, D], BF16)
            ss = Ring(st, nc, tag + "ss", 2, [128, 1], F32)
            hb = Ring(st, nc, tag + "hb", 2, [128, D], BF16)
            hTt = Ring(st, nc, tag + "hTt", 2, [128, NCH, 128], BF16)
            bi = 0
            for t in range(ntiles):
                xtt, xk = xt.next()
                jt, jk = junk.next()
                sst, sk = ss.next()
                hbt, hk = hb.next()
                htt, htk = hTt.next()
                P.dma("sp", xtt[:], src[t * 128:(t + 1) * 128, :], w=[xk])
                P.add("dve", lambda e, a=jt, b=xtt, s=sst: e.scalar_tensor_tensor(
                    out=a[:], in0=b[:], scalar=1.0, in1=b[:], op0=ALU.mult, op1=ALU.mult, accum_out=s[:]),
                    r=[xk], w=[jk, sk])
                P.add("act", lambda e, s=sst: e.activation(out=s[:], in_=s[:], func=AF.Sqrt, scale=1.0 / D,
                                                           bias=epsT[:, 0:1]), r=[sk, "eps"], w=[sk])
                P.add("dve", lambda e, s=sst: e.reciprocal(out=s[:], in_=s[:]), r=[sk], w=[sk])
                P.add("dve", lambda e, a=hbt, b=xtt, s=sst: e.scalar_tensor_tensor(
                    out=a[:], in0=b[:], scalar=s[:, 0:1], in1=gB[:], op0=ALU.mult, op1=ALU.mult),
                    r=[xk, sk, "gB"], w=[hk])
                for c0 in range(0, NCH, 8):
                    bk = bi % 4
                    bi += 1
                    pb = ps[bk][:].bitcast(BF16)

                    def tr(e, hbt=hbt, c0=c0, pb=pb):
                        ins = None
                        for cc in range(c0, c0 + 8):
                            ins = e.transpose(out=pb[:, (cc - c0) * 128:(cc - c0 + 1) * 128],
                                              in_=hbt[:, cc * 128:(cc + 1) * 128], identity=ident[:])
                        return ins
                    P.add("pe", tr, r=[hk, "ident"], w=[f"ps{bk}"])
                    P.add("act", lambda e, htt=htt, c0=c0, pb=pb: e.copy(
                        out=htt[:, c0:c0 + 8, :], in_=pb.rearrange("p (c t) -> p c t", c=8)),
                        r=[f"ps{bk}"], w=[htk])
                P.dma("act", dst[t], htt[:], r=[htk], w=[tag + "dst"])
                if extra_fn is not None:
                    extra_fn(t, ntiles)
            P.flush()

    def make_rope(st, tag):
        tmp = dict(
            qg=Ring(st, nc, tag + "qg", 2, [128, 512], BF16),
            sq=Ring(st, nc, tag + "sq", 2, [128, 512], BF16),
            rst=Ring(st, nc, tag + "rst", 2, [128, 512], F32),
            t1=Ring(st, nc, tag + "t1", 2, [128, 512], F32),
            t2=Ring(st, nc, tag + "t2", 2, [128, 512], F32),
        )
        return tmp

    def rope_epilogue(tmp, pq, pqk, gcol, Ct, Ck, St, Sk, perm, outt, outk, b1, b2):
        qg, qgk = tmp["qg"].next()
        sq, sqk = tmp["sq"].next()
        rst, rsk = tmp["rst"].next()
        t1, t1k = tmp["t1"].next()
        t2, t2k = tmp["t2"].next()
        P.add("dve", lambda e: e.tensor_scalar(out=qg[:], in0=pq, scalar1=gcol, scalar2=None, op0=ALU.mult),
              r=[pqk, "qkg"], w=[qgk])
        P.add("act", lambda e: e.activation(out=sq[:], in_=pq, func=AF.Square), r=[pqk], w=[sqk])
        P.add("pe", lambda e: e.matmul(ps[b1][:], ones[:], sq[:], start=True, stop=True), r=["ones", sqk], w=[f"ps{b1}"])
        P.add("pe", lambda e: e.matmul(ps[b2][:], perm[:], qg[:], start=True, stop=True), r=["perm", qgk], w=[f"ps{b2}"])
        P.add("act", lambda e: e.activation(out=rst[:], in_=ps[b1][:], func=AF.Sqrt, scale=1.0 / 128, bias=epsT[:, 0:1]),
              r=[f"ps{b1}", "eps"], w=[rsk])
        P.add("dve", lambda e: e.reciprocal(out=rst[:], in_=rst[:]), r=[rsk], w=[rsk])
        P.add("dve", lambda e: e.tensor_tensor(out=t1[:], in0=qg[:], in1=Ct, op=ALU.mult), r=[qgk, Ck], w=[t1k])
        P.add("dve", lambda e: e.tensor_tensor(out=t2[:], in0=ps[b2][:], in1=St, op=ALU.mult), r=[f"ps{b2}", Sk], w=[t2k])
        P.add("dve", lambda e: e.tensor_tensor(out=t1[:], in0=t1[:], in1=t2[:], op=ALU.add), r=[t1k, t2k], w=[t1k])
        P.add("dve", lambda e: e.tensor_tensor(out=outt, in0=t1[:], in1=rst[:], op=ALU.mult), r=[t1k, rsk], w=[outk])

    def phase_kv():
        with ExitStack() as st:
            def sb(name, shape, dt):
                return st.enter_context(nc.sbuf_tensor("kv" + name, shape, dt))
            Wk = sb("Wk", [128, NCH, KVW], BF16)
            Wv = sb("Wv", [128, NCH, KVW], BF16)
            perm = sb("perm", [128, 128], BF16)
            qkg = sb("qkg", [128, 2], F32)
            P.dma("sp", perm[:], perm_d, w=["perm"])
            P.dma("sp", qkg[:], qk_g, w=["qkg"])
            P.dma("pool", Wk[:], w_in[:, ATT:ATT + KVW].rearrange("(c p) n -> p c n", p=128), w=["Wk"])
            P.dma("pool", Wv[:], w_in[:, ATT + KVW:ATT + 2 * KVW].rearrange("(c p) n -> p c n", p=128), w=["Wv"])
            hg = Ring(st, nc, "kvhg", 2, [128, 4, NCH, 128], BF16)
            Cr = Ring(st, nc, "kvC", 2, [128, 512], F32)
            Sr = Ring(st, nc, "kvS", 2, [128, 512], F32)
            ktr = Ring(st, nc, "kvkt", 2, [128, 512], BF16)
            vtr = Ring(st, nc, "kvvt", 2, [128, 4, KVW], BF16)
            tmp = make_rope(st, "kv")
            cvr = Ring(st, nc, "kvcv", 2, [128, NCH, 256], BF16)
            ngr = SB // 512
            cv_i = 0
            cv_prev = None
            bi = 0
            for tg in range(SB // 512):
                while cv_i < NWT and cv_i * ngr < (tg + 1) * NWT:
                    cvt, cvk = cvr.next()
                    c0_ = wb_col0(cv_i)
                    P.dma("pool", cvt[:], w_in[:, c0_:c0_ + 256].rearrange("(c p) n -> p c n", p=128), w=[cvk])
                    if cv_prev is not None:
                        P.dma("pool", WB[cv_prev[0]], cv_prev[1][:], r=[cv_prev[2]], w=["WBd"])
                    cv_prev = (cv_i, cvt, cvk)
                    cv_i += 1
                hgt, hk = hg.next()
                Ct, Ck = Cr.next()
                St, Sk = Sr.next()
                P.dma("sp", hgt[:], hT[tg * 4:(tg + 1) * 4].rearrange("t p c k -> p t c k"), w=[hk])
                P.dma("sp", Ct[:], cosT[:, tg * 512:(tg + 1) * 512], w=[Ck])
                P.dma("sp", St[:], sinT[:, tg * 512:(tg + 1) * 512], w=[Sk])
                for j in range(NKV):
                    b = bi % 2
                    bi += 1
                    mm_group(P, ps[b][:].rearrange("p (t k) -> p t k", t=4),
                             [(Wk[:, cc, j * 128:(j + 1) * 128], hgt[:, :, cc, :]) for cc in range(NCH)],
                             r=["Wk", hk], w=[f"ps{b}"])
                    kt, kk = ktr.next()
                    rope_epilogue(tmp, ps[b][:], f"ps{b}", qkg[:, 1:2], Ct[:], Ck, St[:], Sk, perm, kt[:], kk, 2 + b, 4 + b)
                    P.dma("act", KT[j, :, tg * 512:(tg + 1) * 512], kt[:], r=[kk], w=["KTd"])
                vt, vk = vtr.next()
                for s in range(4):
                    b = 6 + (s % 2)
                    mm_group(P, ps[b][:, 0:KVW], [(hgt[:, s, cc, :], Wv[:, cc, :]) for cc in range(NCH)],
                             r=["Wv", hk], w=[f"ps{b}"])
                    P.add("act", lambda e, vt=vt, s=s, b=b: e.copy(out=vt[:, s, :], in_=ps[b][:, 0:KVW]),
                          r=[f"ps{b}"], w=[vk])
                for j in range(NKV):
                    P.dma("act", Vs[j, :, tg * 4:(tg + 1) * 4, :], vt[:, :, j * 128:(j + 1) * 128], r=[vk], w=["Vd"])
            if cv_prev is not None:
                P.dma("pool", WB[cv_prev[0]], cv_prev[1][:], r=[cv_prev[2]], w=["WBd"])
            P.flush()

    def phase_qc():
        with ExitStack() as st:
            def sb(name, shape, dt):
                return st.enter_context(nc.sbuf_tensor("qc" + name, shape, dt))
            perm = sb("perm", [128, 128], BF16)
            qkg = sb("qkg", [128, 2], F32)
            cw = sb("cw", [128, NCB, 3], F32)
            cog = sb("cog", [128, NCB], F32)
            edge = sb("edge", [128, 2], F32)
            P.dma("sp", perm[:], perm_d, w=["perm"])
            P.dma("sp", qkg[:], qk_g, w=["qkg"])
            P.dma("sp", cw[:], conv_w, w=["cw"])
            P.dma("sp", cog[:], co_g, w=["cog"])
            P.dma("sp", edge[:], edge_d, w=["edge"])
            hgt = sb("hg", [128, 4, NCH, 128], BF16)
            hL = sb("hL", [128, NCH, 128], BF16)
            hR = sb("hR", [128, NCH, 128], BF16)
            hH = sb("hH", [128, NCH, 2], BF16)
            Ct = sb("C", [128, 512], F32)
            St = sb("S", [128, 512], F32)
            Wr = Ring(st, nc, "qcW", 6, [128, NCH, 256], BF16)
            qtr = Ring(st, nc, "qcqt", 2, [128, 512], BF16)
            tmp = make_rope(st, "qc")
            gcS = Ring(st, nc, "qcgc", 2, [128, 512], F32)
            hS = Ring(st, nc, "qchS", 2, [128, 4], F32)
            ur = Ring(st, nc, "qcu", 2, [128, 514], F32)
            vr = Ring(st, nc, "qcv", 2, [128, 512], F32)
            ycr = Ring(st, nc, "qcyc", 2, [128, 512], F32)
            sqr = Ring(st, nc, "qcsqc", 2, [128, 512], F32)
            ybr = Ring(st, nc, "qcyb", 2, [128, 512], BF16)
            ssc = sb("ssc", [128, 512], F32)
            stc = sb("stc", [128, NTO], F32)
            bi = 0

            def wload(col0):
                Wt, Wk_ = Wr.next()
                wi = col0 // 256 if col0 < ATT else ATT // 256 + (col0 - (ATT + 2 * KVW)) // 256
                P.dma("pool", Wt[:], WB[wi], w=[Wk_])
                return Wt, Wk_

            for tg in range(OWN // 512):
                P.dma("sp", hgt[:], hT[tg * 4:(tg + 1) * 4].rearrange("t p c k -> p t c k"), w=["hg"])
                P.dma("sp", hL[:], hT[(tg * 4 - 1) % NT], w=["hL"])
                P.dma("sp", hR[:], hT[(tg * 4 + 4) % NT], w=["hR"])
                P.dma("sp", Ct[:], cosT[:, tg * 512:(tg + 1) * 512], w=["C"])
                P.dma("sp", St[:], sinT[:, tg * 512:(tg + 1) * 512], w=["S"])
                P.add("act", lambda e: e.copy(out=hH[:, :, 0:1], in_=hL[:, :, 127:128]), r=["hL"], w=["hH"])
                P.add("act", lambda e: e.copy(out=hH[:, :, 1:2], in_=hR[:, :, 0:1]), r=["hR", "hH"], w=["hH"])
                for q4 in range(ATT // 256):
                    Wt, Wk_ = wload(q4 * 256)
                    for hh in range(2):
                        h = q4 * 2 + hh
                        b = bi % 2
                        bi += 1
                        mm_group(P, ps[b][:].rearrange("p (t k) -> p t k", t=4),
                                 [(Wt[:, cc, hh * 128:(hh + 1) * 128], hgt[:, :, cc, :]) for cc in range(NCH)],
                                 r=[Wk_, "hg"], w=[f"ps{b}"])
                        qt, qk_ = qtr.next()
                        rope_epilogue(tmp, ps[b][:], f"ps{b}", qkg[:, 0:1], Ct[:], "C", St[:], "S", perm, qt[:], qk_, 2 + b, 4 + b)
                        P.dma("act", QT[h, :, tg * 512:(tg + 1) * 512], qt[:], r=[qk_], w=["QTd"])
                c0 = ATT + 2 * KVW
                for c4 in range(CW // 256):
                    Wx, Wxk = wload(c0 + c4 * 256)
                    Wb, Wbk = wload(c0 + CW + c4 * 256)
                    Wc, Wck = wload(c0 + 2 * CW + c4 * 256)
                    for hh in range(2):
                        cb = c4 * 2 + hh
                        cols = slice(hh * 128, (hh + 1) * 128)
                        for (bk, Wt, Wk_) in ((0, Wx, Wxk), (1, Wc, Wck), (2, Wb, Wbk)):
                            mm_group(P, ps[bk][:].rearrange("p (t k) -> p t k", t=4),
                                     [(Wt[:, cc, cols], hgt[:, :, cc, :]) for cc in range(NCH)],
                                     r=[Wk_, "hg"], w=[f"ps{bk}"])
                        mm_group(P, ps[3][:, 0:2], [(Wx[:, cc, cols], hH[:, cc, :]) for cc in range(NCH)],
                                 r=[Wxk, "hH"], w=["ps3"])
                        mm_group(P, ps[3][:, 2:4], [(Wc[:, cc, cols], hH[:, cc, :]) for cc in range(NCH)],
                                 r=[Wck, "hH", "ps3"], w=["ps3"])
                        gc, gck = gcS.next()
                        hs, hsk = hS.next()
                        u, uk = ur.next()
                        v, vk = vr.next()
                        yc, yck = ycr.next()
                        sq, sqk = sqr.next()
                        yb, ybk = ybr.next()
                        P.add("act", lambda e, gc=gc: e.copy(out=gc[:], in_=ps[1][:]), r=["ps1"], w=[gck])
                        P.add("act", lambda e, hs=hs: e.copy(out=hs[:], in_=ps[3][:, 0:4]), r=["ps3"], w=[hsk])
                        P.add("dve", lambda e, u=u, gc=gc: e.tensor_tensor(out=u[:, 1:513], in0=ps[0][:], in1=gc[:], op=ALU.mult),
                              r=["ps0", gck], w=[uk])
                        if tg == 0:
                            P.add("dve", lambda e, u=u, hs=hs: e.scalar_tensor_tensor(
                                out=u[:, 0:1], in0=hs[:, 0:1], scalar=edge[:, 0:1], in1=hs[:, 2:3], op0=ALU.mult, op1=ALU.mult),
                                r=[hsk, "edge", uk], w=[uk])
                        else:
                            P.add("dve", lambda e, u=u, hs=hs: e.tensor_tensor(
                                out=u[:, 0:1], in0=hs[:, 0:1], in1=hs[:, 2:3], op=ALU.mult), r=[hsk, uk], w=[uk])
                        if tg == OWN // 512 - 1:
                            P.add("dve", lambda e, u=u, hs=hs: e.scalar_tensor_tensor(
                                out=u[:, 513:514], in0=hs[:, 1:2], scalar=edge[:, 1:2], in1=hs[:, 3:4], op0=ALU.mult, op1=ALU.mult),
                                r=[hsk, "edge", uk], w=[uk])
                        else:
                            P.add("dve", lambda e, u=u, hs=hs: e.tensor_tensor(
                                out=u[:, 513:514], in0=hs[:, 1:2], in1=hs[:, 3:4], op=ALU.mult), r=[hsk, uk], w=[uk])
                        P.add("dve", lambda e, u=u, v=v, cb=cb: e.tensor_scalar(
                            out=v[:], in0=u[:, 0:512], scalar1=cw[:, cb, 0:1], scalar2=None, op0=ALU.mult),
                            r=[uk, "cw"], w=[vk])
                        P.add("dve", lambda e, u=u, v=v, cb=cb: e.scalar_tensor_tensor(
                            out=v[:], in0=u[:, 1:513], scalar=cw[:, cb, 1:2], in1=v[:], op0=ALU.mult, op1=ALU.add),
                            r=[uk, "cw", vk], w=[vk])
                        P.add("dve", lambda e, u=u, v=v, cb=cb: e.scalar_tensor_tensor(
                            out=v[:], in0=u[:, 2:514], scalar=cw[:, cb, 2:3], in1=v[:], op0=ALU.mult, op1=ALU.add),
                            r=[uk, "cw", vk], w=[vk])
                        P.add("dve", lambda e, v=v, yc=yc: e.tensor_tensor(out=yc[:], in0=ps[2][:], in1=v[:], op=ALU.mult),
                              r=["ps2", vk], w=[yck])
                        P.add("act", lambda e, sq=sq, yc=yc: e.activation(out=sq[:], in_=yc[:], func=AF.Square), r=[yck], w=[sqk])
                        if cb == 0:
                            P.add("pool", lambda e, sq=sq: e.tensor_copy(out=ssc[:], in_=sq[:]), r=[sqk], w=["ssc"])
                        else:
                            P.add("pool", lambda e, sq=sq: e.tensor_tensor(out=ssc[:], in0=ssc[:], in1=sq[:], op=ALU.add),
                                  r=[sqk, "ssc"], w=["ssc"])
                        P.add("dve", lambda e, yb=yb, yc=yc, cb=cb: e.tensor_scalar(
                            out=yb[:], in0=yc[:], scalar1=cog[:, cb:cb + 1], scalar2=None, op0=ALU.mult),
                            r=[yck, "cog"], w=[ybk])
                        P.dma("act", YT[NQH + cb, :, tg * 512:(tg + 1) * 512], yb[:], r=[ybk], w=["YTd"])
                for s in range(4):
                    P.add("pe", lambda e, s=s: e.matmul(ps[4][:, s:s + 1], ssc[:, s * 128:(s + 1) * 128], onesf[:, 0:1],
                                                        start=True, stop=True), r=["ssc", "onesf"], w=["ps4"])
                P.add("act", lambda e, tg=tg: e.copy(out=stc[:, tg * 4:(tg + 1) * 4], in_=ps[4][:, 0:4]), r=["ps4"], w=["stc"])
            P.dma("sp", STA[:, NTO:2 * NTO], stc[:], r=["stc"], w=["STAd"])
            P.flush()

    def phase_attn():
        with ExitStack() as st:
            def sb(name, shape, dt):
                return st.enter_context(nc.sbuf_tensor("at" + name, shape, dt))
            NKB = SB // 128
            aog = sb("aog", [128, NQH], F32)
            P.dma("sp", aog[:], ao_g, w=["aog"])
            Kr = Ring(st, nc, "atK", 2, [128, SB], BF16)
            Vr = Ring(st, nc, "atV", 2, [128, NKB, 128], BF16)
            Qr = Ring(st, nc, "atQ", 2, [128, 512], BF16)
            Pr = Ring(st, nc, "atP", 4, [128, 1024], BF16)
            owr = Ring(st, nc, "atow", 2, [128, 512], F32)
            d0r = Ring(st, nc, "atd0", 2, [128, 1024], F32)
            d1r = Ring(st, nc, "atd1", 2, [128, 1024], F32)
            rdr = Ring(st, nc, "atrd", 2, [128, 512], F32)
            orr = Ring(st, nc, "ato", 2, [128, 512], F32)
            sqr = Ring(st, nc, "atsq", 2, [128, 512], F32)
            ybr = Ring(st, nc, "atyb", 2, [128, 512], BF16)
            ssa = sb("ssa", [128, OWN], F32)
            sta = sb("sta", [128, NTO], F32)
            scale = 128.0 ** -0.5
            NKP = NKB // 2
            items = [(j, tg, g) for j in range(NKV) for tg in range(OWN // 512) for g in range(GQ)]
            kv = {}

            def load_kv(j):
                Kt, Kk = Kr.next()
                Vt, Vk = Vr.next()
                P.dma("sp", Kt[:], KT[j], w=[Kk])
                P.dma("sp", Vt[:], Vs[j], w=[Vk])
                kv[j] = (Kt, Kk, Vt, Vk)

            qs = {}

            def load_q(i):
                j, tg, g = items[i]
                Qt, Qk = Qr.next()
                P.dma("sp", Qt[:], QT[j * GQ + g, :, tg * 512:(tg + 1) * 512], w=[Qk])
                qs[i] = (Qt, Qk)

            def s_mm(i, kp):
                j = items[i][0]
                Kt, Kk = kv[j][0], kv[j][1]
                Qt, Qk = qs[i]
                b0 = 2 * ((i * NKP + kp) % 3)

                def fn(e, kp=kp, b0=b0, Kt=Kt, Qt=Qt):
                    ins = None
                    for u in range(2):
                        kb = kp * 2 + u
                        ins = e.matmul(ps[b0 + u][:], Kt[:, kb * 128:(kb + 1) * 128], Qt[:], start=True, stop=True)
                    return ins
                P.add("pe", fn, r=[Kk, Qk], w=[f"ps{b0}", f"ps{b0 + 1}"])

            load_kv(0)
            load_q(0)
            s_mm(0, 0)
            s_mm(0, 1)
            for i, (j, tg, g) in enumerate(items):
                h = j * GQ + g
                Kt, Kk, Vt, Vk = kv[j]
                if i + 1 < len(items):
                    if items[i + 1][0] != j:
                        load_kv(items[i + 1][0])
                    load_q(i + 1)
                bo = 6
                bd = 7
                d0, d0k = d0r.next()
                d1, d1k = d1r.next()
                pe_den_started = False
                inited = set()
                for kp in range(NKP):
                    if kp + 2 < NKP:
                        s_mm(i, kp + 2)
                    elif i + 1 < len(items):
                        s_mm(i + 1, kp + 2 - NKP)
                    b0 = 2 * ((i * NKP + kp) % 3)
                    pt, pk = Pr.next()
                    P.add("act", lambda e, pt=pt, b0=b0: e.activation(
                        out=pt[:], in_=psall[:, b0 * 512:(b0 + 2) * 512], func=AF.Exp, scale=scale),
                        r=[f"ps{b0}", f"ps{b0 + 1}"], w=[pk])

                    def pv(e, pt=pt, kp=kp, bo=bo, Vt=Vt):
                        ins = None
                        for u in range(2):
                            kb = kp * 2 + u
                            ins = e.matmul(ps[bo][:], Vt[:, kb, :], pt[:, u * 512:(u + 1) * 512],
                                           start=(kb == 0), stop=(kb == NKB - 1))
                        return ins
                    P.add("pe", pv, r=[Vk, pk], w=[f"ps{bo}"])
                    who = ("dve", "pool", "pe", "dve", "pool", "dve", "pool", "pe")[kp % 8]
                    if who == "pe":
                        def dn(e, pt=pt, bd=bd, first=(not pe_den_started)):
                            ins = None
                            for u in range(2):
                                ins = e.matmul(ps[bd][:], ones[:], pt[:, u * 512:(u + 1) * 512],
                                               start=(first and u == 0), stop=False)
                            return ins
                        P.add("pe", dn, r=["ones", pk], w=[f"ps{bd}"])
                        pe_den_started = True
                    else:
                        eng, dd, ddk = ("dve", d0, d0k) if who == "dve" else ("pool", d1, d1k)
                        if ddk not in inited:
                            inited.add(ddk)
                            P.add(eng, lambda e, dd=dd, pt=pt: e.tensor_copy(out=dd[:], in_=pt[:]), r=[pk], w=[ddk])
                        else:
                            P.add(eng, lambda e, dd=dd, pt=pt: e.tensor_tensor(out=dd[:], in0=dd[:], in1=pt[:], op=ALU.add),
                                  r=[pk, ddk], w=[ddk])

                def dfin(e, d0=d0, d1=d1, bd=bd, st0=(not pe_den_started)):
                    ins = None
                    srcs = [d0[:, 0:512], d0[:, 512:1024], d1[:, 0:512], d1[:, 512:1024]]
                    for i_, sr in enumerate(srcs):
                        ins = e.matmul(ps[bd][:], onesf[:], sr, start=(st0 and i_ == 0), stop=(i_ == 3))
                    return ins
                ow, owk = owr.next()
                P.add("act", lambda e, ow=ow, bo=bo: e.copy(out=ow[:], in_=ps[bo][:]), r=[f"ps{bo}"], w=[owk])
                P.add("pe", dfin, r=["onesf", d0k, d1k], w=[f"ps{bd}"])
                rd, rdk = rdr.next()
                o, ok_ = orr.next()
                sq, sqk = sqr.next()
                yb, ybk = ybr.next()
                P.add("dve", lambda e, rd=rd, bd=bd: e.reciprocal(out=rd[:], in_=ps[bd][:]), r=[f"ps{bd}"], w=[rdk])
                P.add("dve", lambda e, o=o, rd=rd, ow=ow: e.tensor_tensor(out=o[:], in0=ow[:], in1=rd[:], op=ALU.mult),
                      r=[owk, rdk], w=[ok_])
                P.add("act", lambda e, sq=sq, o=o: e.activation(out=sq[:], in_=o[:], func=AF.Square), r=[ok_], w=[sqk])
                sl = slice(tg * 512, (tg + 1) * 512)
                if h == 0:
                    P.add("pool", lambda e, sq=sq, sl=sl: e.tensor_copy(out=ssa[:, sl], in_=sq[:]), r=[sqk], w=[f"ssa{tg}"])
                else:
                    P.add("pool", lambda e, sq=sq, sl=sl: e.tensor_tensor(out=ssa[:, sl], in0=ssa[:, sl], in1=sq[:], op=ALU.add),
                          r=[sqk, f"ssa{tg}"], w=[f"ssa{tg}"])
                P.add("dve", lambda e, yb=yb, o=o, h=h: e.tensor_scalar(
                    out=yb[:], in0=o[:], scalar1=aog[:, h:h + 1], scalar2=None, op0=ALU.mult), r=[ok_, "aog"], w=[ybk])
                P.dma("act", YT[h, :, sl], yb[:], r=[ybk], w=["YTd"])
            for t in range(NTO):
                P.add("pe", lambda e, t=t: e.matmul(ps[7][:, t:t + 1], ssa[:, t * 128:(t + 1) * 128], onesf[:, 0:1],
                                                    start=True, stop=True), r=[f"ssa{t // 4}", "onesf"], w=["ps7"])
            P.add("act", lambda e: e.copy(out=sta[:], in_=ps[7][:, 0:NTO]), r=["ps7"], w=["sta"])
            P.dma("sp", STA[:, 0:NTO], sta[:], r=["sta"], w=["STAd"])
            P.flush()

    def phase_out():
        with ExitStack() as st:
            def sb(name, shape, dt):
                return st.enter_context(nc.sbuf_tensor("op" + name, shape, dt))
            rs = sb("rs", [128, 2 * NTO], F32)
            P.dma("sp", rs[:], STA, w=["rs"])
            P.add("act", lambda e: e.activation(out=rs[:], in_=rs[:], func=AF.Sqrt, scale=1.0 / ATT, bias=epsT[:, 0:1]),
                  r=["rs", "eps"], w=["rs"])
            P.add("dve", lambda e: e.reciprocal(out=rs[:], in_=rs[:]), r=["rs"], w=["rs"])
            ygr = Ring(st, nc, "opyg", 2, [128, NYC, 512], BF16)
            Wr = Ring(st, nc, "opW", 2, [128, NYC, 512], BF16)
            xr = Ring(st, nc, "opx", 4, [128, 512], F32)
            tr_ = Ring(st, nc, "opt", 2, [128, 512], F32)
            orr = Ring(st, nc, "opo", 3, [128, 512], F32)
            bi = 0
            for dc in range(D // 512):
                Wt, Wk_ = Wr.next()
                P.dma("pool", Wt[:], w_out[:, dc * 512:(dc + 1) * 512].rearrange("(c p) n -> p c n", p=128), w=[Wk_])
                for tg in range(OWN // 512):
                    yg, ygk = ygr.next()
                    P.dma("sp", yg[:], YT[:, :, tg * 512:(tg + 1) * 512].rearrange("c p t -> p c t"), w=[ygk])
                    for s in range(4):
                        ba = (bi % 4) * 2
                        bi += 1
                        t_ = tg * 4 + s
                        xp, xk = xr.next()
                        P.dma("sp", xp[:], x[t_ * 128:(t_ + 1) * 128, dc * 512:(dc + 1) * 512], w=[xk])
                        mm_group(P, ps[ba][:], [(yg[:, cc, s * 128:(s + 1) * 128], Wt[:, cc, :]) for cc in range(NQH)],
                                 r=[ygk, Wk_], w=[f"ps{ba}"])
                        mm_group(P, ps[ba + 1][:], [(yg[:, cc, s * 128:(s + 1) * 128], Wt[:, cc, :]) for cc in range(NQH, NYC)],
                                 r=[ygk, Wk_], w=[f"ps{ba + 1}"])
                        tt, tk = tr_.next()
                        ot, ok_ = orr.next()
                        P.add("dve", lambda e, tt=tt, ba=ba, t_=t_, xp=xp: e.scalar_tensor_tensor(
                            out=tt[:], in0=ps[ba][:], scalar=rs[:, t_:t_ + 1], in1=xp[:], op0=ALU.mult, op1=ALU.add),
                            r=[f"ps{ba}", "rs", xk], w=[tk])
                        P.add("dve", lambda e, tt=tt, ot=ot, ba=ba, t_=t_: e.scalar_tensor_tensor(
                            out=ot[:], in0=ps[ba + 1][:], scalar=rs[:, NTO + t_:NTO + t_ + 1], in1=tt[:], op0=ALU.mult, op1=ALU.add),
                            r=[f"ps{ba + 1}", "rs", tk], w=[ok_])
                        P.dma("act", X1[t_ * 128:(t_ + 1) * 128, dc * 512:(dc + 1) * 512], ot[:], r=[ok_], w=["X1d"])
            P.flush()

    def wdt_prep(st):
        HC = max(NCH // 2, 1)
        HD = HC * 128
        NPJ = NCH // HC
        Wr = Ring(st, nc, "ppW", 2, [128, HD], BF16)
        Wfr = Ring(st, nc, "ppWf", 2, [128, HD], F32)
        Tr = Ring(st, nc, "ppT", 2, [128, HC, 128], BF16)
        wdv = w_down.rearrange("(i j) d -> j i d", j=128)
        state = {"bi": 0, "n": 0}

        def one(n, eng):
            j, hp = n // NPJ, n % NPJ
            Wt, Wk_ = Wr.next()
            Tt, Tk = Tr.next()
            Wf, Wfk = Wfr.next()
            P.dma("sp", Wf[:], wdv[j][:, hp * HD:(hp + 1) * HD], w=[Wfk])
            if eng == "pool":
                P.add("pool", lambda e, Wt=Wt, Wf=Wf: e.tensor_copy(out=Wt[:], in_=Wf[:]), r=[Wfk], w=[Wk_])
            else:
                P.add("act", lambda e, Wt=Wt, Wf=Wf: e.copy(out=Wt[:], in_=Wf[:]), r=[Wfk], w=[Wk_])
            for c0 in range(0, HC, 8):
                nb = min(8, HC - c0)
                bk = (4, 7)[state["bi"] % 2]
                state["bi"] += 1
                pb = ps[bk][:].bitcast(BF16)

                def tr(e, Wt=Wt, c0=c0, pb=pb, nb=nb):
                    ins = None
                    for cc in range(c0, c0 + nb):
                        ins = e.transpose(out=pb[:, (cc - c0) * 128:(cc - c0 + 1) * 128],
                                          in_=Wt[:, cc * 128:(cc + 1) * 128], identity=ident[:])
                    return ins
                P.add("pe", tr, r=[Wk_, "ident"], w=[f"ps{bk}"])
                P.add("act", lambda e, Tt=Tt, c0=c0, pb=pb, nb=nb: e.copy(
                    out=Tt[:, c0:c0 + nb, :], in_=pb[:, 0:nb * 128].rearrange("p (c t) -> p c t", c=nb)), r=[f"ps{bk}"], w=[Tk])
            P.dma("act", WDT[j][:, hp * HC:(hp + 1) * HC, :], Tt[:], r=[Tk], w=["WDTd"])

        total = 128 * NPJ

        def step(k, nsteps, eng):
            hi_ = (total * (k + 1)) // nsteps
            while state["n"] < hi_:
                one(state["n"], eng)
                state["n"] += 1
        return step

    def phase_peer_prep():
        with ExitStack() as st:
            def sb(name, shape, dt):
                return st.enter_context(nc.sbuf_tensor("pq" + name, shape, dt))
            hgr = Ring(st, nc, "pqhg", 2, [128, 4, NCH, 128], BF16)
            Wr = Ring(st, nc, "pqW", 2, [128, NCH, 512], BF16)
            qr = Ring(st, nc, "pqq", 3, [128, 512], BF16)
            bi = 0
            for q4 in range(PQ // 512):
                Wt, Wk_ = Wr.next()
                P.dma("pool", Wt[:], w_query[:, q4 * 512:(q4 + 1) * 512].rearrange("(c p) n -> p c n", p=128), w=[Wk_])
                for tg in range(OWN // 512):
                    hgt, hgk = hgr.next()
                    P.dma("sp", hgt[:], H2T[tg * 4:(tg + 1) * 4].rearrange("t p c k -> p t c k"), w=[hgk])
                    for hh in range(4):
                        b = bi % 4
                        bi += 1
                        mm_group(P, ps[b][:].rearrange("p (t k) -> p t k", t=4),
                                 [(Wt[:, cc, hh * 128:(hh + 1) * 128], hgt[:, :, cc, :]) for cc in range(NCH)],
                                 r=[Wk_, hgk], w=[f"ps{b}"])
                        qt, qk_ = qr.next()
                        P.add("act", lambda e, qt=qt, b=b: e.copy(out=qt[:], in_=ps[b][:]), r=[f"ps{b}"], w=[qk_])
                        P.dma("act", QPT[q4 * 4 + hh, :, tg * 512:(tg + 1) * 512], qt[:], r=[qk_], w=["QPTd"])
            P.flush()

    def phase_peer_pick():
        with ExitStack() as st:
            def sb(name, shape, dt):
                return st.enter_context(nc.sbuf_tensor("pk" + name, shape, dt))
            kn = sb("kn", [128, 16, 128], BF16)
            kT = sb("kT", [128, 16, 128], BF16)
            iot = sb("iot", [128, 128], F32)
            P.dma("pool", kn[:], sub_keys.rearrange("g n k -> n g k"), w=["kn"])
            P.dma("sp", iot[:], iota_d, w=["iot"])
            for g0 in range(0, 16, 8):
                pb = ps[0][:].bitcast(BF16)

                def tr(e, g0=g0, pb=pb):
                    ins = None
                    for g in range(g0, g0 + 8):
                        ins = e.transpose(out=pb[:, (g - g0) * 128:(g - g0 + 1) * 128], in_=kn[:, g, :], identity=ident[:])
                    return ins
                P.add("pe", tr, r=["kn", "ident"], w=["ps0"])
                P.add("act", lambda e, g0=g0, pb=pb: e.copy(out=kT[:, g0:g0 + 8, :], in_=pb.rearrange("p (c t) -> p c t", c=8)),
                      r=["ps0"], w=["kT"])
            qpr = Ring(st, nc, "pkqp", 2, [128, 16, 128], BF16)
            S = sb("S", [128, 16, 128], F32)
            S2 = sb("S2", [128, 16, 128], F32)
            V = sb("V", [128, 16, 16], F32)
            IX = sb("IX", [128, 16, 16], U32)
            IXf = sb("IXf", [128, 16, 16], F32)
            cand = sb("cand", [128, 8, 256], F32)
            cand2 = sb("cand2", [128, 8, 256], F32)
            Bv = sb("Bv", [128, 8, 16], F32)
            PX = sb("PX", [128, 8, 16], U32)
            R1u = sb("R1u", [128, 8, 16], U32)
            R2u = sb("R2u", [128, 8, 16], U32)
            R1 = sb("R1", [128, 8, 16], F32)
            R2 = sb("R2", [128, 8, 16], F32)
            E = sb("E", [128, 8, 16], F32)
            Z = sb("Z", [128, 8], F32)
            eq = sb("eq", [128, 8, 16, 16], F32)
            PI = sb("PI", [128, 128], F32)
            PJ = sb("PJ", [128, 128], F32)
            PG = sb("PG", [128, 128], F32)
            PT = sb("PT", [128, 3, 128], F32)
            OI = sb("OI", [128, 128, 128], BF16)
            OJ = sb("OJ", [128, 128, 128], BF16)
            Gr = Ring(st, nc, "pkG", 1, [128, 128, 128], BF16)
            prep_step = wdt_prep(st)
            prep_k = 0

            def dv(fn, r, w):
                P.add("dve", fn, r=r, w=w)

            def scores(tt):
                qp, qpk = qpr.next()
                P.dma("sp", qp[:], QPT[:, :, tt * 128:(tt + 1) * 128].rearrange("g p t -> p g t"), w=[qpk])
                for b in range(4):
                    for g4 in range(4):
                        g = b * 4 + g4
                        P.add("pe", lambda e, g=g, g4=g4, b=b, qp=qp: e.matmul(
                            ps[b][:, g4 * 128:(g4 + 1) * 128], qp[:, g, :], kT[:, g, :], start=True, stop=True),
                            r=[qpk, "kT"] + ([f"ps{b}"] if g4 else []), w=[f"ps{b}"])
                    P.add("act", lambda e, b=b: e.copy(out=S[:, b * 4:(b + 1) * 4, :],
                                                       in_=ps[b][:].rearrange("p (g n) -> p g n", g=4)),
                          r=[f"ps{b}"], w=[f"S{g_}" for g_ in range(b * 4, b * 4 + 4)])

            def topk():
                G16 = range(16)
                for g in G16:
                    dv(lambda e, g=g: e.max(out=V[:, g, 0:8], in_=S[:, g, :]), [f"S{g}"], [f"Va{g}"])
                for g in G16:
                    dv(lambda e, g=g: e.max_index(out=IX[:, g, 0:8], in_max=V[:, g, 0:8], in_values=S[:, g, :]),
                       [f"S{g}", f"Va{g}"], [f"IXa{g}"])
                for g in G16:
                    dv(lambda e, g=g: e.match_replace(out=S2[:, g, :], in_to_replace=V[:, g, 0:8], in_values=S[:, g, :],
                                                      imm_value=NEG), [f"S{g}", f"Va{g}"], [f"S2{g}"])
                for g in G16:
                    dv(lambda e, g=g: e.max(out=V[:, g, 8:16], in_=S2[:, g, :]), [f"S2{g}"], [f"Vb{g}"])
                for g in G16:
                    dv(lambda e, g=g: e.max_index(out=IX[:, g, 8:16], in_max=V[:, g, 8:16], in_values=S2[:, g, :]),
                       [f"S2{g}", f"Vb{g}"], [f"IXb{g}"])
                allV = [f"Va{g}" for g in G16] + [f"Vb{g}" for g in G16]
                allIX = [f"IXa{g}" for g in G16] + [f"IXb{g}" for g in G16]
                dv(lambda e: e.tensor_copy(out=IXf[:], in_=IX[:]), allIX, ["IXf"])
                Vv = V[:].rearrange("p (h two) k -> p h two k", two=2)
                H8 = range(8)
                dv(lambda e, Vv=Vv: e.tensor_tensor(
                    out=cand[:].rearrange("p h (a b) -> p h a b", a=16),
                    in0=Vv[:, :, 0, :].unsqueeze(3).to_broadcast([128, 8, 16, 16]),
                    in1=Vv[:, :, 1, :].unsqueeze(2).to_broadcast([128, 8, 16, 16]), op=ALU.add), allV, [f"cand{h}" for h in H8])
                for h in H8:
                    dv(lambda e, h=h: e.max(out=Bv[:, h, 0:8], in_=cand[:, h, :]), [f"cand{h}"], [f"Ba{h}"])
                for h in H8:
                    dv(lambda e, h=h: e.max_index(out=PX[:, h, 0:8], in_max=Bv[:, h, 0:8], in_values=cand[:, h, :]),
                       [f"cand{h}", f"Ba{h}"], [f"PXa{h}"])
                for h in H8:
                    dv(lambda e, h=h: e.match_replace(out=cand2[:, h, :], in_to_replace=Bv[:, h, 0:8], in_values=cand[:, h, :],
                                                      imm_value=NEG), [f"cand{h}", f"Ba{h}"], [f"cand2{h}"])
                for h in H8:
                    dv(lambda e, h=h: e.max(out=Bv[:, h, 8:16], in_=cand2[:, h, :]), [f"cand2{h}"], [f"Bb{h}"])
                for h in H8:
                    dv(lambda e, h=h: e.max_index(out=PX[:, h, 8:16], in_max=Bv[:, h, 8:16], in_values=cand2[:, h, :]),
                       [f"cand2{h}", f"Bb{h}"], [f"PXb{h}"])
                allB = [f"Ba{h}" for h in H8] + [f"Bb{h}" for h in H8]
                allPX = [f"PXa{h}" for h in H8] + [f"PXb{h}" for h in H8]

            def topk_b():
                H8 = range(8)
                allB = [f"Ba{h}" for h in H8] + [f"Bb{h}" for h in H8]
                allPX = [f"PXa{h}" for h in H8] + [f"PXb{h}" for h in H8]
                dv(lambda e: e.tensor_tensor(out=E[:], in0=Bv[:], in1=Bv[:, :, 0:1].to_broadcast([128, 8, 16]), op=ALU.subtract),
                   allB, ["E"])
                P.add("act", lambda e: e.activation(out=E[:], in_=E[:], func=AF.Exp), r=["E"], w=["E"])
                dv(lambda e: e.tensor_reduce(out=Z[:], in_=E[:], axis=AX.X, op=ALU.add), ["E"], ["Z"])
                dv(lambda e: e.reciprocal(out=Z[:], in_=Z[:]), ["Z"], ["Z"])
                dv(lambda e: e.tensor_tensor(out=PG[:].rearrange("p (h k) -> p h k", h=8), in0=E[:],
                                             in1=Z[:].unsqueeze(2).to_broadcast([128, 8, 16]), op=ALU.mult), ["E", "Z"], ["PG"])
                dv(lambda e: e.tensor_single_scalar(out=R1u[:], in_=PX[:], scalar=4, op=ALU.logical_shift_right), allPX, ["R1u"])
                dv(lambda e: e.tensor_single_scalar(out=R2u[:], in_=PX[:], scalar=15, op=ALU.bitwise_and), allPX, ["R2u"])
                dv(lambda e: e.tensor_copy(out=R1[:], in_=R1u[:]), ["R1u"], ["R1"])
                dv(lambda e: e.tensor_copy(out=R2[:], in_=R2u[:]), ["R2u"], ["R2"])
                IXv = IXf[:].rearrange("p (h two) k -> p h two k", two=2)
                for (Rr, Rk, two, Pout, Pk) in ((R1, "R1", 0, PI, "PI"), (R2, "R2", 1, PJ, "PJ")):
                    dv(lambda e, Rr=Rr: e.tensor_tensor(
                        out=eq[:], in0=Rr[:].unsqueeze(3).to_broadcast([128, 8, 16, 16]),
                        in1=iot[:, 0:16].unsqueeze(1).unsqueeze(1).to_broadcast([128, 8, 16, 16]), op=ALU.is_equal),
                        [Rk, "iot"], ["eq"])
                    dv(lambda e, two=two, IXv=IXv: e.tensor_tensor(
                        out=eq[:], in0=eq[:], in1=IXv[:, :, two, :].unsqueeze(2).to_broadcast([128, 8, 16, 16]), op=ALU.mult),
                        ["eq", "IXf"], ["eq"])
                    dv(lambda e, Pout=Pout: e.tensor_reduce(out=Pout[:].rearrange("p (h k) -> p h k", h=8), in_=eq[:],
                                                            axis=AX.X, op=ALU.add), ["eq"], [Pk])

            scores(0)
            topk()
            topk_b()
            for tt in range(NTO):
                for n_, (Pin, Pk) in enumerate(((PI, "PI"), (PJ, "PJ"), (PG, "PG"))):
                    P.add("pe", lambda e, n_=n_, Pin=Pin: e.transpose(out=ps[4][:, n_ * 128:(n_ + 1) * 128], in_=Pin[:], identity=identf[:]),
                          r=[Pk, "identf"] + (["ps4"] if n_ else []), w=["ps4"])
                P.add("act", lambda e: e.copy(out=PT[:], in_=ps[4][:, 0:384].rearrange("p (a t) -> p a t", a=3)), r=["ps4"], w=["PT"])
                dv(lambda e: e.tensor_tensor(
                    out=OJ[:], in0=PT[:, 1, :].unsqueeze(2).to_broadcast([128, 128, 128]),
                    in1=iot[:].unsqueeze(1).to_broadcast([128, 128, 128]), op=ALU.is_equal), ["PT", "iot"], ["OJ"])
                P.add("pool", lambda e: e.tensor_tensor(
                    out=OJ[:], in0=OJ[:], in1=PT[:, 2, :].unsqueeze(2).to_broadcast([128, 128, 128]), op=ALU.mult),
                    r=["PT", "OJ"], w=["OJ"])
                dv(lambda e: e.tensor_tensor(
                    out=OI[:], in0=PT[:, 0, :].unsqueeze(2).to_broadcast([128, 128, 128]),
                    in1=iot[:].unsqueeze(1).to_broadcast([128, 128, 128]), op=ALU.is_equal), ["PT", "iot"], ["OI"])
                if tt + 1 < NTO:
                    scores(tt + 1)
                    topk()
                Gt, Gk = Gr.next()
                for sl_ in range(8):
                    prep_step(tt * 16 + sl_, NTO * 16, "act")
                for t4 in range(32):
                    b = 5 + (t4 % 2)

                    def gm(e, t4=t4, b=b):
                        ins = None
                        for u in range(4):
                            t_ = t4 * 4 + u
                            ins = e.matmul(ps[b][:, u * 128:(u + 1) * 128], OI[:, t_, :], OJ[:, t_, :], start=True, stop=True)
                        return ins
                    P.add("pe", gm, r=["OI", "OJ"], w=[f"ps{b}"])
                    outv = Gt[:, :, t4 * 4:(t4 + 1) * 4].rearrange("p j t -> p t j")
                    inv = ps[b][:].rearrange("p (t j) -> p t j", t=4)
                    P.add("act", lambda e, outv=outv, inv=inv: e.copy(out=outv, in_=inv), r=[f"ps{b}"], w=[Gk])
                    if t4 % 4 == 3:
                        prep_step(tt * 16 + 8 + t4 // 4, NTO * 16, "pool")
                    if t4 == 19 and tt + 1 < NTO:
                        topk_b()
                P.dma("act", GS[tt], Gt[:], r=[Gk], w=["GSd"])
            P.flush()

    def phase_peer_main():
        with ExitStack() as st:
            def sb(name, shape, dt):
                return st.enter_context(nc.sbuf_tensor("pm" + name, shape, dt))
            gB = sb("gB", [128, D], F32)
            P.dma("sp", gB[:], g_fin.partition_broadcast(128), w=["gB"])
            hgt = sb("hg", [128, 4, NCH, 128], BF16)
            acc = sb("acc", [128, 4, D], F32)
            WDr = Ring(st, nc, "pmWD", 4, [128, NCH, 128], BF16)
            WUr = Ring(st, nc, "pmWU", 2, [128, 2, D], BF16)
            Gr = Ring(st, nc, "pmG", 4, [128, 4, 2, 128], BF16)
            Dgr = Ring(st, nc, "pmDg", 2, [128, 2, 512], BF16)
            Ar = Ring(st, nc, "pmA", 3, [128, 2, 512], BF16)
            ss = sb("ss", [128, 4], F32)
            wuv = w_up.rearrange("(i j) d -> i j d", j=128)
            NJG = 64
            NUP = 4 * (D // 512)
            ui = 0
            for tg in range(OWN // 512):
                P.dma("sp", hgt[:], H2T[tg * 4:(tg + 1) * 4].rearrange("t p c k -> p t c k"), w=["hg"])
                P.dma("sp", acc[:], X1[tg * 512:(tg + 1) * 512, :].rearrange("(s p) d -> p s d", p=128), w=["acc"])
                stt = {}

                def load_down(jg, tg=tg, stt=stt):
                    d = stt.setdefault(jg, {})
                    d["WD"] = []
                    for jj in range(2):
                        WD, WDk = WDr.next()
                        P.dma("sp", WD[:], WDT[jg * 2 + jj], w=[WDk])
                        d["WD"].append((WD, WDk))
                    Gt, Gk = Gr.next()
                    P.dma("sp", Gt[:], GS[tg * 4:(tg + 1) * 4, :, jg * 2:(jg + 1) * 2, :].rearrange("s i j t -> i s j t"), w=[Gk])
                    d["G"] = (Gt, Gk)

                def load_up(jg, stt=stt):
                    WU, WUk = WUr.next()
                    P.dma("pool", WU[:], wuv[:, jg * 2:(jg + 1) * 2, :], w=[WUk])
                    stt.setdefault(jg, {})["WU"] = (WU, WUk)

                def down_pieces(jg, stt=stt):
                    d = stt[jg]
                    Dg, Dgk = Dgr.next()
                    A, Ak = Ar.next()
                    d["A"] = (A, Ak)
                    Gt, Gk = d["G"]
                    pieces = []
                    for k in range(NUP):
                        def piece(k=k, d=d, Dg=Dg, Dgk=Dgk, A=A, Ak=Ak, Gt=Gt, Gk=Gk):
                            ms = []
                            for m in (2 * k, 2 * k + 1):
                                jj, cc = m // NCH, m % NCH
                                ms.append((jj, cc))
                            jj = ms[0][0]
                            WD, WDk = d["WD"][jj]

                            def fn(e, ms=ms, WD=WD, jj=jj):
                                ins = None
                                for (_, cc) in ms:
                                    ins = e.matmul(ps[jj][:].rearrange("p (t k) -> p t k", t=4), WD[:, cc, :], hgt[:, :, cc, :],
                                                   start=(cc == 0), stop=(cc == NCH - 1))
                                return ins
                            P.add("pe", fn, r=[WDk, "hg"], w=[f"ps{jj}"])
                            if ms[-1][1] == NCH - 1:
                                P.add("act", lambda e, Dg=Dg, jj=jj: e.activation(out=Dg[:, jj, :], in_=ps[jj][:], func=AF.Gelu),
                                      r=[f"ps{jj}"], w=[Dgk])
                                if jj == 1:
                                    P.add("pool", lambda e, A=A, Dg=Dg, Gt=Gt: e.tensor_tensor(
                                        out=A[:].rearrange("p j (s t) -> p j s t", s=4),
                                        in0=Dg[:].rearrange("p j (s t) -> p j s t", s=4),
                                        in1=Gt[:].rearrange("p s j t -> p j s t"), op=ALU.mult), r=[Dgk, Gk], w=[Ak])
                        pieces.append(piece)
                    return pieces

                load_down(0)
                load_down(1)
                load_up(0)
                for pc in down_pieces(0):
                    pc()
                load_down(2)
                for pc in down_pieces(1):
                    pc()
                for jg in range(NJG):
                    if jg + 3 < NJG:
                        load_down(jg + 3)
                    if jg + 1 < NJG:
                        load_up(jg + 1)
                    pieces = down_pieces(jg + 2) if jg + 2 < NJG else []
                    A, Ak = stt[jg]["A"]
                    WU, WUk = stt[jg]["WU"]
                    k = 0
                    for s_ in range(4):
                        for dc in range(D // 512):
                            b = 2 + (ui % 6)
                            ui += 1
                            mm_group(P, ps[b][:], [(A[:, jj, s_ * 128:(s_ + 1) * 128], WU[:, jj, dc * 512:(dc + 1) * 512]) for jj in range(2)],
                                     r=[Ak, WUk], w=[f"ps{b}"])
                            P.add("dve", lambda e, s_=s_, dc=dc, b=b: e.tensor_tensor(
                                out=acc[:, s_, dc * 512:(dc + 1) * 512], in0=acc[:, s_, dc * 512:(dc + 1) * 512], in1=ps[b][:], op=ALU.add),
                                r=[f"ps{b}", "acc"], w=["acc"])
                            if pieces:
                                pieces[k]()
                            k += 1
                    del stt[jg]
                for s in range(4):
                    P.add("dve", lambda e, s=s: e.scalar_tensor_tensor(
                        out=hgt[:].rearrange("p a c k -> p (a c k)")[:, 0:D], in0=acc[:, s, :], scalar=1.0, in1=acc[:, s, :],
                        op0=ALU.mult, op1=ALU.mult, accum_out=ss[:, s:s + 1]), r=["acc", "hg"], w=["hg", "ss"])
                P.add("act", lambda e: e.activation(out=ss[:], in_=ss[:], func=AF.Sqrt, scale=1.0 / D, bias=epsT[:, 0:1]),
                      r=["ss", "eps"], w=["ss"])
                P.add("dve", lambda e: e.reciprocal(out=ss[:], in_=ss[:]), r=["ss"], w=["ss"])
                for s in range(4):
                    P.add("dve", lambda e, s=s: e.scalar_tensor_tensor(
                        out=acc[:, s, :], in0=acc[:, s, :], scalar=ss[:, s:s + 1], in1=gB[:], op0=ALU.mult, op1=ALU.mult),
                        r=["acc", "ss", "gB"], w=["acc"])
                P.dma("sp", out[tg * 512:(tg + 1) * 512, :].rearrange("(s p) d -> p s d", p=128), acc[:], r=["acc"], w=["outd"])
            P.flush()

    load_consts()
    stages = getattr(c, "stages", 99)
    phase_norm_T(x, g_mix, hT, NT, "na")
    if stages >= 2:
        phase_kv()
    if stages >= 3:
        phase_qc()
    if stages >= 4:
        phase_attn()
    if stages >= 5:
        phase_out()
    if stages >= 6:
        phase_norm_T(X1, g_ffn, H2T, NTO, "nf")
    if stages >= 7:
        phase_peer_prep()
    if stages >= 8:
        phase_peer_pick()
    if stages >= 9:
        phase_peer_main()
    gst.close()
    P.close()
    return nc


def rope_tables(SB):
    half = 64
    inv = (10000.0 ** (-np.arange(0, half, 2, dtype=np.float32) / half)).astype(np.float32)
    t = np.arange(SB)
    row = (t // 64).astype(np.float32)
    col = (t % 64).astype(np.float32)
    ang_r = row[:, None] * inv
    ang_c = col[:, None] * inv
    cosT = np.empty((128, SB), np.float32)
    sinT = np.empty((128, SB), np.float32)
    for blk, ang in ((0, ang_r), (1, ang_c)):
        c = np.cos(ang).T.astype(np.float32)
        s = np.sin(ang).T.astype(np.float32)
        o = blk * 64
        cosT[o:o + 32] = c
        cosT[o + 32:o + 64] = c
        sinT[o:o + 32] = -s
        sinT[o + 32:o + 64] = s
    return cosT, sinT


def make_in_maps(c, inputs, n_cores=8):
    D, SB, OWN = c.D, c.SB, c.OWN
    bf = ml_dtypes.bfloat16
    cosT, sinT = rope_tables(SB)
    perm = np.zeros((128, 128), np.float32)
    for d in range(128):
        perm[(d // 64) * 64 + ((d % 64) + 32) % 64, d] = 1.0
    iota = np.tile(np.arange(128, dtype=np.float32)[None, :], (128, 1))
    f = lambda a: np.ascontiguousarray(a, dtype=np.float32)
    shared = {
        "w_in": f(inputs["w_in"][0]), "w_out": f(inputs["w_out"][0]), "w_query": f(inputs["peer_w_query"][0]),
        "sub_keys": f(inputs["peer_sub_keys"][0].reshape(16, 128, 128)),
        "w_down": f(inputs["peer_w_down"][0]), "w_up": f(inputs["peer_w_up"][0]),
        "g_mix": f(inputs["norm_mix_g"][0][None, :]), "g_ffn": f(inputs["norm_ffn_g"][0][None, :]),
        "g_fin": f(inputs["norm_final_g"][None, :]),
        "qk_g": f(np.stack([inputs["q_norm_g"][0], inputs["k_norm_g"][0]], axis=1)),
        "conv_w": f(inputs["conv_w"][0].T.reshape(c.NCB, 128, 3).transpose(1, 0, 2)),
        "ao_g": f(inputs["attn_out_g"][0].reshape(c.NQH, 128).T),
        "co_g": f(inputs["conv_out_g"][0].reshape(c.NCB, 128).T),
        "ident": np.eye(128, dtype=np.float32).astype(bf), "identf": np.eye(128, dtype=np.float32),
        "perm": perm.astype(bf), "iota": iota,
    }
    maps = []
    for core in range(n_cores):
        b, qd = core // 4, core % 4
        sh = qd * OWN
        m = dict(shared)
        m["x"] = f(np.roll(inputs["x"][b], -sh, axis=0))
        m["cosT"] = f(np.roll(cosT, -sh, axis=1))
        m["sinT"] = f(np.roll(sinT, -sh, axis=1))
        edge = np.ones((128, 2), np.float32)
        if qd == 0:
            edge[:, 0] = 0.0
        if qd == 3:
            edge[:, 1] = 0.0
        m["edge"] = edge
        maps.append(m)
    return maps


def kernel(**inputs):
    inputs = {k: np.asarray(v) for k, v in inputs.items()}
    c = Cfg()
    nc = build(c)
    maps = make_in_maps(c, inputs)
    res = run_bass_kernel_spmd(nc, maps, core_ids=list(range(8)))
    outs = [np.asarray(r["out"], dtype=np.float32) for r in res.results]
    B = inputs["x"].shape[0]
    full = np.stack([np.concatenate(outs[b * 4:(b + 1) * 4], axis=0) for b in range(B)], axis=0)
    return full.astype(np.float32)
```

```python
import numpy as np
from contextlib import ExitStack
import ml_dtypes
import concourse.bass as bass
import concourse.mybir as mybir
from concourse.bass_utils import run_bass_kernel_spmd

F32 = mybir.dt.float32
BF16 = mybir.dt.bfloat16
U32 = mybir.dt.uint32
AF = mybir.ActivationFunctionType
ALU = mybir.AluOpType
AX = mybir.AxisListType
ENGS = ["pe", "act", "dve", "pool", "sp"]
EPS = 1e-6
NEG = -1.0e30


class Prog:
    R = 8

    def __init__(self, nc):
        self.nc = nc
        self.pending = []
        self.gid = 0
        self.sig = {}
        self.eng_of = {}
        self.isdma = {}
        self.last_write = {}
        self.readers = {}
        self.cnt = {e: 0 for e in ENGS}
        self.known = {e: {} for e in ENGS}
        self.last_op = {e: None for e in ENGS}
        self.dma_n = {"sp": 0, "pool": 0, "act": 0}
        self.dma_hist = {"sp": [], "pool": [], "act": []}
        self.stack = ExitStack()
        self.sem = {e: self.stack.enter_context(nc.semaphore("s_" + e)) for e in ENGS}
        self.ring = {q: [self.stack.enter_context(nc.semaphore(f"r_{q}{i}")) for i in range(self.R)]
                     for q in ("sp", "pool", "act")}

    def add(self, eng, fn, r=(), w=(), dma=False):
        w = tuple(w) + tuple(k for k in r if k.startswith("ps") and k not in w)
        self.pending.append(dict(eng=eng, fn=fn, r=tuple(r), w=tuple(w), dma=dma, gid=self.gid, barrier=False))
        self.gid += 1

    def dma(self, q, out, in_, r=(), w=()):
        self.add(q, lambda e, o=out, i=in_: e.dma_start(out=o, in_=i), r, w, dma=True)

    def barrier(self):
        for e in ENGS:
            self.pending.append(dict(eng=e, fn=None, r=(), w=(), dma=False, gid=self.gid, barrier=True))
            self.gid += 1

    def flush(self):
        self.barrier()
        ops = self.pending
        self.pending = []
        needed = set()
        last_op = dict(self.last_op)
        dma_hist = {q: list(v) for q, v in self.dma_hist.items()}
        dma_n = dict(self.dma_n)
        for op in ops:
            g = op["gid"]
            self.eng_of[g] = op["eng"]
            self.isdma[g] = op["dma"]
            deps = set()
            if op["barrier"]:
                for e in ENGS:
                    if last_op[e] is not None:
                        deps.add(last_op[e])
                for q in dma_hist:
                    deps.update(dma_hist[q][-self.R:])
            else:
                for k in op["r"]:
                    if k in self.last_write:
                        deps.add(self.last_write[k])
                for k in op["w"]:
                    if k in self.last_write:
                        deps.add(self.last_write[k])
                    deps.update(self.readers.get(k, ()))
                for k in op["w"]:
                    self.last_write[k] = g
                    self.readers[k] = []
                for k in op["r"]:
                    if k not in op["w"]:
                        lst = self.readers.setdefault(k, [])
                        if not op["dma"]:
                            lst[:] = [x for x in lst if self.isdma[x] or self.eng_of[x] != op["eng"]]
                        lst.append(g)
                if op["dma"]:
                    q = op["eng"]
                    n = dma_n[q]
                    if n >= self.R:
                        deps.add(dma_hist[q][n - self.R])
                    dma_hist[q].append(g)
                    dma_n[q] = n + 1
                else:
                    last_op[op["eng"]] = g
            deps.discard(g)
            op["deps"] = deps
            for d in deps:
                if not self.isdma[d]:
                    needed.add(d)
        for op in ops:
            g = op["gid"]
            if op["barrier"]:
                continue
            e = op["eng"]
            if op["dma"]:
                n = self.dma_n[e]
                self.sig[g] = (self.ring[e][n % self.R], 16 * (n // self.R + 1))
                self.dma_n[e] = n + 1
                self.dma_hist[e].append(g)
                op["inc"] = True
            else:
                self.last_op[e] = g
                if g in needed:
                    self.cnt[e] += 1
                    self.sig[g] = (self.sem[e], self.cnt[e])
                    op["inc"] = True
                else:
                    op["inc"] = False
        for op in ops:
            e = op["eng"]
            waits = {}
            for d in sorted(op["deps"]):
                if (not self.isdma[d]) and self.eng_of[d] == "pe" and e == "pe":
                    continue
                s, v = self.sig[d]
                key = id(s)
                if self.known[e].get(key, 0) >= v:
                    continue
                if key not in waits or waits[key][1] < v:
                    waits[key] = (s, v)
            for key, (s, v) in waits.items():
                self.known[e][key] = v
            op["waits"] = list(waits.values())
        per = {e: [o for o in ops if o["eng"] == e] for e in ENGS}

        def run(eng_obj, lst):
            for op in lst:
                for (s, v) in op["waits"]:
                    eng_obj.wait_ge(s, v)
                if op["fn"] is None:
                    continue
                ins = op["fn"](eng_obj)
                if op["inc"]:
                    s, v = self.sig[op["gid"]]
                    ins.then_inc(s, 16 if op["dma"] else 1)

        with self.nc.Block() as block:
            @block.tensor
            def _(e):
                run(e, per["pe"])

            @block.scalar
            def _(e):
                run(e, per["act"])

            @block.vector
            def _(e):
                run(e, per["dve"])

            @block.gpsimd
            def _(e):
                run(e, per["pool"])

            @block.sync
            def _(e):
                run(e, per["sp"])
        self.last_write.clear()
        self.readers.clear()

    def close(self):
        self.stack.close()


class Ring:
    def __init__(self, st, nc, name, n, shape, dt):
        self.t = [st.enter_context(nc.sbuf_tensor(f"{name}{i}", shape, dt)) for i in range(n)]
        self.name = name
        self.i = 0

    def next(self):
        k = self.i % len(self.t)
        self.i += 1
        return self.t[k], f"{self.name}{k}"


class Cfg:
    def __init__(self, D=4096, SB=8192):
        self.D = D
        self.SB = SB
        self.OWN = SB // 4
        self.NCH = D // 128
        self.ATT = D // 2
        self.NQH = self.ATT // 128
        self.NKV = max(1, self.NQH // 4)
        self.GQ = self.NQH // self.NKV
        self.KVW = self.NKV * 128
        self.CW = D - self.ATT
        self.NCB = self.CW // 128
        self.INC = self.ATT + 2 * self.KVW + 3 * self.CW
        self.NT = SB // 128
        self.NTO = self.OWN // 128
        self.PQ = 2048
        self.NE = 16384


def mm_group(P, out_ap, pairs, r, w):
    def fn(e, pairs=pairs, out_ap=out_ap):
        n = len(pairs)
        ins = None
        for i, (l, rh) in enumerate(pairs):
            ins = e.matmul(out_ap, l, rh, start=(i == 0), stop=(i == n - 1))
        return ins
    P.add("pe", fn, r=r, w=w)


def build(c, debug=False):
    nc = bass.Bass("TRN2", target_bir_lowering=False)
    D, SB, OWN, NCH, ATT, NQH, NKV, GQ, KVW, CW, NCB, INC, NT, NTO, PQ, NE = (
        c.D, c.SB, c.OWN, c.NCH, c.ATT, c.NQH, c.NKV, c.GQ, c.KVW, c.CW, c.NCB, c.INC, c.NT, c.NTO, c.PQ, c.NE)
    NYC = NQH + NCB

    def din(name, shape, dt=F32):
        return nc.dram_tensor(name, shape, dt, kind="ExternalInput").ap()

    def dscr(name, shape, dt):
        return nc.dram_tensor(name, shape, dt, kind=("ExternalOutput" if debug else "Internal")).ap()

    x = din("x", [SB, D])
    w_in = din("w_in", [D, INC])
    w_out = din("w_out", [D, D])
    w_query = din("w_query", [D, PQ])
    sub_keys = din("sub_keys", [16, 128, 128])
    w_down = din("w_down", [NE, D])
    w_up = din("w_up", [NE, D])
    g_mix = din("g_mix", [1, D])
    g_ffn = din("g_ffn", [1, D])
    g_fin = din("g_fin", [1, D])
    qk_g = din("qk_g", [128, 2])
    conv_w = din("conv_w", [128, NCB, 3])
    ao_g = din("ao_g", [128, NQH])
    co_g = din("co_g", [128, NCB])
    cosT = din("cosT", [128, SB])
    sinT = din("sinT", [128, SB])
    ident_d = din("ident", [128, 128], BF16)
    identf_d = din("identf", [128, 128])
    perm_d = din("perm", [128, 128], BF16)
    iota_d = din("iota", [128, 128])
    edge_d = din("edge", [128, 2])
    out = nc.dram_tensor("out", [OWN, D], F32, kind="ExternalOutput").ap()

    hT = dscr("hT", [NT, 128, NCH, 128], BF16)
    KT = dscr("KT", [NKV, 128, SB], BF16)
    Vs = dscr("Vs", [NKV, 128, NT, 128], BF16)
    QT = dscr("QT", [NQH, 128, OWN], BF16)
    YT = dscr("YT", [NYC, 128, OWN], BF16)
    X1 = dscr("X1", [OWN, D], F32)
    H2T = dscr("H2T", [NTO, 128, NCH, 128], BF16)
    QPT = dscr("QPT", [16, 128, OWN], BF16)
    WDT = dscr("WDT", [128, 128, NCH, 128], BF16)
    GS = dscr("GS", [NTO, 128, 128, 128], BF16)
    STA = dscr("STA", [128, 2 * NTO], F32)
    NWT = (ATT + 3 * CW) // 256
    WB = dscr("WB", [NWT, 128, NCH, 256], BF16)

    def wb_col0(i):
        return i * 256 if i < ATT // 256 else (ATT + 2 * KVW) + (i - ATT // 256) * 256

    P = Prog(nc)
    gst = ExitStack()

    def gsb(name, shape, dt):
        return gst.enter_context(nc.sbuf_tensor(name, shape, dt))

    psall = gst.enter_context(nc.psum_tensor("psall", [128, 4096], F32))
    ps = [psall[:, i * 512:(i + 1) * 512] for i in range(8)]
    ident = gsb("identS", [128, 128], BF16)
    identf = gsb("identfS", [128, 128], F32)
    ones = gsb("onesS", [128, 128], BF16)
    onesf = gsb("onesfS", [128, 128], F32)
    epsT = gsb("epsT", [128, 1], F32)
    consts = ["ident", "identf", "ones", "onesf", "eps"]

    def load_consts():
        P.dma("sp", ident[:], ident_d, w=["ident"])
        P.dma("sp", identf[:], identf_d, w=["identf"])
        P.add("dve", lambda e: e.memset(ones[:], 1.0), w=["ones"])
        P.add("dve", lambda e: e.memset(onesf[:], 1.0), w=["onesf"])
        P.add("dve", lambda e: e.memset(epsT[:], EPS), w=["eps"])

    def phase_norm_T(src, gain_d, dst, ntiles, tag, extra=None):
        with ExitStack() as st:
            extra_fn = extra(st) if extra is not None else None
            def sb(name, shape, dt):
                return st.enter_context(nc.sbuf_tensor(tag + name, shape, dt))
            gB = sb("gB", [128, D], F32)
            P.dma("sp", gB[:], gain_d.partition_broadcast(128), w=["gB"])
            xt = Ring(st, nc, tag + "xt", 2, [128, D], F32)
            junk = Ring(st, nc, tag + "junk", 2, [128, D], BF16)
            ss = Ring(st, nc, tag + "ss", 2, [128, 1], F32)
            hb = Ring(st, nc, tag + "hb", 2, [128, D], BF16)
            hTt = Ring(st, nc, tag + "hTt", 2, [128, NCH, 128], BF16)
            bi = 0
            for t in range(ntiles):
                xtt, xk = xt.next()
                jt, jk = junk.next()
                sst, sk = ss.next()
                hbt, hk = hb.next()
                htt, htk = hTt.next()
                P.dma("sp", xtt[:], src[t * 128:(t + 1) * 128, :], w=[xk])
                P.add("dve", lambda e, a=jt, b=xtt, s=sst: e.scalar_tensor_tensor(
                    out=a[:], in0=b[:], scalar=1.0, in1=b[:], op0=ALU.mult, op1=ALU.mult, accum_out=s[:]),
                    r=[xk], w=[jk, sk])
                P.add("act", lambda e, s=sst: e.activation(out=s[:], in_=s[:], func=AF.Sqrt, scale=1.0 / D,
                                                           bias=epsT[:, 0:1]), r=[sk, "eps"], w=[sk])
                P.add("dve", lambda e, s=sst: e.reciprocal(out=s[:], in_=s[:]), r=[sk], w=[sk])
                P.add("dve", lambda e, a=hbt, b=xtt, s=sst: e.scalar_tensor_tensor(
                    out=a[:], in0=b[:], scalar=s[:, 0:1], in1=gB[:], op0=ALU.mult, op1=ALU.mult),
                    r=[xk, sk, "gB"], w=[hk])
                for c0 in range(0, NCH, 8):
                    bk = bi % 4
                    bi += 1
                    pb = ps[bk][:].bitcast(BF16)

                    def tr(e, hbt=hbt, c0=c0, pb=pb):
                        ins = None
                        for cc in range(c0, c0 + 8):
                            ins = e.transpose(out=pb[:, (cc - c0) * 128:(cc - c0 + 1) * 128],
                                              in_=hbt[:, cc * 128:(cc + 1) * 128], identity=ident[:])
                        return ins
                    P.add("pe", tr, r=[hk, "ident"], w=[f"ps{bk}"])
                    P.add("act", lambda e, htt=htt, c0=c0, pb=pb: e.copy(
                        out=htt[:, c0:c0 + 8, :], in_=pb.rearrange("p (c t) -> p c t", c=8)),
                        r=[f"ps{bk}"], w=[htk])
                P.dma("act", dst[t], htt[:], r=[htk], w=[tag + "dst"])
                if extra_fn is not None:
                    extra_fn(t, ntiles)
            P.flush()

    def make_rope(st, tag):
        tmp = dict(
            qg=Ring(st, nc, tag + "qg", 2, [128, 512], BF16),
            sq=Ring(st, nc, tag + "sq", 2, [128, 512], BF16),
            rst=Ring(st, nc, tag + "rst", 2, [128, 512], F32),
            t1=Ring(st, nc, tag + "t1", 2, [128, 512], F32),
            t2=Ring(st, nc, tag + "t2", 2, [128, 512], F32),
        )
        return tmp

    def rope_epilogue(tmp, pq, pqk, gcol, Ct, Ck, St, Sk, perm, outt, outk, b1, b2):
        qg, qgk = tmp["qg"].next()
        sq, sqk = tmp["sq"].next()
        rst, rsk = tmp["rst"].next()
        t1, t1k = tmp["t1"].next()
        t2, t2k = tmp["t2"].next()
        P.add("dve", lambda e: e.tensor_scalar(out=qg[:], in0=pq, scalar1=gcol, scalar2=None, op0=ALU.mult),
              r=[pqk, "qkg"], w=[qgk])
        P.add("act", lambda e: e.activation(out=sq[:], in_=pq, func=AF.Square), r=[pqk], w=[sqk])
        P.add("pe", lambda e: e.matmul(ps[b1][:], ones[:], sq[:], start=True, stop=True), r=["ones", sqk], w=[f"ps{b1}"])
        P.add("pe", lambda e: e.matmul(ps[b2][:], perm[:], qg[:], start=True, stop=True), r=["perm", qgk], w=[f"ps{b2}"])
        P.add("act", lambda e: e.activation(out=rst[:], in_=ps[b1][:], func=AF.Sqrt, scale=1.0 / 128, bias=epsT[:, 0:1]),
              r=[f"ps{b1}", "eps"], w=[rsk])
        P.add("dve", lambda e: e.reciprocal(out=rst[:], in_=rst[:]), r=[rsk], w=[rsk])
        P.add("dve", lambda e: e.tensor_tensor(out=t1[:], in0=qg[:], in1=Ct, op=ALU.mult), r=[qgk, Ck], w=[t1k])
        P.add("dve", lambda e: e.tensor_tensor(out=t2[:], in0=ps[b2][:], in1=St, op=ALU.mult), r=[f"ps{b2}", Sk], w=[t2k])
        P.add("dve", lambda e: e.tensor_tensor(out=t1[:], in0=t1[:], in1=t2[:], op=ALU.add), r=[t1k, t2k], w=[t1k])
        P.add("dve", lambda e: e.tensor_tensor(out=outt, in0=t1[:], in1=rst[:], op=ALU.mult), r=[t1k, rsk], w=[outk])

    def phase_kv():
        with ExitStack() as st:
            def sb(name, shape, dt):
                return st.enter_context(nc.sbuf_tensor("kv" + name, shape, dt))
            Wk = sb("Wk", [128, NCH, KVW], BF16)
            Wv = sb("Wv", [128, NCH, KVW], BF16)
            perm = sb("perm", [128, 128], BF16)
            qkg = sb("qkg", [128, 2], F32)
            P.dma("sp", perm[:], perm_d, w=["perm"])
            P.dma("sp", qkg[:], qk_g, w=["qkg"])
            P.dma("pool", Wk[:], w_in[:, ATT:ATT + KVW].rearrange("(c p) n -> p c n", p=128), w=["Wk"])
            P.dma("pool", Wv[:], w_in[:, ATT + KVW:ATT + 2 * KVW].rearrange("(c p) n -> p c n", p=128), w=["Wv"])
            hg = Ring(st, nc, "kvhg", 2, [128, 4, NCH, 128], BF16)
            Cr = Ring(st, nc, "kvC", 2, [128, 512], F32)
            Sr = Ring(st, nc, "kvS", 2, [128, 512], F32)
            ktr = Ring(st, nc, "kvkt", 2, [128, 512], BF16)
            vtr = Ring(st, nc, "kvvt", 2, [128, 4, KVW], BF16)
            tmp = make_rope(st, "kv")
            cvr = Ring(st, nc, "kvcv", 2, [128, NCH, 256], BF16)
            ngr = SB // 512
            cv_i = 0
            cv_prev = None
            bi = 0
            for tg in range(SB // 512):
                while cv_i < NWT and cv_i * ngr < (tg + 1) * NWT:
                    cvt, cvk = cvr.next()
                    c0_ = wb_col0(cv_i)
                    P.dma("pool", cvt[:], w_in[:, c0_:c0_ + 256].rearrange("(c p) n -> p c n", p=128), w=[cvk])
                    if cv_prev is not None:
                        P.dma("pool", WB[cv_prev[0]], cv_prev[1][:], r=[cv_prev[2]], w=["WBd"])
                    cv_prev = (cv_i, cvt, cvk)
                    cv_i += 1
                hgt, hk = hg.next()
                Ct, Ck = Cr.next()
                St, Sk = Sr.next()
                P.dma("sp", hgt[:], hT[tg * 4:(tg + 1) * 4].rearrange("t p c k -> p t c k"), w=[hk])
                P.dma("sp", Ct[:], cosT[:, tg * 512:(tg + 1) * 512], w=[Ck])
                P.dma("sp", St[:], sinT[:, tg * 512:(tg + 1) * 512], w=[Sk])
                for j in range(NKV):
                    b = bi % 2
                    bi += 1
                    mm_group(P, ps[b][:].rearrange("p (t k) -> p t k", t=4),
                             [(Wk[:, cc, j * 128:(j + 1) * 128], hgt[:, :, cc, :]) for cc in range(NCH)],
                             r=["Wk", hk], w=[f"ps{b}"])
                    kt, kk = ktr.next()
                    rope_epilogue(tmp, ps[b][:], f"ps{b}", qkg[:, 1:2], Ct[:], Ck, St[:], Sk, perm, kt[:], kk, 2 + b, 4 + b)
                    P.dma("act", KT[j, :, tg * 512:(tg + 1) * 512], kt[:], r=[kk], w=["KTd"])
                vt, vk = vtr.next()
                for s in range(4):
                    b = 6 + (s % 2)
                    mm_group(P, ps[b][:, 0:KVW], [(hgt[:, s, cc, :], Wv[:, cc, :]) for cc in range(NCH)],
                             r=["Wv", hk], w=[f"ps{b}"])
                    P.add("act", lambda e, vt=vt, s=s, b=b: e.copy(out=vt[:, s, :], in_=ps[b][:, 0:KVW]),
                          r=[f"ps{b}"], w=[vk])
                for j in range(NKV):
                    P.dma("act", Vs[j, :, tg * 4:(tg + 1) * 4, :], vt[:, :, j * 128:(j + 1) * 128], r=[vk], w=["Vd"])
            if cv_prev is not None:
                P.dma("pool", WB[cv_prev[0]], cv_prev[1][:], r=[cv_prev[2]], w=["WBd"])
            P.flush()

    def phase_qc():
        with ExitStack() as st:
            def sb(name, shape, dt):
                return st.enter_context(nc.sbuf_tensor("qc" + name, shape, dt))
            perm = sb("perm", [128, 128], BF16)
            qkg = sb("qkg", [128, 2], F32)
            cw = sb("cw", [128, NCB, 3], F32)
            cog = sb("cog", [128, NCB], F32)
            edge = sb("edge", [128, 2], F32)
            P.dma("sp", perm[:], perm_d, w=["perm"])
            P.dma("sp", qkg[:], qk_g, w=["qkg"])
            P.dma("sp", cw[:], conv_w, w=["cw"])
            P.dma("sp", cog[:], co_g, w=["cog"])
            P.dma("sp", edge[:], edge_d, w=["edge"])
            hgt = sb("hg", [128, 4, NCH, 128], BF16)
            hL = sb("hL", [128, NCH, 128], BF16)
            hR = sb("hR", [128, NCH, 128], BF16)
            hH = sb("hH", [128, NCH, 2], BF16)
            Ct = sb("C", [128, 512], F32)
            St = sb("S", [128, 512], F32)
            Wr = Ring(st, nc, "qcW", 6, [128, NCH, 256], BF16)
            qtr = Ring(st, nc, "qcqt", 2, [128, 512], BF16)
            tmp = make_rope(st, "qc")
            gcS = Ring(st, nc, "qcgc", 2, [128, 512], F32)
            hS = Ring(st, nc, "qchS", 2, [128, 4], F32)
            ur = Ring(st, nc, "qcu", 2, [128, 514], F32)
            vr = Ring(st, nc, "qcv", 2, [128, 512], F32)
            ycr = Ring(st, nc, "qcyc", 2, [128, 512], F32)
            sqr = Ring(st, nc, "qcsqc", 2, [128, 512], F32)
            ybr = Ring(st, nc, "qcyb", 2, [128, 512], BF16)
            ssc = sb("ssc", [128, 512], F32)
            stc = sb("stc", [128, NTO], F32)
            bi = 0

            def wload(col0):
                Wt, Wk_ = Wr.next()
                wi = col0 // 256 if col0 < ATT else ATT // 256 + (col0 - (ATT + 2 * KVW)) // 256
                P.dma("pool", Wt[:], WB[wi], w=[Wk_])
                return Wt, Wk_

            for tg in range(OWN // 512):
                P.dma("sp", hgt[:], hT[tg * 4:(tg + 1) * 4].rearrange("t p c k -> p t c k"), w=["hg"])
                P.dma("sp", hL[:], hT[(tg * 4 - 1) % NT], w=["hL"])
                P.dma("sp", hR[:], hT[(tg * 4 + 4) % NT], w=["hR"])
                P.dma("sp", Ct[:], cosT[:, tg * 512:(tg + 1) * 512], w=["C"])
                P.dma("sp", St[:], sinT[:, tg * 512:(tg + 1) * 512], w=["S"])
                P.add("act", lambda e: e.copy(out=hH[:, :, 0:1], in_=hL[:, :, 127:128]), r=["hL"], w=["hH"])
                P.add("act", lambda e: e.copy(out=hH[:, :, 1:2], in_=hR[:, :, 0:1]), r=["hR", "hH"], w=["hH"])
                for q4 in range(ATT // 256):
                    Wt, Wk_ = wload(q4 * 256)
                    for hh in range(2):
                        h = q4 * 2 + hh
                        b = bi % 2
                        bi += 1
                        mm_group(P, ps[b][:].rearrange("p (t k) -> p t k", t=4),
                                 [(Wt[:, cc, hh * 128:(hh + 1) * 128], hgt[:, :, cc, :]) for cc in range(NCH)],
                                 r=[Wk_, "hg"], w=[f"ps{b}"])
                        qt, qk_ = qtr.next()
                        rope_epilogue(tmp, ps[b][:], f"ps{b}", qkg[:, 0:1], Ct[:], "C", St[:], "S", perm, qt[:], qk_, 2 + b, 4 + b)
                        P.dma("act", QT[h, :, tg * 512:(tg + 1) * 512], qt[:], r=[qk_], w=["QTd"])
                c0 = ATT + 2 * KVW
                for c4 in range(CW // 256):
                    Wx, Wxk = wload(c0 + c4 * 256)
                    Wb, Wbk = wload(c0 + CW + c4 * 256)
                    Wc, Wck = wload(c0 + 2 * CW + c4 * 256)
                    for hh in range(2):
                        cb = c4 * 2 + hh
                        cols = slice(hh * 128, (hh + 1) * 128)
                        for (bk, Wt, Wk_) in ((0, Wx, Wxk), (1, Wc, Wck), (2, Wb, Wbk)):
                            mm_group(P, ps[bk][:].rearrange("p (t k) -> p t k", t=4),
                                     [(Wt[:, cc, cols], hgt[:, :, cc, :]) for cc in range(NCH)],
                                     r=[Wk_, "hg"], w=[f"ps{bk}"])
                        mm_group(P, ps[3][:, 0:2], [(Wx[:, cc, cols], hH[:, cc, :]) for cc in range(NCH)],
                                 r=[Wxk, "hH"], w=["ps3"])
                        mm_group(P, ps[3][:, 2:4], [(Wc[:, cc, cols], hH[:, cc, :]) for cc in range(NCH)],
                                 r=[Wck, "hH", "ps3"], w=["ps3"])
                        gc, gck = gcS.next()
                        hs, hsk = hS.next()
                        u, uk = ur.next()
                        v, vk = vr.next()
                        yc, yck = ycr.next()
                        sq, sqk = sqr.next()
                        yb, ybk = ybr.next()
                        P.add("act", lambda e, gc=gc: e.copy(out=gc[:], in_=ps[1][:]), r=["ps1"], w=[gck])
                        P.add("act", lambda e, hs=hs: e.copy(out=hs[:], in_=ps[3][:, 0:4]), r=["ps3"], w=[hsk])
                        P.add("dve", lambda e, u=u, gc=gc: e.tensor_tensor(out=u[:, 1:513], in0=ps[0][:], in1=gc[:], op=ALU.mult),
                              r=["ps0", gck], w=[uk])
                        if tg == 0:
                            P.add("dve", lambda e, u=u, hs=hs: e.scalar_tensor_tensor(
                                out=u[:, 0:1], in0=hs[:, 0:1], scalar=edge[:, 0:1], in1=hs[:, 2:3], op0=ALU.mult, op1=ALU.mult),
                                r=[hsk, "edge", uk], w=[uk])
                        else:
                            P.add("dve", lambda e, u=u, hs=hs: e.tensor_tensor(
                                out=u[:, 0:1], in0=hs[:, 0:1], in1=hs[:, 2:3], op=ALU.mult), r=[hsk, uk], w=[uk])
                        if tg == OWN // 512 - 1:
                            P.add("dve", lambda e, u=u, hs=hs: e.scalar_tensor_tensor(
                                out=u[:, 513:514], in0=hs[:, 1:2], scalar=edge[:, 1:2], in1=hs[:, 3:4], op0=ALU.mult, op1=ALU.mult),
                                r=[hsk, "edge", uk], w=[uk])
                        else:
                            P.add("dve", lambda e, u=u, hs=hs: e.tensor_tensor(
                                out=u[:, 513:514], in0=hs[:, 1:2], in1=hs[:, 3:4], op=ALU.mult), r=[hsk, uk], w=[uk])
                        P.add("dve", lambda e, u=u, v=v, cb=cb: e.tensor_scalar(
                            out=v[:], in0=u[:, 0:512], scalar1=cw[:, cb, 0:1], scalar2=None, op0=ALU.mult),
                            r=[uk, "cw"], w=[vk])
                        P.add("dve", lambda e, u=u, v=v, cb=cb: e.scalar_tensor_tensor(
                            out=v[:], in0=u[:, 1:513], scalar=cw[:, cb, 1:2], in1=v[:], op0=ALU.mult, op1=ALU.add),
                            r=[uk, "cw", vk], w=[vk])
                        P.add("dve", lambda e, u=u, v=v, cb=cb: e.scalar_tensor_tensor(
                            out=v[:], in0=u[:, 2:514], scalar=cw[:, cb, 2:3], in1=v[:], op0=ALU.mult, op1=ALU.add),
                            r=[uk, "cw", vk], w=[vk])
                        P.add("dve", lambda e, v=v, yc=yc: e.tensor_tensor(out=yc[:], in0=ps[2][:], in1=v[:], op=ALU.mult),
                              r=["ps2", vk], w=[yck])
                        P.add("act", lambda e, sq=sq, yc=yc: e.activation(out=sq[:], in_=yc[:], func=AF.Square), r=[yck], w=[sqk])
                        if cb == 0:
                            P.add("pool", lambda e, sq=sq: e.tensor_copy(out=ssc[:], in_=sq[:]), r=[sqk], w=["ssc"])
                        else:
                            P.add("pool", lambda e, sq=sq: e.tensor_tensor(out=ssc[:], in0=ssc[:], in1=sq[:], op=ALU.add),
                                  r=[sqk, "ssc"], w=["ssc"])
                        P.add("dve", lambda e, yb=yb, yc=yc, cb=cb: e.tensor_scalar(
                            out=yb[:], in0=yc[:], scalar1=cog[:, cb:cb + 1], scalar2=None, op0=ALU.mult),
                            r=[yck, "cog"], w=[ybk])
                        P.dma("act", YT[NQH + cb, :, tg * 512:(tg + 1) * 512], yb[:], r=[ybk], w=["YTd"])
                for s in range(4):
                    P.add("pe", lambda e, s=s: e.matmul(ps[4][:, s:s + 1], ssc[:, s * 128:(s + 1) * 128], onesf[:, 0:1],
                                                        start=True, stop=True), r=["ssc", "onesf"], w=["ps4"])
                P.add("act", lambda e, tg=tg: e.copy(out=stc[:, tg * 4:(tg + 1) * 4], in_=ps[4][:, 0:4]), r=["ps4"], w=["stc"])
            P.dma("sp", STA[:, NTO:2 * NTO], stc[:], r=["stc"], w=["STAd"])
            P.flush()

    def phase_attn():
        with ExitStack() as st:
            def sb(name, shape, dt):
                return st.enter_context(nc.sbuf_tensor("at" + name, shape, dt))
            NKB = SB // 128
            aog = sb("aog", [128, NQH], F32)
            P.dma("sp", aog[:], ao_g, w=["aog"])
            Kr = Ring(st, nc, "atK", 2, [128, SB], BF16)
            Vr = Ring(st, nc, "atV", 2, [128, NKB, 128], BF16)
            Qr = Ring(st, nc, "atQ", 2, [128, 512], BF16)
            Pr = Ring(st, nc, "atP", 6, [128, 1024], BF16)
            owr = Ring(st, nc, "atow", 2, [128, 512], F32)
            d0r = Ring(st, nc, "atd0", 2, [128, 1024], F32)
            d1r = Ring(st, nc, "atd1", 2, [128, 1024], F32)
            rdr = Ring(st, nc, "atrd", 2, [128, 512], F32)
            orr = Ring(st, nc, "ato", 2, [128, 512], F32)
            sqr = Ring(st, nc, "atsq", 2, [128, 512], F32)
            ybr = Ring(st, nc, "atyb", 2, [128, 512], BF16)
            ssa = sb("ssa", [128, OWN], F32)
            sta = sb("sta", [128, NTO], F32)
            scale = 128.0 ** -0.5
            NKP = NKB // 2
            items = [(j, tg, g) for j in range(NKV) for tg in range(OWN // 512) for g in range(GQ)]
            kv = {}

            def load_kv(j):
                Kt, Kk = Kr.next()
                Vt, Vk = Vr.next()
                P.dma("sp", Kt[:], KT[j], w=[Kk])
                P.dma("sp", Vt[:], Vs[j], w=[Vk])
                kv[j] = (Kt, Kk, Vt, Vk)

            qs = {}

            def load_q(i):
                j, tg, g = items[i]
                Qt, Qk = Qr.next()
                P.dma("sp", Qt[:], QT[j * GQ + g, :, tg * 512:(tg + 1) * 512], w=[Qk])
                qs[i] = (Qt, Qk)

            def s_mm(i, kp):
                j = items[i][0]
                Kt, Kk = kv[j][0], kv[j][1]
                Qt, Qk = qs[i]
                b0 = 2 * ((i * NKP + kp) % 3)

                def fn(e, kp=kp, b0=b0, Kt=Kt, Qt=Qt):
                    ins = None
                    for u in range(2):
                        kb = kp * 2 + u
                        ins = e.matmul(ps[b0 + u][:], Kt[:, kb * 128:(kb + 1) * 128], Qt[:], start=True, stop=True)
                    return ins
                P.add("pe", fn, r=[Kk, Qk], w=[f"ps{b0}", f"ps{b0 + 1}"])

            load_kv(0)
            load_q(0)
            s_mm(0, 0)
            s_mm(0, 1)
            for i, (j, tg, g) in enumerate(items):
                h = j * GQ + g
                Kt, Kk, Vt, Vk = kv[j]
                if i + 1 < len(items):
                    if items[i + 1][0] != j:
                        load_kv(items[i + 1][0])
                    load_q(i + 1)
                bo = 6
                bd = 7
                d0, d0k = d0r.next()
                d1, d1k = d1r.next()
                pe_den_started = False
                inited = set()
                for kp in range(NKP):
                    if kp + 2 < NKP:
                        s_mm(i, kp + 2)
                    elif i + 1 < len(items):
                        s_mm(i + 1, kp + 2 - NKP)
                    b0 = 2 * ((i * NKP + kp) % 3)
                    pt, pk = Pr.next()
                    P.add("act", lambda e, pt=pt, b0=b0: e.activation(
                        out=pt[:], in_=psall[:, b0 * 512:(b0 + 2) * 512], func=AF.Exp, scale=scale),
                        r=[f"ps{b0}", f"ps{b0 + 1}"], w=[pk])

                    def pv(e, pt=pt, kp=kp, bo=bo, Vt=Vt):
                        ins = None
                        for u in range(2):
                            kb = kp * 2 + u
                            ins = e.matmul(ps[bo][:], Vt[:, kb, :], pt[:, u * 512:(u + 1) * 512],
                                           start=(kb == 0), stop=(kb == NKB - 1))
                        return ins
                    P.add("pe", pv, r=[Vk, pk], w=[f"ps{bo}"])
                    who = ("dve", "pool", "pe", "dve", "dve", "pool", "dve", "pe")[kp % 8]
                    if who == "pe":
                        def dn(e, pt=pt, bd=bd, first=(not pe_den_started)):
                            ins = None
                            for u in range(2):
                                ins = e.matmul(ps[bd][:], ones[:], pt[:, u * 512:(u + 1) * 512],
                                               start=(first and u == 0), stop=False)
                            return ins
                        P.add("pe", dn, r=["ones", pk], w=[f"ps{bd}"])
                        pe_den_started = True
                    else:
                        eng, dd, ddk = ("dve", d0, d0k) if who == "dve" else ("pool", d1, d1k)
                        if ddk not in inited:
                            inited.add(ddk)
                            P.add(eng, lambda e, dd=dd, pt=pt: e.tensor_copy(out=dd[:], in_=pt[:]), r=[pk], w=[ddk])
                        else:
                            P.add(eng, lambda e, dd=dd, pt=pt: e.tensor_tensor(out=dd[:], in0=dd[:], in1=pt[:], op=ALU.add),
                                  r=[pk, ddk], w=[ddk])

                def dfin(e, d0=d0, d1=d1, bd=bd, st0=(not pe_den_started)):
                    ins = None
                    srcs = [d0[:, 0:512], d0[:, 512:1024], d1[:, 0:512], d1[:, 512:1024]]
                    for i_, sr in enumerate(srcs):
                        ins = e.matmul(ps[bd][:], onesf[:], sr, start=(st0 and i_ == 0), stop=(i_ == 3))
                    return ins
                ow, owk = owr.next()
                P.add("act", lambda e, ow=ow, bo=bo: e.copy(out=ow[:], in_=ps[bo][:]), r=[f"ps{bo}"], w=[owk])
                P.add("pe", dfin, r=["onesf", d0k, d1k], w=[f"ps{bd}"])
                rd, rdk = rdr.next()
                o, ok_ = orr.next()
                sq, sqk = sqr.next()
                yb, ybk = ybr.next()
                P.add("dve", lambda e, rd=rd, bd=bd: e.reciprocal(out=rd[:], in_=ps[bd][:]), r=[f"ps{bd}"], w=[rdk])
                P.add("dve", lambda e, o=o, rd=rd, ow=ow: e.tensor_tensor(out=o[:], in0=ow[:], in1=rd[:], op=ALU.mult),
                      r=[owk, rdk], w=[ok_])
                P.add("act", lambda e, sq=sq, o=o: e.activation(out=sq[:], in_=o[:], func=AF.Square), r=[ok_], w=[sqk])
                sl = slice(tg * 512, (tg + 1) * 512)
                if h == 0:
                    P.add("pool", lambda e, sq=sq, sl=sl: e.tensor_copy(out=ssa[:, sl], in_=sq[:]), r=[sqk], w=[f"ssa{tg}"])
                else:
                    P.add("pool", lambda e, sq=sq, sl=sl: e.tensor_tensor(out=ssa[:, sl], in0=ssa[:, sl], in1=sq[:], op=ALU.add),
                          r=[sqk, f"ssa{tg}"], w=[f"ssa{tg}"])
                P.add("dve", lambda e, yb=yb, o=o, h=h: e.tensor_scalar(
                    out=yb[:], in0=o[:], scalar1=aog[:, h:h + 1], scalar2=None, op0=ALU.mult), r=[ok_, "aog"], w=[ybk])
                P.dma("act", YT[h, :, sl], yb[:], r=[ybk], w=["YTd"])
            for t in range(NTO):
                P.add("pe", lambda e, t=t: e.matmul(ps[7][:, t:t + 1], ssa[:, t * 128:(t + 1) * 128], onesf[:, 0:1],
                                                    start=True, stop=True), r=[f"ssa{t // 4}", "onesf"], w=["ps7"])
            P.add("act", lambda e: e.copy(out=sta[:], in_=ps[7][:, 0:NTO]), r=["ps7"], w=["sta"])
            P.dma("sp", STA[:, 0:NTO], sta[:], r=["sta"], w=["STAd"])
            P.flush()

    def phase_out():
        with ExitStack() as st:
            def sb(name, shape, dt):
                return st.enter_context(nc.sbuf_tensor("op" + name, shape, dt))
            rs = sb("rs", [128, 2 * NTO], F32)
            P.dma("sp", rs[:], STA, w=["rs"])
            P.add("act", lambda e: e.activation(out=rs[:], in_=rs[:], func=AF.Sqrt, scale=1.0 / ATT, bias=epsT[:, 0:1]),
                  r=["rs", "eps"], w=["rs"])
            P.add("dve", lambda e: e.reciprocal(out=rs[:], in_=rs[:]), r=["rs"], w=["rs"])
            ygr = Ring(st, nc, "opyg", 2, [128, NYC, 512], BF16)
            Wr = Ring(st, nc, "opW", 2, [128, NYC, 512], BF16)
            xr = Ring(st, nc, "opx", 4, [128, 512], F32)
            tr_ = Ring(st, nc, "opt", 2, [128, 512], F32)
            orr = Ring(st, nc, "opo", 3, [128, 512], F32)
            bi = 0
            for dc in range(D // 512):
                Wt, Wk_ = Wr.next()
                P.dma("pool", Wt[:], w_out[:, dc * 512:(dc + 1) * 512].rearrange("(c p) n -> p c n", p=128), w=[Wk_])
                for tg in range(OWN // 512):
                    yg, ygk = ygr.next()
                    P.dma("sp", yg[:], YT[:, :, tg * 512:(tg + 1) * 512].rearrange("c p t -> p c t"), w=[ygk])
                    for s in range(4):
                        ba = (bi % 4) * 2
                        bi += 1
                        t_ = tg * 4 + s
                        xp, xk = xr.next()
                        P.dma("sp", xp[:], x[t_ * 128:(t_ + 1) * 128, dc * 512:(dc + 1) * 512], w=[xk])
                        mm_group(P, ps[ba][:], [(yg[:, cc, s * 128:(s + 1) * 128], Wt[:, cc, :]) for cc in range(NQH)],
                                 r=[ygk, Wk_], w=[f"ps{ba}"])
                        mm_group(P, ps[ba + 1][:], [(yg[:, cc, s * 128:(s + 1) * 128], Wt[:, cc, :]) for cc in range(NQH, NYC)],
                                 r=[ygk, Wk_], w=[f"ps{ba + 1}"])
                        tt, tk = tr_.next()
                        ot, ok_ = orr.next()
                        P.add("dve", lambda e, tt=tt, ba=ba, t_=t_, xp=xp: e.scalar_tensor_tensor(
                            out=tt[:], in0=ps[ba][:], scalar=rs[:, t_:t_ + 1], in1=xp[:], op0=ALU.mult, op1=ALU.add),
                            r=[f"ps{ba}", "rs", xk], w=[tk])
                        P.add("dve", lambda e, tt=tt, ot=ot, ba=ba, t_=t_: e.scalar_tensor_tensor(
                            out=ot[:], in0=ps[ba + 1][:], scalar=rs[:, NTO + t_:NTO + t_ + 1], in1=tt[:], op0=ALU.mult, op1=ALU.add),
                            r=[f"ps{ba + 1}", "rs", tk], w=[ok_])
                        P.dma("act", X1[t_ * 128:(t_ + 1) * 128, dc * 512:(dc + 1) * 512], ot[:], r=[ok_], w=["X1d"])
            P.flush()

    def wdt_prep(st):
        HC = max(NCH // 2, 1)
        HD = HC * 128
        NPJ = NCH // HC
        Wr = Ring(st, nc, "ppW", 2, [128, HD], BF16)
        Wfr = Ring(st, nc, "ppWf", 2, [128, HD], F32)
        Tr = Ring(st, nc, "ppT", 2, [128, HC, 128], BF16)
        wdv = w_down.rearrange("(i j) d -> j i d", j=128)
        state = {"bi": 0, "n": 0}

        def one(n, eng):
            j, hp = n // NPJ, n % NPJ
            Wt, Wk_ = Wr.next()
            Tt, Tk = Tr.next()
            Wf, Wfk = Wfr.next()
            P.dma("sp", Wf[:], wdv[j][:, hp * HD:(hp + 1) * HD], w=[Wfk])
            if eng == "pool":
                P.add("pool", lambda e, Wt=Wt, Wf=Wf: e.tensor_copy(out=Wt[:], in_=Wf[:]), r=[Wfk], w=[Wk_])
            else:
                P.add("act", lambda e, Wt=Wt, Wf=Wf: e.copy(out=Wt[:], in_=Wf[:]), r=[Wfk], w=[Wk_])
            for c0 in range(0, HC, 8):
                nb = min(8, HC - c0)
                bk = (4, 7)[state["bi"] % 2]
                state["bi"] += 1
                pb = ps[bk][:].bitcast(BF16)

                def tr(e, Wt=Wt, c0=c0, pb=pb, nb=nb):
                    ins = None
                    for cc in range(c0, c0 + nb):
                        ins = e.transpose(out=pb[:, (cc - c0) * 128:(cc - c0 + 1) * 128],
                                          in_=Wt[:, cc * 128:(cc + 1) * 128], identity=ident[:])
                    return ins
                P.add("pe", tr, r=[Wk_, "ident"], w=[f"ps{bk}"])
                P.add("act", lambda e, Tt=Tt, c0=c0, pb=pb, nb=nb: e.copy(
                    out=Tt[:, c0:c0 + nb, :], in_=pb[:, 0:nb * 128].rearrange("p (c t) -> p c t", c=nb)), r=[f"ps{bk}"], w=[Tk])
            P.dma("act", WDT[j][:, hp * HC:(hp + 1) * HC, :], Tt[:], r=[Tk], w=["WDTd"])

        total = 128 * NPJ

        def step(k, nsteps, eng):
            hi_ = (total * (k + 1)) // nsteps
            while state["n"] < hi_:
                one(state["n"], eng)
                state["n"] += 1
        return step

    def phase_peer_prep():
        with ExitStack() as st:
            def sb(name, shape, dt):
                return st.enter_context(nc.sbuf_tensor("pq" + name, shape, dt))
            hgr = Ring(st, nc, "pqhg", 2, [128, 4, NCH, 128], BF16)
            Wr = Ring(st, nc, "pqW", 2, [128, NCH, 512], BF16)
            qr = Ring(st, nc, "pqq", 3, [128, 512], BF16)
            bi = 0
            for q4 in range(PQ // 512):
                Wt, Wk_ = Wr.next()
                P.dma("pool", Wt[:], w_query[:, q4 * 512:(q4 + 1) * 512].rearrange("(c p) n -> p c n", p=128), w=[Wk_])
                for tg in range(OWN // 512):
                    hgt, hgk = hgr.next()
                    P.dma("sp", hgt[:], H2T[tg * 4:(tg + 1) * 4].rearrange("t p c k -> p t c k"), w=[hgk])
                    for hh in range(4):
                        b = bi % 4
                        bi += 1
                        mm_group(P, ps[b][:].rearrange("p (t k) -> p t k", t=4),
                                 [(Wt[:, cc, hh * 128:(hh + 1) * 128], hgt[:, :, cc, :]) for cc in range(NCH)],
                                 r=[Wk_, hgk], w=[f"ps{b}"])
                        qt, qk_ = qr.next()
                        P.add("act", lambda e, qt=qt, b=b: e.copy(out=qt[:], in_=ps[b][:]), r=[f"ps{b}"], w=[qk_])
                        P.dma("act", QPT[q4 * 4 + hh, :, tg * 512:(tg + 1) * 512], qt[:], r=[qk_], w=["QPTd"])
            P.flush()

    def phase_peer_pick():
        with ExitStack() as st:
            def sb(name, shape, dt):
                return st.enter_context(nc.sbuf_tensor("pk" + name, shape, dt))
            kn = sb("kn", [128, 16, 128], BF16)
            kT = sb("kT", [128, 16, 128], BF16)
            iot = sb("iot", [128, 128], F32)
            P.dma("pool", kn[:], sub_keys.rearrange("g n k -> n g k"), w=["kn"])
            P.dma("sp", iot[:], iota_d, w=["iot"])
            for g0 in range(0, 16, 8):
                pb = ps[0][:].bitcast(BF16)

                def tr(e, g0=g0, pb=pb):
                    ins = None
                    for g in range(g0, g0 + 8):
                        ins = e.transpose(out=pb[:, (g - g0) * 128:(g - g0 + 1) * 128], in_=kn[:, g, :], identity=ident[:])
                    return ins
                P.add("pe", tr, r=["kn", "ident"], w=["ps0"])
                P.add("act", lambda e, g0=g0, pb=pb: e.copy(out=kT[:, g0:g0 + 8, :], in_=pb.rearrange("p (c t) -> p c t", c=8)),
                      r=["ps0"], w=["kT"])
            qpr = Ring(st, nc, "pkqp", 2, [128, 16, 128], BF16)
            S = sb("S", [128, 16, 128], F32)
            S2 = sb("S2", [128, 16, 128], F32)
            V = sb("V", [128, 16, 16], F32)
            IX = sb("IX", [128, 16, 16], U32)
            IXf = sb("IXf", [128, 16, 16], F32)
            cand = sb("cand", [128, 8, 256], F32)
            cand2 = sb("cand2", [128, 8, 256], F32)
            Bv = sb("Bv", [128, 8, 16], F32)
            PX = sb("PX", [128, 8, 16], U32)
            R1u = sb("R1u", [128, 8, 16], U32)
            R2u = sb("R2u", [128, 8, 16], U32)
            R1 = sb("R1", [128, 8, 16], F32)
            R2 = sb("R2", [128, 8, 16], F32)
            E = sb("E", [128, 8, 16], F32)
            Z = sb("Z", [128, 8], F32)
            eq = sb("eq", [128, 8, 16, 16], F32)
            PI = sb("PI", [128, 128], F32)
            PJ = sb("PJ", [128, 128], F32)
            PG = sb("PG", [128, 128], F32)
            PT = sb("PT", [128, 3, 128], F32)
            OI = sb("OI", [128, 128, 128], BF16)
            OJ = sb("OJ", [128, 128, 128], BF16)
            Gr = Ring(st, nc, "pkG", 1, [128, 128, 128], BF16)
            prep_step = wdt_prep(st)
            prep_k = 0

            def dv(fn, r, w):
                P.add("dve", fn, r=r, w=w)

            def scores(tt):
                qp, qpk = qpr.next()
                P.dma("sp", qp[:], QPT[:, :, tt * 128:(tt + 1) * 128].rearrange("g p t -> p g t"), w=[qpk])
                for b in range(4):
                    for g4 in range(4):
                        g = b * 4 + g4
                        P.add("pe", lambda e, g=g, g4=g4, b=b, qp=qp: e.matmul(
                            ps[b][:, g4 * 128:(g4 + 1) * 128], qp[:, g, :], kT[:, g, :], start=True, stop=True),
                            r=[qpk, "kT"] + ([f"ps{b}"] if g4 else []), w=[f"ps{b}"])
                    P.add("act", lambda e, b=b: e.copy(out=S[:, b * 4:(b + 1) * 4, :],
                                                       in_=ps[b][:].rearrange("p (g n) -> p g n", g=4)),
                          r=[f"ps{b}"], w=[f"S{g_}" for g_ in range(b * 4, b * 4 + 4)])

            def topk():
                G16 = range(16)
                for g in G16:
                    dv(lambda e, g=g: e.max(out=V[:, g, 0:8], in_=S[:, g, :]), [f"S{g}"], [f"Va{g}"])
                for g in G16:
                    dv(lambda e, g=g: e.max_index(out=IX[:, g, 0:8], in_max=V[:, g, 0:8], in_values=S[:, g, :]),
                       [f"S{g}", f"Va{g}"], [f"IXa{g}"])
                for g in G16:
                    dv(lambda e, g=g: e.match_replace(out=S2[:, g, :], in_to_replace=V[:, g, 0:8], in_values=S[:, g, :],
                                                      imm_value=NEG), [f"S{g}", f"Va{g}"], [f"S2{g}"])
                for g in G16:
                    dv(lambda e, g=g: e.max(out=V[:, g, 8:16], in_=S2[:, g, :]), [f"S2{g}"], [f"Vb{g}"])
                for g in G16:
                    dv(lambda e, g=g: e.max_index(out=IX[:, g, 8:16], in_max=V[:, g, 8:16], in_values=S2[:, g, :]),
                       [f"S2{g}", f"Vb{g}"], [f"IXb{g}"])
                allV = [f"Va{g}" for g in G16] + [f"Vb{g}" for g in G16]
                allIX = [f"IXa{g}" for g in G16] + [f"IXb{g}" for g in G16]
                dv(lambda e: e.tensor_copy(out=IXf[:], in_=IX[:]), allIX, ["IXf"])
                Vv = V[:].rearrange("p (h two) k -> p h two k", two=2)
                H8 = range(8)
                dv(lambda e, Vv=Vv: e.tensor_tensor(
                    out=cand[:].rearrange("p h (a b) -> p h a b", a=16),
                    in0=Vv[:, :, 0, :].unsqueeze(3).to_broadcast([128, 8, 16, 16]),
                    in1=Vv[:, :, 1, :].unsqueeze(2).to_broadcast([128, 8, 16, 16]), op=ALU.add), allV, [f"cand{h}" for h in H8])
                for h in H8:
                    dv(lambda e, h=h: e.max(out=Bv[:, h, 0:8], in_=cand[:, h, :]), [f"cand{h}"], [f"Ba{h}"])
                for h in H8:
                    dv(lambda e, h=h: e.max_index(out=PX[:, h, 0:8], in_max=Bv[:, h, 0:8], in_values=cand[:, h, :]),
                       [f"cand{h}", f"Ba{h}"], [f"PXa{h}"])
                for h in H8:
                    dv(lambda e, h=h: e.match_replace(out=cand2[:, h, :], in_to_replace=Bv[:, h, 0:8], in_values=cand[:, h, :],
                                                      imm_value=NEG), [f"cand{h}", f"Ba{h}"], [f"cand2{h}"])
                for h in H8:
                    dv(lambda e, h=h: e.max(out=Bv[:, h, 8:16], in_=cand2[:, h, :]), [f"cand2{h}"], [f"Bb{h}"])
                for h in H8:
                    dv(lambda e, h=h: e.max_index(out=PX[:, h, 8:16], in_max=Bv[:, h, 8:16], in_values=cand2[:, h, :]),
                       [f"cand2{h}", f"Bb{h}"], [f"PXb{h}"])
                allB = [f"Ba{h}" for h in H8] + [f"Bb{h}" for h in H8]
                allPX = [f"PXa{h}" for h in H8] + [f"PXb{h}" for h in H8]

            def topk_b():
                H8 = range(8)
                allB = [f"Ba{h}" for h in H8] + [f"Bb{h}" for h in H8]
                allPX = [f"PXa{h}" for h in H8] + [f"PXb{h}" for h in H8]
                dv(lambda e: e.tensor_tensor(out=E[:], in0=Bv[:], in1=Bv[:, :, 0:1].to_broadcast([128, 8, 16]), op=ALU.subtract),
                   allB, ["E"])
                P.add("act", lambda e: e.activation(out=E[:], in_=E[:], func=AF.Exp), r=["E"], w=["E"])
                dv(lambda e: e.tensor_reduce(out=Z[:], in_=E[:], axis=AX.X, op=ALU.add), ["E"], ["Z"])
                dv(lambda e: e.reciprocal(out=Z[:], in_=Z[:]), ["Z"], ["Z"])
                dv(lambda e: e.tensor_tensor(out=PG[:].rearrange("p (h k) -> p h k", h=8), in0=E[:],
                                             in1=Z[:].unsqueeze(2).to_broadcast([128, 8, 16]), op=ALU.mult), ["E", "Z"], ["PG"])
                dv(lambda e: e.tensor_single_scalar(out=R1u[:], in_=PX[:], scalar=4, op=ALU.logical_shift_right), allPX, ["R1u"])
                dv(lambda e: e.tensor_single_scalar(out=R2u[:], in_=PX[:], scalar=15, op=ALU.bitwise_and), allPX, ["R2u"])
                dv(lambda e: e.tensor_copy(out=R1[:], in_=R1u[:]), ["R1u"], ["R1"])
                dv(lambda e: e.tensor_copy(out=R2[:], in_=R2u[:]), ["R2u"], ["R2"])
                IXv = IXf[:].rearrange("p (h two) k -> p h two k", two=2)
                for (Rr, Rk, two, Pout, Pk) in ((R1, "R1", 0, PI, "PI"), (R2, "R2", 1, PJ, "PJ")):
                    dv(lambda e, Rr=Rr: e.tensor_tensor(
                        out=eq[:], in0=Rr[:].unsqueeze(3).to_broadcast([128, 8, 16, 16]),
                        in1=iot[:, 0:16].unsqueeze(1).unsqueeze(1).to_broadcast([128, 8, 16, 16]), op=ALU.is_equal),
                        [Rk, "iot"], ["eq"])
                    dv(lambda e, two=two, IXv=IXv: e.tensor_tensor(
                        out=eq[:], in0=eq[:], in1=IXv[:, :, two, :].unsqueeze(2).to_broadcast([128, 8, 16, 16]), op=ALU.mult),
                        ["eq", "IXf"], ["eq"])
                    dv(lambda e, Pout=Pout: e.tensor_reduce(out=Pout[:].rearrange("p (h k) -> p h k", h=8), in_=eq[:],
                                                            axis=AX.X, op=ALU.add), ["eq"], [Pk])

            scores(0)
            topk()
            topk_b()
            for tt in range(NTO):
                for n_, (Pin, Pk) in enumerate(((PI, "PI"), (PJ, "PJ"), (PG, "PG"))):
                    P.add("pe", lambda e, n_=n_, Pin=Pin: e.transpose(out=ps[4][:, n_ * 128:(n_ + 1) * 128], in_=Pin[:], identity=identf[:]),
                          r=[Pk, "identf"] + (["ps4"] if n_ else []), w=["ps4"])
                P.add("act", lambda e: e.copy(out=PT[:], in_=ps[4][:, 0:384].rearrange("p (a t) -> p a t", a=3)), r=["ps4"], w=["PT"])
                dv(lambda e: e.tensor_tensor(
                    out=OJ[:], in0=PT[:, 1, :].unsqueeze(2).to_broadcast([128, 128, 128]),
                    in1=iot[:].unsqueeze(1).to_broadcast([128, 128, 128]), op=ALU.is_equal), ["PT", "iot"], ["OJ"])
                P.add("pool", lambda e: e.tensor_tensor(
                    out=OJ[:], in0=OJ[:], in1=PT[:, 2, :].unsqueeze(2).to_broadcast([128, 128, 128]), op=ALU.mult),
                    r=["PT", "OJ"], w=["OJ"])
                dv(lambda e: e.tensor_tensor(
                    out=OI[:], in0=PT[:, 0, :].unsqueeze(2).to_broadcast([128, 128, 128]),
                    in1=iot[:].unsqueeze(1).to_broadcast([128, 128, 128]), op=ALU.is_equal), ["PT", "iot"], ["OI"])
                if tt + 1 < NTO:
                    scores(tt + 1)
                    topk()
                Gt, Gk = Gr.next()
                for sl_ in range(8):
                    prep_step(tt * 16 + sl_, NTO * 16, "act")
                for t4 in range(32):
                    b = 5 + (t4 % 2)

                    def gm(e, t4=t4, b=b):
                        ins = None
                        for u in range(4):
                            t_ = t4 * 4 + u
                            ins = e.matmul(ps[b][:, u * 128:(u + 1) * 128], OI[:, t_, :], OJ[:, t_, :], start=True, stop=True)
                        return ins
                    P.add("pe", gm, r=["OI", "OJ"], w=[f"ps{b}"])
                    outv = Gt[:, :, t4 * 4:(t4 + 1) * 4].rearrange("p j t -> p t j")
                    inv = ps[b][:].rearrange("p (t j) -> p t j", t=4)
                    P.add("act", lambda e, outv=outv, inv=inv: e.copy(out=outv, in_=inv), r=[f"ps{b}"], w=[Gk])
                    if t4 % 4 == 3:
                        prep_step(tt * 16 + 8 + t4 // 4, NTO * 16, "pool")
                    if t4 == 19 and tt + 1 < NTO:
                        topk_b()
                P.dma("act", GS[tt], Gt[:], r=[Gk], w=["GSd"])
            P.flush()

    def phase_peer_main():
        with ExitStack() as st:
            def sb(name, shape, dt):
                return st.enter_context(nc.sbuf_tensor("pm" + name, shape, dt))
            gB = sb("gB", [128, D], F32)
            P.dma("sp", gB[:], g_fin.partition_broadcast(128), w=["gB"])
            hgt = sb("hg", [128, 4, NCH, 128], BF16)
            acc = sb("acc", [128, 4, D], F32)
            WDr = Ring(st, nc, "pmWD", 4, [128, NCH, 128], BF16)
            WUr = Ring(st, nc, "pmWU", 2, [128, 2, D], BF16)
            Gr = Ring(st, nc, "pmG", 4, [128, 4, 2, 128], BF16)
            Dgr = Ring(st, nc, "pmDg", 2, [128, 2, 512], BF16)
            Ar = Ring(st, nc, "pmA", 3, [128, 2, 512], BF16)
            ss = sb("ss", [128, 4], F32)
            wuv = w_up.rearrange("(i j) d -> i j d", j=128)
            NJG = 64
            NUP = 4 * (D // 512)
            ui = 0
            for tg in range(OWN // 512):
                P.dma("sp", hgt[:], H2T[tg * 4:(tg + 1) * 4].rearrange("t p c k -> p t c k"), w=["hg"])
                P.dma("sp", acc[:], X1[tg * 512:(tg + 1) * 512, :].rearrange("(s p) d -> p s d", p=128), w=["acc"])
                stt = {}

                def load_down(jg, tg=tg, stt=stt):
                    d = stt.setdefault(jg, {})
                    d["WD"] = []
                    for jj in range(2):
                        WD, WDk = WDr.next()
                        P.dma("sp", WD[:], WDT[jg * 2 + jj], w=[WDk])
                        d["WD"].append((WD, WDk))
                    Gt, Gk = Gr.next()
                    P.dma("sp", Gt[:], GS[tg * 4:(tg + 1) * 4, :, jg * 2:(jg + 1) * 2, :].rearrange("s i j t -> i s j t"), w=[Gk])
                    d["G"] = (Gt, Gk)

                def load_up(jg, stt=stt):
                    WU, WUk = WUr.next()
                    P.dma("pool", WU[:], wuv[:, jg * 2:(jg + 1) * 2, :], w=[WUk])
                    stt.setdefault(jg, {})["WU"] = (WU, WUk)

                def down_pieces(jg, stt=stt):
                    d = stt[jg]
                    Dg, Dgk = Dgr.next()
                    A, Ak = Ar.next()
                    d["A"] = (A, Ak)
                    Gt, Gk = d["G"]
                    pieces = []
                    for k in range(NUP):
                        def piece(k=k, d=d, Dg=Dg, Dgk=Dgk, A=A, Ak=Ak, Gt=Gt, Gk=Gk):
                            ms = []
                            for m in (2 * k, 2 * k + 1):
                                jj, cc = m // NCH, m % NCH
                                ms.append((jj, cc))
                            jj = ms[0][0]
                            WD, WDk = d["WD"][jj]

                            def fn(e, ms=ms, WD=WD, jj=jj):
                                ins = None
                                for (_, cc) in ms:
                                    ins = e.matmul(ps[jj][:].rearrange("p (t k) -> p t k", t=4), WD[:, cc, :], hgt[:, :, cc, :],
                                                   start=(cc == 0), stop=(cc == NCH - 1))
                                return ins
                            P.add("pe", fn, r=[WDk, "hg"], w=[f"ps{jj}"])
                            if ms[-1][1] == NCH - 1:
                                P.add("act", lambda e, Dg=Dg, jj=jj: e.activation(out=Dg[:, jj, :], in_=ps[jj][:], func=AF.Gelu),
                                      r=[f"ps{jj}"], w=[Dgk])
                                if jj == 1:
                                    P.add("pool", lambda e, A=A, Dg=Dg, Gt=Gt: e.tensor_tensor(
                                        out=A[:].rearrange("p j (s t) -> p j s t", s=4),
                                        in0=Dg[:].rearrange("p j (s t) -> p j s t", s=4),
                                        in1=Gt[:].rearrange("p s j t -> p j s t"), op=ALU.mult), r=[Dgk, Gk], w=[Ak])
                        pieces.append(piece)
                    return pieces

                load_down(0)
                load_down(1)
                load_up(0)
                for pc in down_pieces(0):
                    pc()
                load_down(2)
                for pc in down_pieces(1):
                    pc()
                for jg in range(NJG):
                    if jg + 3 < NJG:
                        load_down(jg + 3)
                    if jg + 1 < NJG:
                        load_up(jg + 1)
                    pieces = down_pieces(jg + 2) if jg + 2 < NJG else []
                    A, Ak = stt[jg]["A"]
                    WU, WUk = stt[jg]["WU"]
                    k = 0
                    for s_ in range(4):
                        for dc in range(D // 512):
                            b = 2 + (ui % 6)
                            ui += 1
                            mm_group(P, ps[b][:], [(A[:, jj, s_ * 128:(s_ + 1) * 128], WU[:, jj, dc * 512:(dc + 1) * 512]) for jj in range(2)],
                                     r=[Ak, WUk], w=[f"ps{b}"])
                            P.add("dve", lambda e, s_=s_, dc=dc, b=b: e.tensor_tensor(
                                out=acc[:, s_, dc * 512:(dc + 1) * 512], in0=acc[:, s_, dc * 512:(dc + 1) * 512], in1=ps[b][:], op=ALU.add),
                                r=[f"ps{b}", "acc"], w=["acc"])
                            if pieces:
                                pieces[k]()
                            k += 1
                    del stt[jg]
                for s in range(4):
                    P.add("dve", lambda e, s=s: e.scalar_tensor_tensor(
                        out=hgt[:].rearrange("p a c k -> p (a c k)")[:, 0:D], in0=acc[:, s, :], scalar=1.0, in1=acc[:, s, :],
                        op0=ALU.mult, op1=ALU.mult, accum_out=ss[:, s:s + 1]), r=["acc", "hg"], w=["hg", "ss"])
                P.add("act", lambda e: e.activation(out=ss[:], in_=ss[:], func=AF.Sqrt, scale=1.0 / D, bias=epsT[:, 0:1]),
                      r=["ss", "eps"], w=["ss"])
                P.add("dve", lambda e: e.reciprocal(out=ss[:], in_=ss[:]), r=["ss"], w=["ss"])
                for s in range(4):
                    P.add("dve", lambda e, s=s: e.scalar_tensor_tensor(
                        out=acc[:, s, :], in0=acc[:, s, :], scalar=ss[:, s:s + 1], in1=gB[:], op0=ALU.mult, op1=ALU.mult),
                        r=["acc", "ss", "gB"], w=["acc"])
                P.dma("sp", out[tg * 512:(tg + 1) * 512, :].rearrange("(s p) d -> p s d", p=128), acc[:], r=["acc"], w=["outd"])
            P.flush()

    load_consts()
    stages = getattr(c, "stages", 99)
    phase_norm_T(x, g_mix, hT, NT, "na")
    if stages >= 2:
        phase_kv()
    if stages >= 3:
        phase_qc()
    if stages >= 4:
        phase_attn()
    if stages >= 5:
        phase_out()
    if stages >= 6:
        phase_norm_T(X1, g_ffn, H2T, NTO, "nf")
    if stages >= 7:
        phase_peer_prep()
    if stages >= 8:
        phase_peer_pick()
    if stages >= 9:
        phase_peer_main()
    gst.close()
    P.close()
    return nc


def rope_tables(SB):
    half = 64
    inv = (10000.0 ** (-np.arange(0, half, 2, dtype=np.float32) / half)).astype(np.float32)
    t = np.arange(SB)
    row = (t // 64).astype(np.float32)
    col = (t % 64).astype(np.float32)
    ang_r = row[:, None] * inv
    ang_c = col[:, None] * inv
    cosT = np.empty((128, SB), np.float32)
    sinT = np.empty((128, SB), np.float32)
    for blk, ang in ((0, ang_r), (1, ang_c)):
        c = np.cos(ang).T.astype(np.float32)
        s = np.sin(ang).T.astype(np.float32)
        o = blk * 64
        cosT[o:o + 32] = c
        cosT[o + 32:o + 64] = c
        sinT[o:o + 32] = -s
        sinT[o + 32:o + 64] = s
    return cosT, sinT


def make_in_maps(c, inputs, n_cores=8):
    D, SB, OWN = c.D, c.SB, c.OWN
    bf = ml_dtypes.bfloat16
    cosT, sinT = rope_tables(SB)
    perm = np.zeros((128, 128), np.float32)
    for d in range(128):
        perm[(d // 64) * 64 + ((d % 64) + 32) % 64, d] = 1.0
    iota = np.tile(np.arange(128, dtype=np.float32)[None, :], (128, 1))
    f = lambda a: np.ascontiguousarray(a, dtype=np.float32)
    shared = {
        "w_in": f(inputs["w_in"][0]), "w_out": f(inputs["w_out"][0]), "w_query": f(inputs["peer_w_query"][0]),
        "sub_keys": f(inputs["peer_sub_keys"][0].reshape(16, 128, 128)),
        "w_down": f(inputs["peer_w_down"][0]), "w_up": f(inputs["peer_w_up"][0]),
        "g_mix": f(inputs["norm_mix_g"][0][None, :]), "g_ffn": f(inputs["norm_ffn_g"][0][None, :]),
        "g_fin": f(inputs["norm_final_g"][None, :]),
        "qk_g": f(np.stack([inputs["q_norm_g"][0], inputs["k_norm_g"][0]], axis=1)),
        "conv_w": f(inputs["conv_w"][0].T.reshape(c.NCB, 128, 3).transpose(1, 0, 2)),
        "ao_g": f(inputs["attn_out_g"][0].reshape(c.NQH, 128).T),
        "co_g": f(inputs["conv_out_g"][0].reshape(c.NCB, 128).T),
        "ident": np.eye(128, dtype=np.float32).astype(bf), "identf": np.eye(128, dtype=np.float32),
        "perm": perm.astype(bf), "iota": iota,
    }
    maps = []
    for core in range(n_cores):
        b, qd = core // 4, core % 4
        sh = qd * OWN
        m = dict(shared)
        m["x"] = f(np.roll(inputs["x"][b], -sh, axis=0))
        m["cosT"] = f(np.roll(cosT, -sh, axis=1))
        m["sinT"] = f(np.roll(sinT, -sh, axis=1))
        edge = np.ones((128, 2), np.float32)
        if qd == 0:
            edge[:, 0] = 0.0
        if qd == 3:
            edge[:, 1] = 0.0
        m["edge"] = edge
        maps.append(m)
    return maps


def kernel(**inputs):
    inputs = {k: np.asarray(v) for k, v in inputs.items()}
    c = Cfg()
    nc = build(c)
    maps = make_in_maps(c, inputs)
    res = run_bass_kernel_spmd(nc, maps, core_ids=list(range(8)))
    outs = [np.asarray(r["out"], dtype=np.float32) for r in res.results]
    B = inputs["x"].shape[0]
    full = np.stack([np.concatenate(outs[b * 4:(b + 1) * 4], axis=0) for b in range(B)], axis=0)
    return full.astype(np.float32)
```

```python
import numpy as np
from contextlib import ExitStack
import ml_dtypes
import concourse.bass as bass
import concourse.mybir as mybir
from concourse.bass_utils import run_bass_kernel_spmd

F32 = mybir.dt.float32
BF16 = mybir.dt.bfloat16
U32 = mybir.dt.uint32
AF = mybir.ActivationFunctionType
ALU = mybir.AluOpType
AX = mybir.AxisListType
ENGS = ["pe", "act", "dve", "pool", "sp"]
EPS = 1e-6
NEG = -1.0e30


class Prog:
    R = 8

    def __init__(self, nc):
        self.nc = nc
        self.pending = []
        self.gid = 0
        self.sig = {}
        self.eng_of = {}
        self.isdma = {}
        self.last_write = {}
        self.readers = {}
        self.cnt = {e: 0 for e in ENGS}
        self.known = {e: {} for e in ENGS}
        self.last_op = {e: None for e in ENGS}
        self.dma_n = {"sp": 0, "pool": 0, "act": 0}
        self.dma_hist = {"sp": [], "pool": [], "act": []}
        self.stack = ExitStack()
        self.sem = {e: self.stack.enter_context(nc.semaphore("s_" + e)) for e in ENGS}
        self.ring = {q: [self.stack.enter_context(nc.semaphore(f"r_{q}{i}")) for i in range(self.R)]
                     for q in ("sp", "pool", "act")}

    def add(self, eng, fn, r=(), w=(), dma=False):
        w = tuple(w) + tuple(k for k in r if k.startswith("ps") and k not in w)
        self.pending.append(dict(eng=eng, fn=fn, r=tuple(r), w=tuple(w), dma=dma, gid=self.gid, barrier=False))
        self.gid += 1

    def dma(self, q, out, in_, r=(), w=()):
        self.add(q, lambda e, o=out, i=in_: e.dma_start(out=o, in_=i), r, w, dma=True)

    def barrier(self):
        for e in ENGS:
            self.pending.append(dict(eng=e, fn=None, r=(), w=(), dma=False, gid=self.gid, barrier=True))
            self.gid += 1

    def flush(self):
        self.barrier()
        ops = self.pending
        self.pending = []
        needed = set()
        last_op = dict(self.last_op)
        dma_hist = {q: list(v) for q, v in self.dma_hist.items()}
        dma_n = dict(self.dma_n)
        for op in ops:
            g = op["gid"]
            self.eng_of[g] = op["eng"]
            self.isdma[g] = op["dma"]
            deps = set()
            if op["barrier"]:
                for e in ENGS:
                    if last_op[e] is not None:
                        deps.add(last_op[e])
                for q in dma_hist:
                    deps.update(dma_hist[q][-self.R:])
            else:
                for k in op["r"]:
                    if k in self.last_write:
                        deps.add(self.last_write[k])
                for k in op["w"]:
                    if k in self.last_write:
                        deps.add(self.last_write[k])
                    deps.update(self.readers.get(k, ()))
                for k in op["w"]:
                    self.last_write[k] = g
                    self.readers[k] = []
                for k in op["r"]:
                    if k not in op["w"]:
                        lst = self.readers.setdefault(k, [])
                        if not op["dma"]:
                            lst[:] = [x for x in lst if self.isdma[x] or self.eng_of[x] != op["eng"]]
                        lst.append(g)
                if op["dma"]:
                    q = op["eng"]
                    n = dma_n[q]
                    if n >= self.R:
                        deps.add(dma_hist[q][n - self.R])
                    dma_hist[q].append(g)
                    dma_n[q] = n + 1
                else:
                    last_op[op["eng"]] = g
            deps.discard(g)
            op["deps"] = deps
            for d in deps:
                if not self.isdma[d]:
                    needed.add(d)
        for op in ops:
            g = op["gid"]
            if op["barrier"]:
                continue
            e = op["eng"]
            if op["dma"]:
                n = self.dma_n[e]
                self.sig[g] = (self.ring[e][n % self.R], 16 * (n // self.R + 1))
                self.dma_n[e] = n + 1
                self.dma_hist[e].append(g)
                op["inc"] = True
            else:
                self.last_op[e] = g
                if g in needed:
                    self.cnt[e] += 1
                    self.sig[g] = (self.sem[e], self.cnt[e])
                    op["inc"] = True
                else:
                    op["inc"] = False
        for op in ops:
            e = op["eng"]
            waits = {}
            for d in sorted(op["deps"]):
                if (not self.isdma[d]) and self.eng_of[d] == "pe" and e == "pe":
                    continue
                s, v = self.sig[d]
                key = id(s)
                if self.known[e].get(key, 0) >= v:
                    continue
                if key not in waits or waits[key][1] < v:
                    waits[key] = (s, v)
            for key, (s, v) in waits.items():
                self.known[e][key] = v
            op["waits"] = list(waits.values())
        per = {e: [o for o in ops if o["eng"] == e] for e in ENGS}

        def run(eng_obj, lst):
            for op in lst:
                for (s, v) in op["waits"]:
                    eng_obj.wait_ge(s, v)
                if op["fn"] is None:
                    continue
                ins = op["fn"](eng_obj)
                if op["inc"]:
                    s, v = self.sig[op["gid"]]
                    ins.then_inc(s, 16 if op["dma"] else 1)

        with self.nc.Block() as block:
            @block.tensor
            def _(e):
                run(e, per["pe"])

            @block.scalar
            def _(e):
                run(e, per["act"])

            @block.vector
            def _(e):
                run(e, per["dve"])

            @block.gpsimd
            def _(e):
                run(e, per["pool"])

            @block.sync
            def _(e):
                run(e, per["sp"])
        self.last_write.clear()
        self.readers.clear()

    def close(self):
        self.stack.close()


class Ring:
    def __init__(self, st, nc, name, n, shape, dt):
        self.t = [st.enter_context(nc.sbuf_tensor(f"{name}{i}", shape, dt)) for i in range(n)]
        self.name = name
        self.i = 0

    def next(self):
        k = self.i % len(self.t)
        self.i += 1
        return self.t[k], f"{self.name}{k}"


class Cfg:
    def __init__(self, D=4096, SB=8192):
        self.D = D
        self.SB = SB
        self.OWN = SB // 4
        self.NCH = D // 128
        self.ATT = D // 2
        self.NQH = self.ATT // 128
        self.NKV = max(1, self.NQH // 4)
        self.GQ = self.NQH // self.NKV
        self.KVW = self.NKV * 128
        self.CW = D - self.ATT
        self.NCB = self.CW // 128
        self.INC = self.ATT + 2 * self.KVW + 3 * self.CW
        self.NT = SB // 128
        self.NTO = self.OWN // 128
        self.PQ = 2048
        self.NE = 16384


def mm_group(P, out_ap, pairs, r, w):
    def fn(e, pairs=pairs, out_ap=out_ap):
        n = len(pairs)
        ins = None
        for i, (l, rh) in enumerate(pairs):
            ins = e.matmul(out_ap, l, rh, start=(i == 0), stop=(i == n - 1))
        return ins
    P.add("pe", fn, r=r, w=w)


def build(c, debug=False):
    nc = bass.Bass("TRN2", target_bir_lowering=False)
    D, SB, OWN, NCH, ATT, NQH, NKV, GQ, KVW, CW, NCB, INC, NT, NTO, PQ, NE = (
        c.D, c.SB, c.OWN, c.NCH, c.ATT, c.NQH, c.NKV, c.GQ, c.KVW, c.CW, c.NCB, c.INC, c.NT, c.NTO, c.PQ, c.NE)
    NYC = NQH + NCB

    def din(name, shape, dt=F32):
        return nc.dram_tensor(name, shape, dt, kind="ExternalInput").ap()

    def dscr(name, shape, dt):
        return nc.dram_tensor(name, shape, dt, kind=("ExternalOutput" if debug else "Internal")).ap()

    x = din("x", [SB, D])
    w_in = din("w_in", [D, INC])
    w_out = din("w_out", [D, D])
    w_query = din("w_query", [D, PQ])
    sub_keys = din("sub_keys", [16, 128, 128])
    w_down = din("w_down", [NE, D])
    w_up = din("w_up", [NE, D])
    g_mix = din("g_mix", [1, D])
    g_ffn = din("g_ffn", [1, D])
    g_fin = din("g_fin", [1, D])
    qk_g = din("qk_g", [128, 2])
    conv_w = din("conv_w", [128, NCB, 3])
    ao_g = din("ao_g", [128, NQH])
    co_g = din("co_g", [128, NCB])
    cosT = din("cosT", [128, SB])
    sinT = din("sinT", [128, SB])
    ident_d = din("ident", [128, 128], BF16)
    identf_d = din("identf", [128, 128])
    perm_d = din("perm", [128, 128], BF16)
    iota_d = din("iota", [128, 128])
    edge_d = din("edge", [128, 2])
    out = nc.dram_tensor("out", [OWN, D], F32, kind="ExternalOutput").ap()

    hT = dscr("hT", [NT, 128, NCH, 128], BF16)
    KT = dscr("KT", [NKV, 128, SB], BF16)
    Vs = dscr("Vs", [NKV, 128, NT, 128], BF16)
    QT = dscr("QT", [NQH, 128, OWN], BF16)
    YT = dscr("YT", [NYC, 128, OWN], BF16)
    X1 = dscr("X1", [OWN, D], F32)
    H2T = dscr("H2T", [NTO, 128, NCH, 128], BF16)
    QPT = dscr("QPT", [16, 128, OWN], BF16)
    WDT = dscr("WDT", [128, 128, NCH, 128], BF16)
    GS = dscr("GS", [NTO, 128, 128, 128], BF16)
    STA = dscr("STA", [128, 2 * NTO], F32)
    NWT = (ATT + 3 * CW) // 256
    WB = dscr("WB", [NWT, 128, NCH, 256], BF16)

    def wb_col0(i):
        return i * 256 if i < ATT // 256 else (ATT + 2 * KVW) + (i - ATT // 256) * 256

    P = Prog(nc)
    gst = ExitStack()

    def gsb(name, shape, dt):
        return gst.enter_context(nc.sbuf_tensor(name, shape, dt))

    psall = gst.enter_context(nc.psum_tensor("psall", [128, 4096], F32))
    ps = [psall[:, i * 512:(i + 1) * 512] for i in range(8)]
    ident = gsb("identS", [128, 128], BF16)
    identf = gsb("identfS", [128, 128], F32)
    ones = gsb("onesS", [128, 128], BF16)
    onesf = gsb("onesfS", [128, 128], F32)
    epsT = gsb("epsT", [128, 1], F32)
    consts = ["ident", "identf", "ones", "onesf", "eps"]

    def load_consts():
        P.dma("sp", ident[:], ident_d, w=["ident"])
        P.dma("sp", identf[:], identf_d, w=["identf"])
        P.add("dve", lambda e: e.memset(ones[:], 1.0), w=["ones"])
        P.add("dve", lambda e: e.memset(onesf[:], 1.0), w=["onesf"])
        P.add("dve", lambda e: e.memset(epsT[:], EPS), w=["eps"])

    def phase_norm_T(src, gain_d, dst, ntiles, tag, extra=None):
        with ExitStack() as st:
            extra_fn = extra(st) if extra is not None else None
            def sb(name, shape, dt):
                return st.enter_context(nc.sbuf_tensor(tag + name, shape, dt))
            gB = sb("gB", [128, D], F32)
            P.dma("sp", gB[:], gain_d.partition_broadcast(128), w=["gB"])
            xt = Ring(st, nc, tag + "xt", 2, [128, D], F32)
            junk = Ring(st, nc, tag + "junk", 2, [128, D], BF16)
            ss = Ring(st, nc, tag + "ss", 2, [128, 1], F32)
            hb = Ring(st, nc, tag + "hb", 2, [128, D], BF16)
            hTt = Ring(st, nc, tag + "hTt", 2, [128, NCH, 128], BF16)
            bi = 0
            for t in range(ntiles):
                xtt, xk = xt.next()
                jt, jk = junk.next()
                sst, sk = ss.next()
                hbt, hk = hb.next()
                htt, htk = hTt.next()
                P.dma("sp", xtt[:], src[t * 128:(t + 1) * 128, :], w=[xk])
                P.add("dve", lambda e, a=jt, b=xtt, s=sst: e.scalar_tensor_tensor(
                    out=a[:], in0=b[:], scalar=1.0, in1=b[:], op0=ALU.mult, op1=ALU.mult, accum_out=s[:]),
                    r=[xk], w=[jk, sk])
                P.add("act", lambda e, s=sst: e.activation(out=s[:], in_=s[:], func=AF.Sqrt, scale=1.0 / D,
                                                           bias=epsT[:, 0:1]), r=[sk, "eps"], w=[sk])
                P.add("dve", lambda e, s=sst: e.reciprocal(out=s[:], in_=s[:]), r=[sk], w=[sk])
                P.add("dve", lambda e, a=hbt, b=xtt, s=sst: e.scalar_tensor_tensor(
                    out=a[:], in0=b[:], scalar=s[:, 0:1], in1=gB[:], op0=ALU.mult, op1=ALU.mult),
                    r=[xk, sk, "gB"], w=[hk])
                for c0 in range(0, NCH, 8):
                    bk = bi % 4
                    bi += 1
                    pb = ps[bk][:].bitcast(BF16)

                    def tr(e, hbt=hbt, c0=c0, pb=pb):
                        ins = None
                        for cc in range(c0, c0 + 8):
                            ins = e.transpose(out=pb[:, (cc - c0) * 128:(cc - c0 + 1) * 128],
                                              in_=hbt[:, cc * 128:(cc + 1) * 128], identity=ident[:])
                        return ins
                    P.add("pe", tr, r=[hk, "ident"], w=[f"ps{bk}"])
                    P.add("act", lambda e, htt=htt, c0=c0, pb=pb: e.copy(
                        out=htt[:, c0:c0 + 8, :], in_=pb.rearrange("p (c t) -> p c t", c=8)),
                        r=[f"ps{bk}"], w=[htk])
                P.dma("act", dst[t], htt[:], r=[htk], w=[tag + "dst"])
                if extra_fn is not None:
                    extra_fn(t, ntiles)
            P.flush()

    def make_rope(st, tag):
        tmp = dict(
            qg=Ring(st, nc, tag + "qg", 2, [128, 512], BF16),
            sq=Ring(st, nc, tag + "sq", 2, [128, 512], BF16),
            rst=Ring(st, nc, tag + "rst", 2, [128, 512], F32),
            t1=Ring(st, nc, tag + "t1", 2, [128, 512], F32),
            t2=Ring(st, nc, tag + "t2", 2, [128, 512], F32),
        )
        return tmp

    def rope_epilogue(tmp, pq, pqk, gcol, Ct, Ck, St, Sk, perm, outt, outk, b1, b2):
        qg, qgk = tmp["qg"].next()
        sq, sqk = tmp["sq"].next()
        rst, rsk = tmp["rst"].next()
        t1, t1k = tmp["t1"].next()
        t2, t2k = tmp["t2"].next()
        P.add("dve", lambda e: e.tensor_scalar(out=qg[:], in0=pq, scalar1=gcol, scalar2=None, op0=ALU.mult),
              r=[pqk, "qkg"], w=[qgk])
        P.add("act", lambda e: e.activation(out=sq[:], in_=pq, func=AF.Square), r=[pqk], w=[sqk])
        P.add("pe", lambda e: e.matmul(ps[b1][:], ones[:], sq[:], start=True, stop=True), r=["ones", sqk], w=[f"ps{b1}"])
        P.add("pe", lambda e: e.matmul(ps[b2][:], perm[:], qg[:], start=True, stop=True), r=["perm", qgk], w=[f"ps{b2}"])
        P.add("act", lambda e: e.activation(out=rst[:], in_=ps[b1][:], func=AF.Sqrt, scale=1.0 / 128, bias=epsT[:, 0:1]),
              r=[f"ps{b1}", "eps"], w=[rsk])
        P.add("dve", lambda e: e.reciprocal(out=rst[:], in_=rst[:]), r=[rsk], w=[rsk])
        P.add("dve", lambda e: e.tensor_tensor(out=t1[:], in0=qg[:], in1=Ct, op=ALU.mult), r=[qgk, Ck], w=[t1k])
        P.add("dve", lambda e: e.tensor_tensor(out=t2[:], in0=ps[b2][:], in1=St, op=ALU.mult), r=[f"ps{b2}", Sk], w=[t2k])
        P.add("dve", lambda e: e.tensor_tensor(out=t1[:], in0=t1[:], in1=t2[:], op=ALU.add), r=[t1k, t2k], w=[t1k])
        P.add("dve", lambda e: e.tensor_tensor(out=outt, in0=t1[:], in1=rst[:], op=ALU.mult), r=[t1k, rsk], w=[outk])

    def phase_kv():
        with ExitStack() as st:
            def sb(name, shape, dt):
                return st.enter_context(nc.sbuf_tensor("kv" + name, shape, dt))
            Wk = sb("Wk", [128, NCH, KVW], BF16)
            Wv = sb("Wv", [128, NCH, KVW], BF16)
            perm = sb("perm", [128, 128], BF16)
            qkg = sb("qkg", [128, 2], F32)
            P.dma("sp", perm[:], perm_d, w=["perm"])
            P.dma("sp", qkg[:], qk_g, w=["qkg"])
            P.dma("pool", Wk[:], w_in[:, ATT:ATT + KVW].rearrange("(c p) n -> p c n", p=128), w=["Wk"])
            P.dma("pool", Wv[:], w_in[:, ATT + KVW:ATT + 2 * KVW].rearrange("(c p) n -> p c n", p=128), w=["Wv"])
            hg = Ring(st, nc, "kvhg", 2, [128, 4, NCH, 128], BF16)
            Cr = Ring(st, nc, "kvC", 2, [128, 512], F32)
            Sr = Ring(st, nc, "kvS", 2, [128, 512], F32)
            ktr = Ring(st, nc, "kvkt", 2, [128, 512], BF16)
            vtr = Ring(st, nc, "kvvt", 2, [128, 4, KVW], BF16)
            tmp = make_rope(st, "kv")
            cvr = Ring(st, nc, "kvcv", 2, [128, NCH, 256], BF16)
            ngr = SB // 512
            cv_i = 0
            cv_prev = None
            bi = 0
            for tg in range(SB // 512):
                while cv_i < NWT and cv_i * ngr < (tg + 1) * NWT:
                    cvt, cvk = cvr.next()
                    c0_ = wb_col0(cv_i)
                    P.dma("pool", cvt[:], w_in[:, c0_:c0_ + 256].rearrange("(c p) n -> p c n", p=128), w=[cvk])
                    if cv_prev is not None:
                        P.dma("pool", WB[cv_prev[0]], cv_prev[1][:], r=[cv_prev[2]], w=["WBd"])
                    cv_prev = (cv_i, cvt, cvk)
                    cv_i += 1
                hgt, hk = hg.next()
                Ct, Ck = Cr.next()
                St, Sk = Sr.next()
                P.dma("sp", hgt[:], hT[tg * 4:(tg + 1) * 4].rearrange("t p c k -> p t c k"), w=[hk])
                P.dma("sp", Ct[:], cosT[:, tg * 512:(tg + 1) * 512], w=[Ck])
                P.dma("sp", St[:], sinT[:, tg * 512:(tg + 1) * 512], w=[Sk])
                for j in range(NKV):
                    b = bi % 2
                    bi += 1
                    mm_group(P, ps[b][:].rearrange("p (t k) -> p t k", t=4),
                             [(Wk[:, cc, j * 128:(j + 1) * 128], hgt[:, :, cc, :]) for cc in range(NCH)],
                             r=["Wk", hk], w=[f"ps{b}"])
                    kt, kk = ktr.next()
                    rope_epilogue(tmp, ps[b][:], f"ps{b}", qkg[:, 1:2], Ct[:], Ck, St[:], Sk, perm, kt[:], kk, 2 + b, 4 + b)
                    P.dma("act", KT[j, :, tg * 512:(tg + 1) * 512], kt[:], r=[kk], w=["KTd"])
                vt, vk = vtr.next()
                for s in range(4):
                    b = 6 + (s % 2)
                    mm_group(P, ps[b][:, 0:KVW], [(hgt[:, s, cc, :], Wv[:, cc, :]) for cc in range(NCH)],
                             r=["Wv", hk], w=[f"ps{b}"])
                    P.add("act", lambda e, vt=vt, s=s, b=b: e.copy(out=vt[:, s, :], in_=ps[b][:, 0:KVW]),
                          r=[f"ps{b}"], w=[vk])
                for j in range(NKV):
                    P.dma("act", Vs[j, :, tg * 4:(tg + 1) * 4, :], vt[:, :, j * 128:(j + 1) * 128], r=[vk], w=["Vd"])
            if cv_prev is not None:
                P.dma("pool", WB[cv_prev[0]], cv_prev[1][:], r=[cv_prev[2]], w=["WBd"])
            P.flush()

    def phase_qc():
        with ExitStack() as st:
            def sb(name, shape, dt):
                return st.enter_context(nc.sbuf_tensor("qc" + name, shape, dt))
            perm = sb("perm", [128, 128], BF16)
            qkg = sb("qkg", [128, 2], F32)
            cw = sb("cw", [128, NCB, 3], F32)
            cog = sb("cog", [128, NCB], F32)
            edge = sb("edge", [128, 2], F32)
            P.dma("sp", perm[:], perm_d, w=["perm"])
            P.dma("sp", qkg[:], qk_g, w=["qkg"])
            P.dma("sp", cw[:], conv_w, w=["cw"])
            P.dma("sp", cog[:], co_g, w=["cog"])
            P.dma("sp", edge[:], edge_d, w=["edge"])
            hgt = sb("hg", [128, 4, NCH, 128], BF16)
            hL = sb("hL", [128, NCH, 128], BF16)
            hR = sb("hR", [128, NCH, 128], BF16)
            hH = sb("hH", [128, NCH, 2], BF16)
            Ct = sb("C", [128, 512], F32)
            St = sb("S", [128, 512], F32)
            Wr = Ring(st, nc, "qcW", 6, [128, NCH, 256], BF16)
            qtr = Ring(st, nc, "qcqt", 2, [128, 512], BF16)
            tmp = make_rope(st, "qc")
            gcS = Ring(st, nc, "qcgc", 2, [128, 512], F32)
            hS = Ring(st, nc, "qchS", 2, [128, 4], F32)
            ur = Ring(st, nc, "qcu", 2, [128, 514], F32)
            vr = Ring(st, nc, "qcv", 2, [128, 512], F32)
            ycr = Ring(st, nc, "qcyc", 2, [128, 512], F32)
            sqr = Ring(st, nc, "qcsqc", 2, [128, 512], F32)
            ybr = Ring(st, nc, "qcyb", 2, [128, 512], BF16)
            ssc = sb("ssc", [128, 512], F32)
            stc = sb("stc", [128, NTO], F32)
            bi = 0

            def wload(col0):
                Wt, Wk_ = Wr.next()
                wi = col0 // 256 if col0 < ATT else ATT // 256 + (col0 - (ATT + 2 * KVW)) // 256
                P.dma("pool", Wt[:], WB[wi], w=[Wk_])
                return Wt, Wk_

            for tg in range(OWN // 512):
                P.dma("sp", hgt[:], hT[tg * 4:(tg + 1) * 4].rearrange("t p c k -> p t c k"), w=["hg"])
                P.dma("sp", hL[:], hT[(tg * 4 - 1) % NT], w=["hL"])
                P.dma("sp", hR[:], hT[(tg * 4 + 4) % NT], w=["hR"])
                P.dma("sp", Ct[:], cosT[:, tg * 512:(tg + 1) * 512], w=["C"])
                P.dma("sp", St[:], sinT[:, tg * 512:(tg + 1) * 512], w=["S"])
                P.add("act", lambda e: e.copy(out=hH[:, :, 0:1], in_=hL[:, :, 127:128]), r=["hL"], w=["hH"])
                P.add("act", lambda e: e.copy(out=hH[:, :, 1:2], in_=hR[:, :, 0:1]), r=["hR", "hH"], w=["hH"])
                for q4 in range(ATT // 256):
                    Wt, Wk_ = wload(q4 * 256)
                    for hh in range(2):
                        h = q4 * 2 + hh
                        b = bi % 2
                        bi += 1
                        mm_group(P, ps[b][:].rearrange("p (t k) -> p t k", t=4),
                                 [(Wt[:, cc, hh * 128:(hh + 1) * 128], hgt[:, :, cc, :]) for cc in range(NCH)],
                                 r=[Wk_, "hg"], w=[f"ps{b}"])
                        qt, qk_ = qtr.next()
                        rope_epilogue(tmp, ps[b][:], f"ps{b}", qkg[:, 0:1], Ct[:], "C", St[:], "S", perm, qt[:], qk_, 2 + b, 4 + b)
                        P.dma("act", QT[h, :, tg * 512:(tg + 1) * 512], qt[:], r=[qk_], w=["QTd"])
                c0 = ATT + 2 * KVW
                for c4 in range(CW // 256):
                    Wx, Wxk = wload(c0 + c4 * 256)
                    Wb, Wbk = wload(c0 + CW + c4 * 256)
                    Wc, Wck = wload(c0 + 2 * CW + c4 * 256)
                    for hh in range(2):
                        cb = c4 * 2 + hh
                        cols = slice(hh * 128, (hh + 1) * 128)
                        for (bk, Wt, Wk_) in ((0, Wx, Wxk), (1, Wc, Wck), (2, Wb, Wbk)):
                            mm_group(P, ps[bk][:].rearrange("p (t k) -> p t k", t=4),
                                     [(Wt[:, cc, cols], hgt[:, :, cc, :]) for cc in range(NCH)],
                                     r=[Wk_, "hg"], w=[f"ps{bk}"])
                        mm_group(P, ps[3][:, 0:2], [(Wx[:, cc, cols], hH[:, cc, :]) for cc in range(NCH)],
                                 r=[Wxk, "hH"], w=["ps3"])
                        mm_group(P, ps[3][:, 2:4], [(Wc[:, cc, cols], hH[:, cc, :]) for cc in range(NCH)],
                                 r=[Wck, "hH", "ps3"], w=["ps3"])
                        gc, gck = gcS.next()
                        hs, hsk = hS.next()
                        u, uk = ur.next()
                        v, vk = vr.next()
                        yc, yck = ycr.next()
                        sq, sqk = sqr.next()
                        yb, ybk = ybr.next()
                        P.add("act", lambda e, gc=gc: e.copy(out=gc[:], in_=ps[1][:]), r=["ps1"], w=[gck])
                        P.add("act", lambda e, hs=hs: e.copy(out=hs[:], in_=ps[3][:, 0:4]), r=["ps3"], w=[hsk])
                        P.add("dve", lambda e, u=u, gc=gc: e.tensor_tensor(out=u[:, 1:513], in0=ps[0][:], in1=gc[:], op=ALU.mult),
                              r=["ps0", gck], w=[uk])
                        if tg == 0:
                            P.add("dve", lambda e, u=u, hs=hs: e.scalar_tensor_tensor(
                                out=u[:, 0:1], in0=hs[:, 0:1], scalar=edge[:, 0:1], in1=hs[:, 2:3], op0=ALU.mult, op1=ALU.mult),
                                r=[hsk, "edge", uk], w=[uk])
                        else:
                            P.add("dve", lambda e, u=u, hs=hs: e.tensor_tensor(
                                out=u[:, 0:1], in0=hs[:, 0:1], in1=hs[:, 2:3], op=ALU.mult), r=[hsk, uk], w=[uk])
                        if tg == OWN // 512 - 1:
                            P.add("dve", lambda e, u=u, hs=hs: e.scalar_tensor_tensor(
                                out=u[:, 513:514], in0=hs[:, 1:2], scalar=edge[:, 1:2], in1=hs[:, 3:4], op0=ALU.mult, op1=ALU.mult),
                                r=[hsk, "edge", uk], w=[uk])
                        else:
                            P.add("dve", lambda e, u=u, hs=hs: e.tensor_tensor(
                                out=u[:, 513:514], in0=hs[:, 1:2], in1=hs[:, 3:4], op=ALU.mult), r=[hsk, uk], w=[uk])
                        P.add("dve", lambda e, u=u, v=v, cb=cb: e.tensor_scalar(
                            out=v[:], in0=u[:, 0:512], scalar1=cw[:, cb, 0:1], scalar2=None, op0=ALU.mult),
                            r=[uk, "cw"], w=[vk])
                        P.add("dve", lambda e, u=u, v=v, cb=cb: e.scalar_tensor_tensor(
                            out=v[:], in0=u[:, 1:513], scalar=cw[:, cb, 1:2], in1=v[:], op0=ALU.mult, op1=ALU.add),
                            r=[uk, "cw", vk], w=[vk])
                        P.add("dve", lambda e, u=u, v=v, cb=cb: e.scalar_tensor_tensor(
                            out=v[:], in0=u[:, 2:514], scalar=cw[:, cb, 2:3], in1=v[:], op0=ALU.mult, op1=ALU.add),
                            r=[uk, "cw", vk], w=[vk])
                        P.add("dve", lambda e, v=v, yc=yc: e.tensor_tensor(out=yc[:], in0=ps[2][:], in1=v[:], op=ALU.mult),
                              r=["ps2", vk], w=[yck])
                        P.add("act", lambda e, sq=sq, yc=yc: e.activation(out=sq[:], in_=yc[:], func=AF.Square), r=[yck], w=[sqk])
                        if cb == 0:
                            P.add("pool", lambda e, sq=sq: e.tensor_copy(out=ssc[:], in_=sq[:]), r=[sqk], w=["ssc"])
                        else:
                            P.add("pool", lambda e, sq=sq: e.tensor_tensor(out=ssc[:], in0=ssc[:], in1=sq[:], op=ALU.add),
                                  r=[sqk, "ssc"], w=["ssc"])
                        P.add("dve", lambda e, yb=yb, yc=yc, cb=cb: e.tensor_scalar(
                            out=yb[:], in0=yc[:], scalar1=cog[:, cb:cb + 1], scalar2=None, op0=ALU.mult),
                            r=[yck, "cog"], w=[ybk])
                        P.dma("act", YT[NQH + cb, :, tg * 512:(tg + 1) * 512], yb[:], r=[ybk], w=["YTd"])
                for s in range(4):
                    P.add("pe", lambda e, s=s: e.matmul(ps[4][:, s:s + 1], ssc[:, s * 128:(s + 1) * 128], onesf[:, 0:1],
                                                        start=True, stop=True), r=["ssc", "onesf"], w=["ps4"])
                P.add("act", lambda e, tg=tg: e.copy(out=stc[:, tg * 4:(tg + 1) * 4], in_=ps[4][:, 0:4]), r=["ps4"], w=["stc"])
            P.dma("sp", STA[:, NTO:2 * NTO], stc[:], r=["stc"], w=["STAd"])
            P.flush()

    def phase_attn():
        with ExitStack() as st:
            def sb(name, shape, dt):
                return st.enter_context(nc.sbuf_tensor("at" + name, shape, dt))
            NKB = SB // 128
            aog = sb("aog", [128, NQH], F32)
            P.dma("sp", aog[:], ao_g, w=["aog"])
            Kr = Ring(st, nc, "atK", 2, [128, SB], BF16)
            Vr = Ring(st, nc, "atV", 2, [128, NKB, 128], BF16)
            Qr = Ring(st, nc, "atQ", 2, [128, 512], BF16)
            Pr = Ring(st, nc, "atP", 6, [128, 1024], BF16)
            owr = Ring(st, nc, "atow", 2, [128, 512], F32)
            d0r = Ring(st, nc, "atd0", 2, [128, 1024], F32)
            d1r = Ring(st, nc, "atd1", 2, [128, 1024], F32)
            rdr = Ring(st, nc, "atrd", 2, [128, 512], F32)
            orr = Ring(st, nc, "ato", 2, [128, 512], F32)
            sqr = Ring(st, nc, "atsq", 2, [128, 512], F32)
            ybr = Ring(st, nc, "atyb", 2, [128, 512], BF16)
            ssa = sb("ssa", [128, OWN], F32)
            sta = sb("sta", [128, NTO], F32)
            scale = 128.0 ** -0.5
            NKP = NKB // 2
            items = [(j, tg, g) for j in range(NKV) for tg in range(OWN // 512) for g in range(GQ)]
            kv = {}

            def load_kv(j):
                Kt, Kk = Kr.next()
                Vt, Vk = Vr.next()
                P.dma("sp", Kt[:], KT[j], w=[Kk])
                P.dma("sp", Vt[:], Vs[j], w=[Vk])
                kv[j] = (Kt, Kk, Vt, Vk)

            qs = {}

            def load_q(i):
                j, tg, g = items[i]
                Qt, Qk = Qr.next()
                P.dma("sp", Qt[:], QT[j * GQ + g, :, tg * 512:(tg + 1) * 512], w=[Qk])
                qs[i] = (Qt, Qk)

            def s_mm(i, kp):
                j = items[i][0]
                Kt, Kk = kv[j][0], kv[j][1]
                Qt, Qk = qs[i]
                b0 = 2 * ((i * NKP + kp) % 3)

                def fn(e, kp=kp, b0=b0, Kt=Kt, Qt=Qt):
                    ins = None
                    for u in range(2):
                        kb = kp * 2 + u
                        ins = e.matmul(ps[b0 + u][:], Kt[:, kb * 128:(kb + 1) * 128], Qt[:], start=True, stop=True)
                    return ins
                P.add("pe", fn, r=[Kk, Qk], w=[f"ps{b0}", f"ps{b0 + 1}"])

            EPI_KP = min(5, NKP - 1)
            pend_epi = []
            load_kv(0)
            load_q(0)
            s_mm(0, 0)
            s_mm(0, 1)
            for i, (j, tg, g) in enumerate(items):
                h = j * GQ + g
                Kt, Kk, Vt, Vk = kv[j]
                if i + 1 < len(items):
                    if items[i + 1][0] != j:
                        load_kv(items[i + 1][0])
                    load_q(i + 1)
                bo = 6
                bd = 7
                d0, d0k = d0r.next()
                d1, d1k = d1r.next()
                pe_den_started = False
                inited = set()
                for kp in range(NKP):
                    if kp == EPI_KP and pend_epi:
                        pend_epi.pop(0)()
                    if kp + 2 < NKP:
                        s_mm(i, kp + 2)
                    elif i + 1 < len(items):
                        s_mm(i + 1, kp + 2 - NKP)
                    b0 = 2 * ((i * NKP + kp) % 3)
                    pt, pk = Pr.next()
                    P.add("act", lambda e, pt=pt, b0=b0: e.activation(
                        out=pt[:], in_=psall[:, b0 * 512:(b0 + 2) * 512], func=AF.Exp, scale=scale),
                        r=[f"ps{b0}", f"ps{b0 + 1}"], w=[pk])

                    def pv(e, pt=pt, kp=kp, bo=bo, Vt=Vt):
                        ins = None
                        for u in range(2):
                            kb = kp * 2 + u
                            ins = e.matmul(ps[bo][:], Vt[:, kb, :], pt[:, u * 512:(u + 1) * 512],
                                           start=(kb == 0), stop=(kb == NKB - 1))
                        return ins
                    P.add("pe", pv, r=[Vk, pk], w=[f"ps{bo}"])
                    who = ("dve", "pool", "dve", "dve", "pool", "dve", "pe", "pe")[kp % 8]
                    if who == "pe":
                        def dn(e, pt=pt, bd=bd, first=(not pe_den_started)):
                            ins = None
                            for u in range(2):
                                ins = e.matmul(ps[bd][:], ones[:], pt[:, u * 512:(u + 1) * 512],
                                               start=(first and u == 0), stop=False)
                            return ins
                        P.add("pe", dn, r=["ones", pk], w=[f"ps{bd}"])
                        pe_den_started = True
                    else:
                        eng, dd, ddk = ("dve", d0, d0k) if who == "dve" else ("pool", d1, d1k)
                        if ddk not in inited:
                            inited.add(ddk)
                            P.add(eng, lambda e, dd=dd, pt=pt: e.tensor_copy(out=dd[:], in_=pt[:]), r=[pk], w=[ddk])
                        else:
                            P.add(eng, lambda e, dd=dd, pt=pt: e.tensor_tensor(out=dd[:], in0=dd[:], in1=pt[:], op=ALU.add),
                                  r=[pk, ddk], w=[ddk])

                def dfin(e, d0=d0, d1=d1, bd=bd, st0=(not pe_den_started)):
                    ins = None
                    srcs = [d0[:, 0:512], d0[:, 512:1024], d1[:, 0:512], d1[:, 512:1024]]
                    for i_, sr in enumerate(srcs):
                        ins = e.matmul(ps[bd][:], onesf[:], sr, start=(st0 and i_ == 0), stop=(i_ == 3))
                    return ins
                ow, owk = owr.next()
                P.add("act", lambda e, ow=ow, bo=bo: e.copy(out=ow[:], in_=ps[bo][:]), r=[f"ps{bo}"], w=[owk])
                def epi(dfin=dfin, d0k=d0k, d1k=d1k, bd=bd, ow=ow, owk=owk, h=h, tg=tg):
                    P.add("pe", dfin, r=["onesf", d0k, d1k], w=[f"ps{bd}"])
                    rd, rdk = rdr.next()
                    o, ok_ = orr.next()
                    sq, sqk = sqr.next()
                    yb, ybk = ybr.next()
                    P.add("dve", lambda e, rd=rd, bd=bd: e.reciprocal(out=rd[:], in_=ps[bd][:]), r=[f"ps{bd}"], w=[rdk])
                    P.add("dve", lambda e, o=o, rd=rd, ow=ow: e.tensor_tensor(out=o[:], in0=ow[:], in1=rd[:], op=ALU.mult),
                          r=[owk, rdk], w=[ok_])
                    P.add("act", lambda e, sq=sq, o=o: e.activation(out=sq[:], in_=o[:], func=AF.Square), r=[ok_], w=[sqk])
                    sl = slice(tg * 512, (tg + 1) * 512)
                    if h == 0:
                        P.add("pool", lambda e, sq=sq, sl=sl: e.tensor_copy(out=ssa[:, sl], in_=sq[:]), r=[sqk], w=[f"ssa{tg}"])
                    else:
                        P.add("pool", lambda e, sq=sq, sl=sl: e.tensor_tensor(out=ssa[:, sl], in0=ssa[:, sl], in1=sq[:], op=ALU.add),
                              r=[sqk, f"ssa{tg}"], w=[f"ssa{tg}"])
                    P.add("dve", lambda e, yb=yb, o=o, h=h: e.tensor_scalar(
                        out=yb[:], in0=o[:], scalar1=aog[:, h:h + 1], scalar2=None, op0=ALU.mult), r=[ok_, "aog"], w=[ybk])
                    P.dma("act", YT[h, :, sl], yb[:], r=[ybk], w=["YTd"])
                pend_epi.append(epi)
            while pend_epi:
                pend_epi.pop(0)()
            for t in range(NTO):
                P.add("pe", lambda e, t=t: e.matmul(ps[7][:, t:t + 1], ssa[:, t * 128:(t + 1) * 128], onesf[:, 0:1],
                                                    start=True, stop=True), r=[f"ssa{t // 4}", "onesf"], w=["ps7"])
            P.add("act", lambda e: e.copy(out=sta[:], in_=ps[7][:, 0:NTO]), r=["ps7"], w=["sta"])
            P.dma("sp", STA[:, 0:NTO], sta[:], r=["sta"], w=["STAd"])
            P.flush()

    def phase_out():
        with ExitStack() as st:
            def sb(name, shape, dt):
                return st.enter_context(nc.sbuf_tensor("op" + name, shape, dt))
            rs = sb("rs", [128, 2 * NTO], F32)
            P.dma("sp", rs[:], STA, w=["rs"])
            P.add("act", lambda e: e.activation(out=rs[:], in_=rs[:], func=AF.Sqrt, scale=1.0 / ATT, bias=epsT[:, 0:1]),
                  r=["rs", "eps"], w=["rs"])
            P.add("dve", lambda e: e.reciprocal(out=rs[:], in_=rs[:]), r=["rs"], w=["rs"])
            ygr = Ring(st, nc, "opyg", 2, [128, NYC, 512], BF16)
            Wr = Ring(st, nc, "opW", 2, [128, NYC, 512], BF16)
            xr = Ring(st, nc, "opx", 4, [128, 512], F32)
            tr_ = Ring(st, nc, "opt", 2, [128, 512], F32)
            orr = Ring(st, nc, "opo", 3, [128, 512], F32)
            bi = 0
            for dc in range(D // 512):
                Wt, Wk_ = Wr.next()
                P.dma("pool", Wt[:], w_out[:, dc * 512:(dc + 1) * 512].rearrange("(c p) n -> p c n", p=128), w=[Wk_])
                for tg in range(OWN // 512):
                    yg, ygk = ygr.next()
                    P.dma("sp", yg[:], YT[:, :, tg * 512:(tg + 1) * 512].rearrange("c p t -> p c t"), w=[ygk])
                    for s in range(4):
                        ba = (bi % 4) * 2
                        bi += 1
                        t_ = tg * 4 + s
                        xp, xk = xr.next()
                        P.dma("sp", xp[:], x[t_ * 128:(t_ + 1) * 128, dc * 512:(dc + 1) * 512], w=[xk])
                        mm_group(P, ps[ba][:], [(yg[:, cc, s * 128:(s + 1) * 128], Wt[:, cc, :]) for cc in range(NQH)],
                                 r=[ygk, Wk_], w=[f"ps{ba}"])
                        mm_group(P, ps[ba + 1][:], [(yg[:, cc, s * 128:(s + 1) * 128], Wt[:, cc, :]) for cc in range(NQH, NYC)],
                                 r=[ygk, Wk_], w=[f"ps{ba + 1}"])
                        tt, tk = tr_.next()
                        ot, ok_ = orr.next()
                        P.add("dve", lambda e, tt=tt, ba=ba, t_=t_, xp=xp: e.scalar_tensor_tensor(
                            out=tt[:], in0=ps[ba][:], scalar=rs[:, t_:t_ + 1], in1=xp[:], op0=ALU.mult, op1=ALU.add),
                            r=[f"ps{ba}", "rs", xk], w=[tk])
                        P.add("dve", lambda e, tt=tt, ot=ot, ba=ba, t_=t_: e.scalar_tensor_tensor(
                            out=ot[:], in0=ps[ba + 1][:], scalar=rs[:, NTO + t_:NTO + t_ + 1], in1=tt[:], op0=ALU.mult, op1=ALU.add),
                            r=[f"ps{ba + 1}", "rs", tk], w=[ok_])
                        P.dma("act", X1[t_ * 128:(t_ + 1) * 128, dc * 512:(dc + 1) * 512], ot[:], r=[ok_], w=["X1d"])
            P.flush()

    def wdt_prep(st):
        HC = max(NCH // 2, 1)
        HD = HC * 128
        NPJ = NCH // HC
        Wr = Ring(st, nc, "ppW", 2, [128, HD], BF16)
        Wfr = Ring(st, nc, "ppWf", 2, [128, HD], F32)
        Tr = Ring(st, nc, "ppT", 2, [128, HC, 128], BF16)
        wdv = w_down.rearrange("(i j) d -> j i d", j=128)
        state = {"bi": 0, "n": 0}

        def one(n, eng):
            j, hp = n // NPJ, n % NPJ
            Wt, Wk_ = Wr.next()
            Tt, Tk = Tr.next()
            Wf, Wfk = Wfr.next()
            P.dma("sp", Wf[:], wdv[j][:, hp * HD:(hp + 1) * HD], w=[Wfk])
            if eng == "pool":
                P.add("pool", lambda e, Wt=Wt, Wf=Wf: e.tensor_copy(out=Wt[:], in_=Wf[:]), r=[Wfk], w=[Wk_])
            else:
                P.add("act", lambda e, Wt=Wt, Wf=Wf: e.copy(out=Wt[:], in_=Wf[:]), r=[Wfk], w=[Wk_])
            for c0 in range(0, HC, 8):
                nb = min(8, HC - c0)
                bk = (4, 7)[state["bi"] % 2]
                state["bi"] += 1
                pb = ps[bk][:].bitcast(BF16)

                def tr(e, Wt=Wt, c0=c0, pb=pb, nb=nb):
                    ins = None
                    for cc in range(c0, c0 + nb):
                        ins = e.transpose(out=pb[:, (cc - c0) * 128:(cc - c0 + 1) * 128],
                                          in_=Wt[:, cc * 128:(cc + 1) * 128], identity=ident[:])
                    return ins
                P.add("pe", tr, r=[Wk_, "ident"], w=[f"ps{bk}"])
                P.add("act", lambda e, Tt=Tt, c0=c0, pb=pb, nb=nb: e.copy(
                    out=Tt[:, c0:c0 + nb, :], in_=pb[:, 0:nb * 128].rearrange("p (c t) -> p c t", c=nb)), r=[f"ps{bk}"], w=[Tk])
            P.dma("act", WDT[j][:, hp * HC:(hp + 1) * HC, :], Tt[:], r=[Tk], w=["WDTd"])

        total = 128 * NPJ

        def step(k, nsteps, eng):
            hi_ = (total * (k + 1)) // nsteps
            while state["n"] < hi_:
                one(state["n"], eng)
                state["n"] += 1
        return step

    def phase_peer_prep():
        with ExitStack() as st:
            def sb(name, shape, dt):
                return st.enter_context(nc.sbuf_tensor("pq" + name, shape, dt))
            hgr = Ring(st, nc, "pqhg", 2, [128, 4, NCH, 128], BF16)
            Wr = Ring(st, nc, "pqW", 2, [128, NCH, 512], BF16)
            qr = Ring(st, nc, "pqq", 3, [128, 512], BF16)
            bi = 0
            for q4 in range(PQ // 512):
                Wt, Wk_ = Wr.next()
                P.dma("pool", Wt[:], w_query[:, q4 * 512:(q4 + 1) * 512].rearrange("(c p) n -> p c n", p=128), w=[Wk_])
                for tg in range(OWN // 512):
                    hgt, hgk = hgr.next()
                    P.dma("sp", hgt[:], H2T[tg * 4:(tg + 1) * 4].rearrange("t p c k -> p t c k"), w=[hgk])
                    for hh in range(4):
                        b = bi % 4
                        bi += 1
                        mm_group(P, ps[b][:].rearrange("p (t k) -> p t k", t=4),
                                 [(Wt[:, cc, hh * 128:(hh + 1) * 128], hgt[:, :, cc, :]) for cc in range(NCH)],
                                 r=[Wk_, hgk], w=[f"ps{b}"])
                        qt, qk_ = qr.next()
                        P.add("act", lambda e, qt=qt, b=b: e.copy(out=qt[:], in_=ps[b][:]), r=[f"ps{b}"], w=[qk_])
                        P.dma("act", QPT[q4 * 4 + hh, :, tg * 512:(tg + 1) * 512], qt[:], r=[qk_], w=["QPTd"])
            P.flush()

    def phase_peer_pick():
        with ExitStack() as st:
            def sb(name, shape, dt):
                return st.enter_context(nc.sbuf_tensor("pk" + name, shape, dt))
            kn = sb("kn", [128, 16, 128], BF16)
            kT = sb("kT", [128, 16, 128], BF16)
            iot = sb("iot", [128, 128], F32)
            P.dma("pool", kn[:], sub_keys.rearrange("g n k -> n g k"), w=["kn"])
            P.dma("sp", iot[:], iota_d, w=["iot"])
            for g0 in range(0, 16, 8):
                pb = ps[0][:].bitcast(BF16)

                def tr(e, g0=g0, pb=pb):
                    ins = None
                    for g in range(g0, g0 + 8):
                        ins = e.transpose(out=pb[:, (g - g0) * 128:(g - g0 + 1) * 128], in_=kn[:, g, :], identity=ident[:])
                    return ins
                P.add("pe", tr, r=["kn", "ident"], w=["ps0"])
                P.add("act", lambda e, g0=g0, pb=pb: e.copy(out=kT[:, g0:g0 + 8, :], in_=pb.rearrange("p (c t) -> p c t", c=8)),
                      r=["ps0"], w=["kT"])
            qpr = Ring(st, nc, "pkqp", 2, [128, 16, 128], BF16)
            S = sb("S", [128, 16, 128], F32)
            S2 = sb("S2", [128, 16, 128], F32)
            V = sb("V", [128, 16, 16], F32)
            IX = sb("IX", [128, 16, 16], U32)
            IXf = sb("IXf", [128, 16, 16], F32)
            cand = sb("cand", [128, 8, 256], F32)
            cand2 = sb("cand2", [128, 8, 256], F32)
            Bv = sb("Bv", [128, 8, 16], F32)
            PX = sb("PX", [128, 8, 16], U32)
            R1u = sb("R1u", [128, 8, 16], U32)
            R2u = sb("R2u", [128, 8, 16], U32)
            R1 = sb("R1", [128, 8, 16], F32)
            R2 = sb("R2", [128, 8, 16], F32)
            E = sb("E", [128, 8, 16], F32)
            Z = sb("Z", [128, 8], F32)
            eq = sb("eq", [128, 8, 16, 16], F32)
            PI = sb("PI", [128, 128], F32)
            PJ = sb("PJ", [128, 128], F32)
            PG = sb("PG", [128, 128], F32)
            PT = sb("PT", [128, 3, 128], F32)
            OI = sb("OI", [128, 128, 128], BF16)
            OJ = sb("OJ", [128, 128, 128], BF16)
            Gr = Ring(st, nc, "pkG", 1, [128, 128, 128], BF16)
            prep_step = wdt_prep(st)
            prep_k = 0

            def dv(fn, r, w):
                P.add("dve", fn, r=r, w=w)

            def scores(tt):
                qp, qpk = qpr.next()
                P.dma("sp", qp[:], QPT[:, :, tt * 128:(tt + 1) * 128].rearrange("g p t -> p g t"), w=[qpk])
                for b in range(4):
                    for g4 in range(4):
                        g = b * 4 + g4
                        P.add("pe", lambda e, g=g, g4=g4, b=b, qp=qp: e.matmul(
                            ps[b][:, g4 * 128:(g4 + 1) * 128], qp[:, g, :], kT[:, g, :], start=True, stop=True),
                            r=[qpk, "kT"] + ([f"ps{b}"] if g4 else []), w=[f"ps{b}"])
                    P.add("act", lambda e, b=b: e.copy(out=S[:, b * 4:(b + 1) * 4, :],
                                                       in_=ps[b][:].rearrange("p (g n) -> p g n", g=4)),
                          r=[f"ps{b}"], w=[f"S{g_}" for g_ in range(b * 4, b * 4 + 4)])

            def topk():
                G16 = range(16)
                for g in G16:
                    dv(lambda e, g=g: e.max(out=V[:, g, 0:8], in_=S[:, g, :]), [f"S{g}"], [f"Va{g}"])
                for g in G16:
                    dv(lambda e, g=g: e.max_index(out=IX[:, g, 0:8], in_max=V[:, g, 0:8], in_values=S[:, g, :]),
                       [f"S{g}", f"Va{g}"], [f"IXa{g}"])
                for g in G16:
                    dv(lambda e, g=g: e.match_replace(out=S2[:, g, :], in_to_replace=V[:, g, 0:8], in_values=S[:, g, :],
                                                      imm_value=NEG), [f"S{g}", f"Va{g}"], [f"S2{g}"])
                for g in G16:
                    dv(lambda e, g=g: e.max(out=V[:, g, 8:16], in_=S2[:, g, :]), [f"S2{g}"], [f"Vb{g}"])
                for g in G16:
                    dv(lambda e, g=g: e.max_index(out=IX[:, g, 8:16], in_max=V[:, g, 8:16], in_values=S2[:, g, :]),
                       [f"S2{g}", f"Vb{g}"], [f"IXb{g}"])
                allV = [f"Va{g}" for g in G16] + [f"Vb{g}" for g in G16]
                allIX = [f"IXa{g}" for g in G16] + [f"IXb{g}" for g in G16]
                dv(lambda e: e.tensor_copy(out=IXf[:], in_=IX[:]), allIX, ["IXf"])
                Vv = V[:].rearrange("p (h two) k -> p h two k", two=2)
                H8 = range(8)
                dv(lambda e, Vv=Vv: e.tensor_tensor(
                    out=cand[:].rearrange("p h (a b) -> p h a b", a=16),
                    in0=Vv[:, :, 0, :].unsqueeze(3).to_broadcast([128, 8, 16, 16]),
                    in1=Vv[:, :, 1, :].unsqueeze(2).to_broadcast([128, 8, 16, 16]), op=ALU.add), allV, [f"cand{h}" for h in H8])
                for h in H8:
                    dv(lambda e, h=h: e.max(out=Bv[:, h, 0:8], in_=cand[:, h, :]), [f"cand{h}"], [f"Ba{h}"])
                for h in H8:
                    dv(lambda e, h=h: e.max_index(out=PX[:, h, 0:8], in_max=Bv[:, h, 0:8], in_values=cand[:, h, :]),
                       [f"cand{h}", f"Ba{h}"], [f"PXa{h}"])
                for h in H8:
                    dv(lambda e, h=h: e.match_replace(out=cand2[:, h, :], in_to_replace=Bv[:, h, 0:8], in_values=cand[:, h, :],
                                                      imm_value=NEG), [f"cand{h}", f"Ba{h}"], [f"cand2{h}"])
                for h in H8:
                    dv(lambda e, h=h: e.max(out=Bv[:, h, 8:16], in_=cand2[:, h, :]), [f"cand2{h}"], [f"Bb{h}"])
                for h in H8:
                    dv(lambda e, h=h: e.max_index(out=PX[:, h, 8:16], in_max=Bv[:, h, 8:16], in_values=cand2[:, h, :]),
                       [f"cand2{h}", f"Bb{h}"], [f"PXb{h}"])
                allB = [f"Ba{h}" for h in H8] + [f"Bb{h}" for h in H8]
                allPX = [f"PXa{h}" for h in H8] + [f"PXb{h}" for h in H8]

            def topk_b():
                H8 = range(8)
                allB = [f"Ba{h}" for h in H8] + [f"Bb{h}" for h in H8]
                allPX = [f"PXa{h}" for h in H8] + [f"PXb{h}" for h in H8]
                dv(lambda e: e.tensor_tensor(out=E[:], in0=Bv[:], in1=Bv[:, :, 0:1].to_broadcast([128, 8, 16]), op=ALU.subtract),
                   allB, ["E"])
                P.add("act", lambda e: e.activation(out=E[:], in_=E[:], func=AF.Exp), r=["E"], w=["E"])
                dv(lambda e: e.tensor_reduce(out=Z[:], in_=E[:], axis=AX.X, op=ALU.add), ["E"], ["Z"])
                dv(lambda e: e.reciprocal(out=Z[:], in_=Z[:]), ["Z"], ["Z"])
                dv(lambda e: e.tensor_tensor(out=PG[:].rearrange("p (h k) -> p h k", h=8), in0=E[:],
                                             in1=Z[:].unsqueeze(2).to_broadcast([128, 8, 16]), op=ALU.mult), ["E", "Z"], ["PG"])
                dv(lambda e: e.tensor_single_scalar(out=R1u[:], in_=PX[:], scalar=4, op=ALU.logical_shift_right), allPX, ["R1u"])
                dv(lambda e: e.tensor_single_scalar(out=R2u[:], in_=PX[:], scalar=15, op=ALU.bitwise_and), allPX, ["R2u"])
                dv(lambda e: e.tensor_copy(out=R1[:], in_=R1u[:]), ["R1u"], ["R1"])
                dv(lambda e: e.tensor_copy(out=R2[:], in_=R2u[:]), ["R2u"], ["R2"])
                IXv = IXf[:].rearrange("p (h two) k -> p h two k", two=2)
                for (Rr, Rk, two, Pout, Pk) in ((R1, "R1", 0, PI, "PI"), (R2, "R2", 1, PJ, "PJ")):
                    dv(lambda e, Rr=Rr: e.tensor_tensor(
                        out=eq[:], in0=Rr[:].unsqueeze(3).to_broadcast([128, 8, 16, 16]),
                        in1=iot[:, 0:16].unsqueeze(1).unsqueeze(1).to_broadcast([128, 8, 16, 16]), op=ALU.is_equal),
                        [Rk, "iot"], ["eq"])
                    dv(lambda e, two=two, IXv=IXv: e.tensor_tensor(
                        out=eq[:], in0=eq[:], in1=IXv[:, :, two, :].unsqueeze(2).to_broadcast([128, 8, 16, 16]), op=ALU.mult),
                        ["eq", "IXf"], ["eq"])
                    dv(lambda e, Pout=Pout: e.tensor_reduce(out=Pout[:].rearrange("p (h k) -> p h k", h=8), in_=eq[:],
                                                            axis=AX.X, op=ALU.add), ["eq"], [Pk])

            scores(0)
            topk()
            topk_b()
            for tt in range(NTO):
                for n_, (Pin, Pk) in enumerate(((PI, "PI"), (PJ, "PJ"), (PG, "PG"))):
                    P.add("pe", lambda e, n_=n_, Pin=Pin: e.transpose(out=ps[4][:, n_ * 128:(n_ + 1) * 128], in_=Pin[:], identity=identf[:]),
                          r=[Pk, "identf"] + (["ps4"] if n_ else []), w=["ps4"])
                P.add("act", lambda e: e.copy(out=PT[:], in_=ps[4][:, 0:384].rearrange("p (a t) -> p a t", a=3)), r=["ps4"], w=["PT"])
                dv(lambda e: e.tensor_tensor(
                    out=OJ[:], in0=PT[:, 1, :].unsqueeze(2).to_broadcast([128, 128, 128]),
                    in1=iot[:].unsqueeze(1).to_broadcast([128, 128, 128]), op=ALU.is_equal), ["PT", "iot"], ["OJ"])
                P.add("pool", lambda e: e.tensor_tensor(
                    out=OJ[:], in0=OJ[:], in1=PT[:, 2, :].unsqueeze(2).to_broadcast([128, 128, 128]), op=ALU.mult),
                    r=["PT", "OJ"], w=["OJ"])
                dv(lambda e: e.tensor_tensor(
                    out=OI[:], in0=PT[:, 0, :].unsqueeze(2).to_broadcast([128, 128, 128]),
                    in1=iot[:].unsqueeze(1).to_broadcast([128, 128, 128]), op=ALU.is_equal), ["PT", "iot"], ["OI"])
                if tt + 1 < NTO:
                    scores(tt + 1)
                    topk()
                Gt, Gk = Gr.next()
                for sl_ in range(8):
                    prep_step(tt * 16 + sl_, NTO * 16, "act")
                for t4 in range(32):
                    b = 5 + (t4 % 2)

                    def gm(e, t4=t4, b=b):
                        ins = None
                        for u in range(4):
                            t_ = t4 * 4 + u
                            ins = e.matmul(ps[b][:, u * 128:(u + 1) * 128], OI[:, t_, :], OJ[:, t_, :], start=True, stop=True)
                        return ins
                    P.add("pe", gm, r=["OI", "OJ"], w=[f"ps{b}"])
                    outv = Gt[:, :, t4 * 4:(t4 + 1) * 4].rearrange("p j t -> p t j")
                    inv = ps[b][:].rearrange("p (t j) -> p t j", t=4)
                    P.add("act", lambda e, outv=outv, inv=inv: e.copy(out=outv, in_=inv), r=[f"ps{b}"], w=[Gk])
                    if t4 % 4 == 3:
                        prep_step(tt * 16 + 8 + t4 // 4, NTO * 16, "pool")
                    if t4 == 19 and tt + 1 < NTO:
                        topk_b()
                P.dma("act", GS[tt], Gt[:], r=[Gk], w=["GSd"])
            P.flush()

    def phase_peer_main():
        with ExitStack() as st:
            def sb(name, shape, dt):
                return st.enter_context(nc.sbuf_tensor("pm" + name, shape, dt))
            gB = sb("gB", [128, D], F32)
            P.dma("sp", gB[:], g_fin.partition_broadcast(128), w=["gB"])
            hgt = sb("hg", [128, 4, NCH, 128], BF16)
            acc = sb("acc", [128, 4, D], F32)
            WDr = Ring(st, nc, "pmWD", 4, [128, NCH, 128], BF16)
            WUr = Ring(st, nc, "pmWU", 2, [128, 2, D], BF16)
            Gr = Ring(st, nc, "pmG", 4, [128, 4, 2, 128], BF16)
            Dgr = Ring(st, nc, "pmDg", 2, [128, 2, 512], BF16)
            Ar = Ring(st, nc, "pmA", 3, [128, 2, 512], BF16)
            ss = sb("ss", [128, 4], F32)
            wuv = w_up.rearrange("(i j) d -> i j d", j=128)
            NJG = 64
            NUP = 4 * (D // 512)
            ui = 0
            for tg in range(OWN // 512):
                P.dma("sp", hgt[:], H2T[tg * 4:(tg + 1) * 4].rearrange("t p c k -> p t c k"), w=["hg"])
                P.dma("sp", acc[:], X1[tg * 512:(tg + 1) * 512, :].rearrange("(s p) d -> p s d", p=128), w=["acc"])
                stt = {}

                def load_down(jg, tg=tg, stt=stt):
                    d = stt.setdefault(jg, {})
                    d["WD"] = []
                    for jj in range(2):
                        WD, WDk = WDr.next()
                        P.dma("sp", WD[:], WDT[jg * 2 + jj], w=[WDk])
                        d["WD"].append((WD, WDk))
                    Gt, Gk = Gr.next()
                    P.dma("sp", Gt[:], GS[tg * 4:(tg + 1) * 4, :, jg * 2:(jg + 1) * 2, :].rearrange("s i j t -> i s j t"), w=[Gk])
                    d["G"] = (Gt, Gk)

                def load_up(jg, stt=stt):
                    WU, WUk = WUr.next()
                    P.dma("pool", WU[:], wuv[:, jg * 2:(jg + 1) * 2, :], w=[WUk])
                    stt.setdefault(jg, {})["WU"] = (WU, WUk)

                def down_pieces(jg, stt=stt):
                    d = stt[jg]
                    Dg, Dgk = Dgr.next()
                    A, Ak = Ar.next()
                    d["A"] = (A, Ak)
                    Gt, Gk = d["G"]
                    pieces = []
                    for k in range(NUP):
                        def piece(k=k, d=d, Dg=Dg, Dgk=Dgk, A=A, Ak=Ak, Gt=Gt, Gk=Gk):
                            ms = []
                            for m in (2 * k, 2 * k + 1):
                                jj, cc = m // NCH, m % NCH
                                ms.append((jj, cc))
                            jj = ms[0][0]
                            WD, WDk = d["WD"][jj]

                            def fn(e, ms=ms, WD=WD, jj=jj):
                                ins = None
                                for (_, cc) in ms:
                                    ins = e.matmul(ps[jj][:].rearrange("p (t k) -> p t k", t=4), WD[:, cc, :], hgt[:, :, cc, :],
                                                   start=(cc == 0), stop=(cc == NCH - 1))
                                return ins
                            P.add("pe", fn, r=[WDk, "hg"], w=[f"ps{jj}"])
                            if ms[-1][1] == NCH - 1:
                                P.add("act", lambda e, Dg=Dg, jj=jj: e.activation(out=Dg[:, jj, :], in_=ps[jj][:], func=AF.Gelu),
                                      r=[f"ps{jj}"], w=[Dgk])
                                if jj == 1:
                                    P.add("pool", lambda e, A=A, Dg=Dg, Gt=Gt: e.tensor_tensor(
                                        out=A[:].rearrange("p j (s t) -> p j s t", s=4),
                                        in0=Dg[:].rearrange("p j (s t) -> p j s t", s=4),
                                        in1=Gt[:].rearrange("p s j t -> p j s t"), op=ALU.mult), r=[Dgk, Gk], w=[Ak])
                        pieces.append(piece)
                    return pieces

                load_down(0)
                load_down(1)
                load_up(0)
                for pc in down_pieces(0):
                    pc()
                load_down(2)
                for pc in down_pieces(1):
                    pc()
                for jg in range(NJG):
                    if jg + 3 < NJG:
                        load_down(jg + 3)
                    if jg + 1 < NJG:
                        load_up(jg + 1)
                    pieces = down_pieces(jg + 2) if jg + 2 < NJG else []
                    A, Ak = stt[jg]["A"]
                    WU, WUk = stt[jg]["WU"]
                    k = 0
                    for s_ in range(4):
                        for dc in range(D // 512):
                            b = 2 + (ui % 6)
                            ui += 1
                            mm_group(P, ps[b][:], [(A[:, jj, s_ * 128:(s_ + 1) * 128], WU[:, jj, dc * 512:(dc + 1) * 512]) for jj in range(2)],
                                     r=[Ak, WUk], w=[f"ps{b}"])
                            P.add("dve", lambda e, s_=s_, dc=dc, b=b: e.tensor_tensor(
                                out=acc[:, s_, dc * 512:(dc + 1) * 512], in0=acc[:, s_, dc * 512:(dc + 1) * 512], in1=ps[b][:], op=ALU.add),
                                r=[f"ps{b}", "acc"], w=["acc"])
                            if pieces:
                                pieces[k]()
                            k += 1
                    del stt[jg]
                for s in range(4):
                    P.add("dve", lambda e, s=s: e.scalar_tensor_tensor(
                        out=hgt[:].rearrange("p a c k -> p (a c k)")[:, 0:D], in0=acc[:, s, :], scalar=1.0, in1=acc[:, s, :],
                        op0=ALU.mult, op1=ALU.mult, accum_out=ss[:, s:s + 1]), r=["acc", "hg"], w=["hg", "ss"])
                P.add("act", lambda e: e.activation(out=ss[:], in_=ss[:], func=AF.Sqrt, scale=1.0 / D, bias=epsT[:, 0:1]),
                      r=["ss", "eps"], w=["ss"])
                P.add("dve", lambda e: e.reciprocal(out=ss[:], in_=ss[:]), r=["ss"], w=["ss"])
                for s in range(4):
                    P.add("dve", lambda e, s=s: e.scalar_tensor_tensor(
                        out=acc[:, s, :], in0=acc[:, s, :], scalar=ss[:, s:s + 1], in1=gB[:], op0=ALU.mult, op1=ALU.mult),
                        r=["acc", "ss", "gB"], w=["acc"])
                P.dma("sp", out[tg * 512:(tg + 1) * 512, :].rearrange("(s p) d -> p s d", p=128), acc[:], r=["acc"], w=["outd"])
            P.flush()

    load_consts()
    stages = getattr(c, "stages", 99)
    phase_norm_T(x, g_mix, hT, NT, "na")
    if stages >= 2:
        phase_kv()
    if stages >= 3:
        phase_qc()
    if stages >= 4:
        phase_attn()
    if stages >= 5:
        phase_out()
    if stages >= 6:
        phase_norm_T(X1, g_ffn, H2T, NTO, "nf")
    if stages >= 7:
        phase_peer_prep()
    if stages >= 8:
        phase_peer_pick()
    if stages >= 9:
        phase_peer_main()
    gst.close()
    P.close()
    return nc


def rope_tables(SB):
    half = 64
    inv = (10000.0 ** (-np.arange(0, half, 2, dtype=np.float32) / half)).astype(np.float32)
    t = np.arange(SB)
    row = (t // 64).astype(np.float32)
    col = (t % 64).astype(np.float32)
    ang_r = row[:, None] * inv
    ang_c = col[:, None] * inv
    cosT = np.empty((128, SB), np.float32)
    sinT = np.empty((128, SB), np.float32)
    for blk, ang in ((0, ang_r), (1, ang_c)):
        c = np.cos(ang).T.astype(np.float32)
        s = np.sin(ang).T.astype(np.float32)
        o = blk * 64
        cosT[o:o + 32] = c
        cosT[o + 32:o + 64] = c
        sinT[o:o + 32] = -s
        sinT[o + 32:o + 64] = s
    return cosT, sinT


def make_in_maps(c, inputs, n_cores=8):
    D, SB, OWN = c.D, c.SB, c.OWN
    bf = ml_dtypes.bfloat16
    cosT, sinT = rope_tables(SB)
    perm = np.zeros((128, 128), np.float32)
    for d in range(128):
        perm[(d // 64) * 64 + ((d % 64) + 32) % 64, d] = 1.0
    iota = np.tile(np.arange(128, dtype=np.float32)[None, :], (128, 1))
    f = lambda a: np.ascontiguousarray(a, dtype=np.float32)
    shared = {
        "w_in": f(inputs["w_in"][0]), "w_out": f(inputs["w_out"][0]), "w_query": f(inputs["peer_w_query"][0]),
        "sub_keys": f(inputs["peer_sub_keys"][0].reshape(16, 128, 128)),
        "w_down": f(inputs["peer_w_down"][0]), "w_up": f(inputs["peer_w_up"][0]),
        "g_mix": f(inputs["norm_mix_g"][0][None, :]), "g_ffn": f(inputs["norm_ffn_g"][0][None, :]),
        "g_fin": f(inputs["norm_final_g"][None, :]),
        "qk_g": f(np.stack([inputs["q_norm_g"][0], inputs["k_norm_g"][0]], axis=1)),
        "conv_w": f(inputs["conv_w"][0].T.reshape(c.NCB, 128, 3).transpose(1, 0, 2)),
        "ao_g": f(inputs["attn_out_g"][0].reshape(c.NQH, 128).T),
        "co_g": f(inputs["conv_out_g"][0].reshape(c.NCB, 128).T),
        "ident": np.eye(128, dtype=np.float32).astype(bf), "identf": np.eye(128, dtype=np.float32),
        "perm": perm.astype(bf), "iota": iota,
    }
    maps = []
    for core in range(n_cores):
        b, qd = core // 4, core % 4
        sh = qd * OWN
        m = dict(shared)
        m["x"] = f(np.roll(inputs["x"][b], -sh, axis=0))
        m["cosT"] = f(np.roll(cosT, -sh, axis=1))
        m["sinT"] = f(np.roll(sinT, -sh, axis=1))
        edge = np.ones((128, 2), np.float32)
        if qd == 0:
            edge[:, 0] = 0.0
        if qd == 3:
            edge[:, 1] = 0.0
        m["edge"] = edge
        maps.append(m)
    return maps


def kernel(**inputs):
    inputs = {k: np.asarray(v) for k, v in inputs.items()}
    c = Cfg()
    nc = build(c)
    maps = make_in_maps(c, inputs)
    res = run_bass_kernel_spmd(nc, maps, core_ids=list(range(8)))
    outs = [np.asarray(r["out"], dtype=np.float32) for r in res.results]
    B = inputs["x"].shape[0]
    full = np.stack([np.concatenate(outs[b * 4:(b + 1) * 4], axis=0) for b in range(B)], axis=0)
    return full.astype(np.float32)
```
